# Optimizing a Trainium2 kernel written in Bass

```python
import jax
import jax.numpy as jnp
from jax import lax
import numpy as np

D_MODEL = 1024
BATCH = 1
SEQ = 16384
DEPTH = 2

GRID_W = 64
CTX_LEN = 256
HEAD_DIM = D_MODEL // 16
POOL_GROUPS = 4
POOL_WINDOWS = (2, 4, 8, 16)
POOL_GROUP_DIM = HEAD_DIM
POOL_DIM = POOL_GROUPS * POOL_GROUP_DIM
NA_HEADS = 8
NA_DIM = NA_HEADS * HEAD_DIM
NA_WIN_ROWS = 8
NA_WIN_COLS = 16
SG_HEADS = 4
SG_DIM = SG_HEADS * HEAD_DIM
SG_CHUNK = 128
MIX_DIM = POOL_DIM + NA_DIM + SG_DIM
Q_OFF = POOL_DIM
K_OFF = Q_OFF + NA_DIM
V_OFF = K_OFF + NA_DIM
U_OFF = V_OFF + NA_DIM
G_OFF = U_OFF + SG_DIM
IN_DIM = G_OFF + SG_DIM
N_EXPERTS = 16
N_GROUPS = 4
TOP_K = 2
D_EXPERT = D_MODEL // 2
EPS = 1e-6

kernel_name = 'hybrid_pool_natten_gmlp_moe_dit'


def rms_norm(x, g):
    xf = x.astype(jnp.float32)
    y = xf * lax.rsqrt(jnp.mean(xf * xf, axis=-1, keepdims=True) + EPS)
    return (y * g.astype(jnp.float32)).astype(x.dtype)


def modulate(x, shift, scale):
    return x * (1 + scale) + shift


def centred_pool_minus_self(x, window):
    L = x.shape[1]
    xf = x.astype(jnp.float32)
    cs = jnp.concatenate([jnp.zeros_like(xf[:, :1]), lax.cumsum(xf, axis=1)], axis=1)
    pos = jnp.arange(L)
    lo = jnp.clip(pos - window // 2, 0, L)
    hi = jnp.clip(pos + window // 2, 0, L)
    mean = (cs[:, hi] - cs[:, lo]) / (hi - lo).astype(jnp.float32)[None, :, None]
    return (mean - xf).astype(x.dtype)


def pool_mixer(p, w_pool, pool_scale):
    B, L, _ = p.shape
    pg = p.reshape(B, L, POOL_GROUPS, POOL_GROUP_DIM)
    y = jnp.stack([centred_pool_minus_self(pg[:, :, g], w) for g, w in enumerate(POOL_WINDOWS)], axis=2)
    y = jnp.einsum('blgc,gcd->blgd', y, w_pool)
    return y.reshape(B, L, POOL_DIM) * pool_scale


def spatial_gate(u, v, w_sg, b_sg, sg_norm):
    B, L, _ = u.shape
    nc = L // SG_CHUNK
    u = jax.nn.gelu(u)
    v = rms_norm(jax.nn.gelu(v).reshape(B, nc, SG_CHUNK, SG_HEADS, HEAD_DIM), sg_norm)
    mixed = jnp.einsum('hpq,bnqhd->bnphd', w_sg, v) + b_sg.T[:, :, None]
    return u * mixed.reshape(B, L, SG_DIM)


def neighbourhood_attention(q, k, v, k_ctx, v_ctx, rpb):
    B, N, H, dh = q.shape
    rows = N // GRID_W
    wr = min(NA_WIN_ROWS, rows)
    scale = HEAD_DIM ** -0.5
    qg = q.reshape(B, rows, GRID_W, H, dh)
    kg = k.reshape(B, rows, GRID_W, H, dh)
    vg = v.reshape(B, rows, GRID_W, H, dh)
    cols = jnp.arange(GRID_W)
    col_start = jnp.clip(cols - NA_WIN_COLS // 2, 0, GRID_W - NA_WIN_COLS)
    col_idx = col_start[:, None] + jnp.arange(NA_WIN_COLS)[None, :]
    dc = col_idx - cols[:, None] + NA_WIN_COLS - 1

    def one_row(r):
        rs = jnp.clip(r - wr // 2, 0, rows - wr)
        kb = lax.dynamic_slice_in_dim(kg, rs, wr, axis=1)[:, :, col_idx]
        vb = lax.dynamic_slice_in_dim(vg, rs, wr, axis=1)[:, :, col_idx]
        qr = lax.dynamic_index_in_dim(qg, r, axis=1, keepdims=False)
        dr = rs + jnp.arange(wr) - r + NA_WIN_ROWS - 1
        bias = rpb[:, dr[None, :, None], dc[:, None, :]]
        s_loc = jnp.einsum('bqhd,baqwhd->bhqaw', qr, kb).astype(jnp.float32) * scale + bias.astype(jnp.float32)[None]
        s_loc = s_loc.reshape(B, H, GRID_W, wr * NA_WIN_COLS)
        s_ctx = jnp.einsum('bqhd,bchd->bhqc', qr, k_ctx).astype(jnp.float32) * scale
        p = jax.nn.softmax(jnp.concatenate([s_loc, s_ctx], axis=-1), axis=-1)
        p_loc = p[..., :wr * NA_WIN_COLS].reshape(B, H, GRID_W, wr, NA_WIN_COLS).astype(v.dtype)
        p_ctx = p[..., wr * NA_WIN_COLS:].astype(v.dtype)
        return (jnp.einsum('bhqaw,baqwhd->bqhd', p_loc, vb)
                + jnp.einsum('bhqc,bchd->bqhd', p_ctx, v_ctx))

    out = lax.map(one_row, jnp.arange(rows))
    return jnp.moveaxis(out, 0, 1).reshape(B, N, H * dh)


def context_attention(q, k, v):
    B, Lc, H, dh = q.shape
    s = jnp.einsum('bqhd,bkhd->bhqk', q, k).astype(jnp.float32) * (HEAD_DIM ** -0.5)
    p = jax.nn.softmax(s, axis=-1).astype(v.dtype)
    return jnp.einsum('bhqk,bkhd->bqhd', p, v).reshape(B, Lc, H * dh)


def moe(h, w_router, b_router, w_gate, w_up, w_down):
    B, L, _ = h.shape
    logits = (h @ w_router).astype(jnp.float32) + b_router.astype(jnp.float32)
    scores = jax.nn.softmax(logits, axis=-1)
    sg = scores.reshape(B, L, N_GROUPS, N_EXPERTS // N_GROUPS)
    grp_score = lax.top_k(sg, TOP_K)[0].sum(-1)
    grp_mask = jax.nn.one_hot(jnp.argmax(grp_score, axis=-1), N_GROUPS, dtype=jnp.bool_)
    masked = jnp.where(grp_mask[..., None], sg, -1.0).reshape(B, L, N_EXPERTS)
    top_w, top_i = lax.top_k(masked, TOP_K)
    top_w = top_w / jnp.sum(top_w, axis=-1, keepdims=True)
    gates = jnp.sum(jax.nn.one_hot(top_i, N_EXPERTS, dtype=jnp.float32) * top_w[..., None], axis=-2)
    gates = gates.astype(h.dtype)
    y = jnp.zeros_like(h)
    for e in range(N_EXPERTS):
        he = jax.nn.silu(h @ w_gate[e]) * (h @ w_up[e])
        y = y + gates[..., e:e + 1] * (he @ w_down[e])
    return y


def setup_inputs(seed: int = 0) -> dict:
    key = jax.random.key(seed)
    ks = jax.random.split(key, 24)
    f32 = jnp.float32
    D = D_MODEL

    def nrm(k, shape, s):
        return jax.random.normal(k, shape, f32) * s

    return {
        'x': nrm(ks[0], (BATCH, SEQ, D), 1.0),
        'c': nrm(ks[1], (BATCH, D), 1.0),
        'ctx': nrm(ks[2], (BATCH, CTX_LEN, D), 1.0),
        'c_ctx': nrm(ks[3], (D,), 1.0),
        'w_ada': nrm(ks[4], (DEPTH, D, 6 * D), 0.5 * D ** -0.5),
        'b_ada': nrm(ks[5], (DEPTH, 6 * D), 0.02),
        'norm1': 1.0 + nrm(ks[6], (DEPTH, D), 0.01),
        'w_in': nrm(ks[7], (DEPTH, D, IN_DIM), D ** -0.5),
        'pool_w': nrm(ks[8], (DEPTH, POOL_GROUPS, POOL_GROUP_DIM, POOL_GROUP_DIM), POOL_GROUP_DIM ** -0.5),
        'pool_scale': 1.0 + nrm(ks[9], (DEPTH, POOL_DIM), 0.02),
        'q_norm': 1.0 + nrm(ks[10], (DEPTH, NA_HEADS, HEAD_DIM), 0.01),
        'k_norm': 1.0 + nrm(ks[11], (DEPTH, NA_HEADS, HEAD_DIM), 0.01),
        'rpb': nrm(ks[12], (DEPTH, NA_HEADS, 2 * NA_WIN_ROWS - 1, 2 * NA_WIN_COLS - 1), 0.1),
        'sg_w': nrm(ks[13], (DEPTH, SG_HEADS, SG_CHUNK, SG_CHUNK), SG_CHUNK ** -0.5),
        'sg_b': 1.0 + nrm(ks[14], (DEPTH, SG_HEADS, SG_CHUNK), 0.01),
        'sg_norm': 1.0 + nrm(ks[15], (DEPTH, SG_HEADS, HEAD_DIM), 0.01),
        'w_out': nrm(ks[16], (DEPTH, MIX_DIM, D), MIX_DIM ** -0.5),
        'norm2': 1.0 + nrm(ks[17], (DEPTH, D), 0.01),
        'w_router': nrm(ks[18], (D, N_EXPERTS), D ** -0.5),
        'b_router': nrm(ks[19], (N_EXPERTS,), 0.01),
        'w_gate': nrm(ks[20], (DEPTH, N_EXPERTS, D, D_EXPERT), D ** -0.5),
        'w_up': nrm(ks[21], (DEPTH, N_EXPERTS, D, D_EXPERT), D ** -0.5),
        'w_down': nrm(ks[22], (DEPTH, N_EXPERTS, D_EXPERT, D), D_EXPERT ** -0.5),
    }


def reference(x, c, ctx, c_ctx, w_ada, b_ada, norm1, w_in, pool_w, pool_scale, q_norm, k_norm, rpb,
              sg_w, sg_b, sg_norm, w_out, norm2, w_router, b_router, w_gate, w_up, w_down):
    cond_lat = jax.nn.silu(c)[:, None]
    cond_ctx = jax.nn.silu(c_ctx)[None, None]
    h_lat, h_ctx = x, ctx
    for l in range(DEPTH):
        last = l == DEPTH - 1
        sh1, sc1, g1, sh2, sc2, g2 = jnp.split(cond_lat @ w_ada[l] + b_ada[l], 6, axis=-1)
        csh1, csc1, cg1, csh2, csc2, cg2 = jnp.split(cond_ctx @ w_ada[l] + b_ada[l], 6, axis=-1)

        hn_ctx = modulate(rms_norm(h_ctx, norm1[l]), csh1, csc1)
        Bc, Lc, _ = hn_ctx.shape
        if last:
            a_c = hn_ctx @ w_in[l][:, K_OFF:U_OFF]
            kc_raw, vc_raw = a_c[..., :NA_DIM], a_c[..., NA_DIM:]
        else:
            a_c = hn_ctx @ w_in[l]
            kc_raw, vc_raw = a_c[..., K_OFF:V_OFF], a_c[..., V_OFF:U_OFF]
        k_ctx = rms_norm(kc_raw.reshape(Bc, Lc, NA_HEADS, HEAD_DIM), k_norm[l])
        v_ctx = vc_raw.reshape(Bc, Lc, NA_HEADS, HEAD_DIM)

        hn_lat = modulate(rms_norm(h_lat, norm1[l]), sh1, sc1)
        a = hn_lat @ w_in[l]
        B, N, _ = a.shape
        q = rms_norm(a[..., Q_OFF:K_OFF].reshape(B, N, NA_HEADS, HEAD_DIM), q_norm[l])
        k = rms_norm(a[..., K_OFF:V_OFF].reshape(B, N, NA_HEADS, HEAD_DIM), k_norm[l])
        v = a[..., V_OFF:U_OFF].reshape(B, N, NA_HEADS, HEAD_DIM)
        mix_lat = jnp.concatenate([
            pool_mixer(a[..., :Q_OFF], pool_w[l], pool_scale[l]),
            neighbourhood_attention(q, k, v, k_ctx, v_ctx, rpb[l]),
            spatial_gate(a[..., U_OFF:G_OFF], a[..., G_OFF:], sg_w[l], sg_b[l], sg_norm[l]),
        ], axis=-1)
        h_lat = h_lat + g1 * (mix_lat @ w_out[l])
        h_lat = h_lat + g2 * moe(modulate(rms_norm(h_lat, norm2[l]), sh2, sc2),
                                 w_router, b_router, w_gate[l], w_up[l], w_down[l])

        if not last:
            qc = rms_norm(a_c[..., Q_OFF:K_OFF].reshape(Bc, Lc, NA_HEADS, HEAD_DIM), q_norm[l])
            mix_ctx = jnp.concatenate([
                pool_mixer(a_c[..., :Q_OFF], pool_w[l], pool_scale[l]),
                context_attention(qc, k_ctx, v_ctx),
                spatial_gate(a_c[..., U_OFF:G_OFF], a_c[..., G_OFF:], sg_w[l], sg_b[l], sg_norm[l]),
            ], axis=-1)
            h_ctx = h_ctx + cg1 * (mix_ctx @ w_out[l])
            h_ctx = h_ctx + cg2 * moe(modulate(rms_norm(h_ctx, norm2[l]), csh2, csc2),
                                     w_router, b_router, w_gate[l], w_up[l], w_down[l])
    return h_lat
```

```python
from contextlib import ExitStack
import numpy as np
import concourse.bass as bass
import concourse.mybir as mybir
from concourse.bass_utils import run_bass_kernel_spmd

F32 = mybir.dt.float32
BF16 = mybir.dt.bfloat16
AF = mybir.ActivationFunctionType
ALU = mybir.AluOpType
AX = mybir.AxisListType

NCORES = 8
D = 1024
KC = 8
BLK = 256
NBL = 12
CTXB = 12
TA = 13 * BLK
TH = 11 * BLK
PIT = 8 + 12 * BLK + 16 + BLK + 8
PI_CTX = 8 + 12 * BLK + 16
NEG = -30000.0
EPS = 1e-6
NE = 16
FE = 512


def hslot(b):
    return 10 if b == CTXB else b - 1


class Prog:
    def __init__(self, nc, es):
        self.nc = nc
        self.es = es
        self.eng = {"pe": nc.tensor, "act": nc.scalar, "dve": nc.vector, "pool": nc.gpsimd, "sp": nc.sync}
        self.stream = {k: [] for k in self.eng}
        self.cnt = {k: 0 for k in self.eng}
        self.sems = {}
        self.dcnt = {}
        self.waited = {k: {} for k in self.eng}
        self.lastw = {}
        self.readers = {}
        for k in self.eng:
            self._sem("E_" + k)

    def _sem(self, key):
        if key not in self.sems:
            self.sems[key] = self.es.enter_context(self.nc.semaphore("s_" + key))
        return self.sems[key]

    def _cur(self, semkey):
        if semkey.startswith("E_"):
            return self.cnt[semkey[2:]]
        return 16 * self.dcnt[semkey]

    def _deps(self, eng, reads, writes):
        deps = {}
        def add(tok):
            if tok is None:
                return
            sk, val = tok
            if sk.startswith("D_"):
                val = self._cur(sk)
            if eng == "pe" and sk == "E_pe":
                return
            if deps.get(sk, 0) < val:
                deps[sk] = val
        for k in reads:
            add(self.lastw.get(k))
        for k in writes:
            add(self.lastw.get(k))
            for t in self.readers.get(k, ()):
                add(t)
        for sk, val in deps.items():
            if self.waited[eng].get(sk, 0) < val:
                self.waited[eng][sk] = val
                self.stream[eng].append(("w", sk, val))

    def _commit(self, tok, reads, writes):
        for k in reads:
            self.readers.setdefault(k, []).append(tok)
        for k in writes:
            self.lastw[k] = tok
            self.readers[k] = []

    def op(self, eng, fn, reads=(), writes=()):
        self._deps(eng, reads, writes)
        self.cnt[eng] += 1
        tok = ("E_" + eng, self.cnt[eng])
        self.stream[eng].append(("o", fn, "E_" + eng, 1))
        self._commit(tok, reads, writes)

    def dma(self, q, out, in_, reads, writes, dkey):
        sk = "D_" + dkey
        self._sem(sk)
        self.dcnt.setdefault(sk, 0)
        self._deps(q, reads, writes)
        self.dcnt[sk] += 1
        tok = (sk, 16 * self.dcnt[sk])
        self.stream[q].append(("o", lambda e: e.dma_start(out=out, in_=in_), sk, 16))
        self._commit(tok, reads, writes)

    def fence(self):
        for e in self.eng:
            for o in self.eng:
                if o == "sp":
                    continue
                v = self.cnt[o]
                if (e != o or e != "pe") and self.waited[e].get("E_" + o, 0) < v:
                    self.waited[e]["E_" + o] = v
                    self.stream[e].append(("w", "E_" + o, v))
            for sk, c in self.dcnt.items():
                if self.waited[e].get(sk, 0) < 16 * c:
                    self.waited[e][sk] = 16 * c
                    self.stream[e].append(("w", sk, 16 * c))

    def emit(self):
        nc = self.nc
        with nc.Block() as block:
            def replay(name):
                def body(e):
                    for it in self.stream[name]:
                        if it[0] == "w":
                            e.wait_ge(self.sems[it[1]], it[2])
                        else:
                            it[1](e).then_inc(self.sems[it[2]], it[3])
                return body
            block.tensor(replay("pe"))
            block.scalar(replay("act"))
            block.vector(replay("dve"))
            block.gpsimd(replay("pool"))
            block.sync(replay("sp"))


class Arena:
    def __init__(self, nc, nbytes):
        self.t = nc.alloc_sbuf_tensor("arena", [128, nbytes // 2], BF16)
        self.nbytes = nbytes
        self.top = 0
        self.hi = nbytes
        self.marks = []

    def alloc(self, shape, dt, hi=False):
        esz = 4 if dt == F32 else 2
        n = 1
        for s in shape[1:]:
            n *= s
        nb = (n * esz + 31) // 32 * 32
        if hi:
            self.hi -= nb
            off = self.hi
        else:
            off = self.top
            self.top += nb
        assert self.top <= self.hi, ("arena overflow", self.top, self.hi, self.nbytes)
        ap = self.t[:, off // 2: off // 2 + n * esz // 2]
        if dt == F32:
            ap = ap.bitcast(F32)
        ap = ap[0:shape[0], :]
        if len(shape) == 3:
            ap = ap.rearrange("p (a b) -> p a b", b=shape[2])
        elif len(shape) == 4:
            ap = ap.rearrange("p (a b c) -> p a b c", b=shape[2], c=shape[3])
        return ap

    def mark(self):
        self.marks.append(self.top)

    def release(self):
        self.top = self.marks.pop()


def build_program(debug=False, stop=None, b_experts=NE):
    nc = bass.Bass("TRN2", target_bir_lowering=False)
    es = ExitStack()

    def din(name, shape, dt=F32):
        return nc.dram_tensor(name, list(shape), dt, kind="ExternalInput").ap()

    xT = din("xT", [KC, 128, NBL * BLK])
    ctxT = din("ctxT", [KC, 128, BLK])
    cT = din("cT", [128, KC, 2])
    w_ada = din("w_ada", [2, D, 6 * D])
    b_adaT = din("b_adaT", [2, 128, 48])
    n1T = din("n1T", [2, 128, KC])
    n2T = din("n2T", [2, 128, KC])
    w_in = din("w_in", [2, D, 2304])
    pw_bd = din("pw_bd", [2, 128, 2, 128])
    pscaleT = din("pscaleT", [2, 128, 2])
    qgT = din("qgT", [2, 128, 4])
    kgT = din("kgT", [2, 128, 4])
    bt_in = din("bt", [2, 128, 8, 14 * 64])
    rm_in = din("rm", [128, 10, 6, 4])
    rc_in = din("rc", [128, 2 + 3 * 2 * 16])
    bvalid_in = din("bvalid", [128, 13])
    sgwT = din("sgwT", [2, 128, 4, 128])
    sgbT = din("sgbT", [2, 128, 2, 128])
    sggain = din("sggain", [2, 256])
    w_out = din("w_out", [2, D, D])
    w_router = din("w_router", [D, NE])
    b_router = din("b_router", [NE])
    w_gate = din("w_gate", [2, NE, D, FE])
    w_up = din("w_up", [2, NE, D, FE])
    w_down = din("w_down", [2, NE, FE, D])
    consts = din("consts", [128, 3 * 128 + 2 * 128])
    sel_in = din("sel", [NE, NE, 128])
    yT = nc.dram_tensor("yT", [KC, 128, 8 * BLK], F32, kind="ExternalOutput").ap()

    skind = "ExternalOutput" if debug else "Internal"
    QTd = nc.dram_tensor("QTd", [4, 128, TA], BF16, kind=skind).ap()
    KTd = nc.dram_tensor("KTd", [4, 128, TA], BF16, kind=skind).ap()
    Vd = nc.dram_tensor("Vd", [TA, 512], BF16, kind=skind).ap()
    VSd = nc.dram_tensor("VSd", [TA, 4 * 128], BF16, kind=skind).ap()
    PId = nc.dram_tensor("PId", [2, 128, PIT], F32, kind=skind).ap()
    Ud = nc.dram_tensor("Ud", [2, 128, TA], BF16, kind=skind).ap()
    HN2d = nc.dram_tensor("HN2d", [KC, 128, TH], BF16, kind=skind).ap()
    if debug:
        dbgH = nc.dram_tensor("dbgH", [KC, 128, TH], F32, kind="ExternalOutput").ap()
        dbgGT = nc.dram_tensor("dbgGT", [NE, TH], BF16, kind="ExternalOutput").ap()
        dbgAda = nc.dram_tensor("dbgAda", [128, 96], F32, kind="ExternalOutput").ap()
    done = [False]

    P = Prog(nc, es)
    A = Arena(nc, (nc.sbuf_bytes_remaining - 512) // 64 * 64)
    banks = [nc.alloc_psum_tensor("bank%d" % i, [128, 512], F32) for i in range(8)]

    H = A.alloc([128, KC, TH], F32)
    GT = A.alloc([NE, TH], BF16)
    RC = A.alloc([128, 2 + 3 * 2 * 16], F32)
    RCW = RC[:, 0:2]
    RCB = RC[:, 2:98].rearrange("p (s c t) -> p s c t", s=3, c=2)
    CONST = A.alloc([128, 128], F32)
    identF = CONST
    cbf = A.alloc([128, 4 * 128], BF16)
    onesBD = cbf[:, 0:128]
    onesA = cbf[:, 128:256]
    half = [cbf[:, 256:384], cbf[:, 384:512]]
    cTs = A.alloc([128, KC, 2], F32)
    condT = A.alloc([128, KC, 2], BF16)
    adaS = A.alloc([128, 48, 2], F32)
    A1t = A.alloc([128, KC, 2], F32)
    A2t = A.alloc([128, KC, 2], F32)
    bAda = A.alloc([128, 48], F32)
    n1s = A.alloc([128, KC], F32)
    n2s = A.alloc([128, KC], F32)
    qg = A.alloc([128, 4], F32)
    kg = A.alloc([128, 4], F32)
    pscale = A.alloc([128, 2], F32)
    bvalid = A.alloc([128, 13], F32)
    epsT = A.alloc([128, 1], F32)
    brbc = A.alloc([128, NE], F32)
    Wr = A.alloc([128, KC, NE], F32)
    RM = A.alloc([128, 10, 6, 4], BF16)
    sgB = A.alloc([128, 2, 128], F32)

    def ps_slot(bank, halfi=None):
        if halfi is None:
            return banks[bank][:, :], "PS%d" % bank
        return banks[bank][:, halfi * 256:(halfi + 1) * 256], "PS%d" % bank

    class Rot:
        def __init__(self, items):
            self.items = items
            self.i = 0
        def next(self):
            it = self.items[self.i % len(self.items)]
            self.i += 1
            return it

    P.dma("sp", CONST, consts[:, 0:128], [], ["CONST"], "CONST")
    P.dma("pool", cbf, consts[:, 128:640], [], ["cbf"], "cbf")
    P.dma("sp", RC, rc_in, [], ["RC"], "RC")
    P.dma("sp", cTs, cT, [], ["cTs"], "cTs")
    P.dma("sp", bvalid, bvalid_in, [], ["bvalid"], "bvalid")
    P.dma("sp", brbc, b_router.partition_broadcast(128), [], ["brbc"], "brbc")
    P.dma("sp", Wr, w_router.rearrange("(kc p) e -> p kc e", p=128), [], ["Wr"], "Wr")
    P.dma("pool", RM, rm_in, [], ["RM"], "RM")
    P.op("pool", lambda e: e.memset(epsT, EPS), [], ["eps"])
    if debug:
        P.op("pool", lambda e: e.memset(GT, 0.0), [], ["GT"])
    for kc in range(KC):
        P.dma("sp", H[:, kc, 0:10 * BLK], xT[kc, :, BLK:11 * BLK], [], ["H%d_%d" % (s, kc) for s in range(10)], "Hinit")
        P.dma("sp", H[:, kc, 10 * BLK:11 * BLK], ctxT[kc], [], ["H10_%d" % kc], "Hinit")
    P.op("act", lambda e: e.activation(out=condT, in_=cTs, func=AF.Silu), ["cTs"], ["condT"])

    A.mark()
    zt = A.alloc([128, 2, 16], F32)
    P.op("pool", lambda e: e.memset(zt, 0.0), [], ["zt"])
    P.dma("sp", PId[:, :, 0:8].rearrange("c p t -> p c t"), zt[:, :, 0:8], ["zt"], ["PIpad"], "zt")
    P.dma("sp", PId[:, :, 8 + 12 * BLK:8 + 12 * BLK + 16].rearrange("c p t -> p c t"), zt, ["zt"], ["PIpad"], "zt")
    P.dma("sp", PId[:, :, PI_CTX + BLK:PI_CTX + BLK + 8].rearrange("c p t -> p c t"), zt[:, :, 0:8], ["zt"], ["PIpad"], "zt")
    P.fence()
    A.release()

    dbg = {}

    for l in range(2):
        last = l == 1
        if done[0]:
            break
        A.mark()
        P.dma("sp", bAda, b_adaT[l], [], ["bAda"], "bAda")
        P.dma("sp", n1s, n1T[l], [], ["n1s"], "n1s")
        P.dma("sp", n2s, n2T[l], [], ["n2s"], "n2s")
        P.dma("sp", qg, qgT[l], [], ["qg"], "qg")
        P.dma("sp", kg, kgT[l], [], ["kg"], "kg")
        P.dma("sp", pscale, pscaleT[l], [], ["pscale"], "pscale")
        P.dma("sp", sgB, sgbT[l], [], ["sgB"], "sgB")
        wa = [A.alloc([128, KC, 768], BF16) for _ in range(2)]
        adaps, adak = ps_slot(7)
        for pc in range(8):
            wt = wa[pc % 2]
            wk = "wa%d" % (pc % 2)
            P.dma("pool", wt, w_ada[l, :, pc * 768:(pc + 1) * 768].rearrange("(kc p) n -> p kc n", p=128), [], [wk], wk)
            for jj in range(6):
                j = pc * 6 + jj
                for kc in range(KC):
                    P.op("pe", lambda e, wt=wt, jj=jj, kc=kc, j=j: e.matmul(adaps[:, 2 * j:2 * j + 2], wt[:, kc, jj * 128:(jj + 1) * 128], condT[:, kc, :], start=(kc == 0), stop=(kc == KC - 1)),
                         [wk, "condT"], [adak])
        P.op("dve", lambda e: e.tensor_tensor(out=adaS, in0=adaps[:, 0:96].rearrange("p (j w) -> p j w", w=2), in1=bAda.unsqueeze(2).to_broadcast([128, 48, 2]), op=ALU.add),
             [adak, "bAda"], ["adaS"])
        P.op("dve", lambda e: e.scalar_tensor_tensor(out=A1t, in0=adaS[:, 8:16, :], scalar=1.0, in1=n1s.unsqueeze(2).to_broadcast([128, KC, 2]), op0=ALU.add, op1=ALU.mult),
             ["adaS", "n1s"], ["A1t"])
        P.op("dve", lambda e: e.scalar_tensor_tensor(out=A2t, in0=adaS[:, 32:40, :], scalar=1.0, in1=n2s.unsqueeze(2).to_broadcast([128, KC, 2]), op0=ALU.add, op1=ALU.mult),
             ["adaS", "n2s"], ["A2t"])
        P.op("dve", lambda e: e.tensor_scalar(out=qg, in0=qg, scalar1=0.125, scalar2=None, op0=ALU.mult), ["qg"], ["qg"])
        P.fence()
        A.release()

        if debug and l == 0:
            P.dma("sp", dbgAda, adaS.rearrange("p j w -> p (j w)"), ["adaS"], ["dbgAda"], "dbgAda")
        if stop == "ada%d" % l:
            done[0] = True
            break

        def B1(kc, w): return adaS[:, kc, w:w + 1]
        def G1(kc, w): return adaS[:, 16 + kc, w:w + 1]
        def B2(kc, w): return adaS[:, 24 + kc, w:w + 1]
        def G2(kc, w): return adaS[:, 40 + kc, w:w + 1]

        A.mark()
        Win = A.alloc([128, KC, 2304], BF16)
        for kc in range(KC):
            P.dma("pool", Win[:, kc, :], w_in[l, kc * 128:(kc + 1) * 128, :], [], ["Win"], "Win")
        hi_save = A.hi
        Wout = A.alloc([128, KC, D], BF16, hi=True)
        BT = A.alloc([128, 8, 14 * 64], BF16, hi=True)
        P.dma("pool", BT, bt_in[l], [], ["BT"], "BT")
        for kc in range(KC):
            P.dma("pool", Wout[:, kc, :], w_out[l, kc * 128:(kc + 1) * 128, :], [], ["Wout"], "Wout")
        sgG = A.alloc([128, 256], F32)
        P.dma("sp", sgG, sggain[l].partition_broadcast(128), [], ["sgG"], "sgG")
        hn = A.alloc([128, KC, BLK], BF16)
        hn_b = A.alloc([128, KC, BLK], BF16)
        sqk = [A.alloc([128, BLK], BF16) for _ in range(2)]
        tmpk = [A.alloc([128, BLK], F32) for _ in range(2)]
        xtmp = A.alloc([128, KC, BLK], F32)
        rr = A.alloc([128, BLK], F32)
        rstd = A.alloc([128, BLK], F32)
        pis = [A.alloc([128, BLK], F32) for _ in range(2)]
        qs = [A.alloc([128, BLK], BF16) for _ in range(2)]
        sqb = [A.alloc([128, BLK], BF16) for _ in range(2)]
        r2 = [A.alloc([128, BLK], F32) for _ in range(2)]
        ri2 = [A.alloc([128, BLK], F32) for _ in range(2)]
        us = [A.alloc([128, BLK], BF16) for _ in range(2)]
        vun = [A.alloc([128, 512], BF16) for _ in range(2)]
        vspad = [A.alloc([128, 4, 128], BF16) for _ in range(2)]
        ggs = [A.alloc([128, 256], F32) for _ in range(2)]
        gsq = A.alloc([128, 256], F32)
        ss4 = A.alloc([128, 4], F32)
        for i in range(2):
            P.op("pool", lambda e, i=i: e.memset(vspad[i], 0.0), [], ["vspad%d" % i])
        rot_f = Rot([(1, 0), (2, 0), (3, 0)])
        rot_s = Rot([(0, 0)])
        rot_s2 = Rot([(6, 0), (7, 0)])
        rot_v = Rot([4, 5])
        cnt2 = [0]

        if l == 0:
            a1_blocks = [(CTXB, "full"), (0, "kvp")] + [(b, "full") for b in range(1, 11)] + [(11, "kvp")]
        else:
            a1_blocks = [(CTXB, "kv"), (1, "kvp")] + [(b, "full") for b in range(2, 10)] + [(10, "kvp")]

        hn_bufs = [hn, hn_b]

        def a1_norm(bi):
            (b, mode) = a1_blocks[bi]
            hb = bi % 2
            hn_ = hn_bufs[hb]
            w = 1 if b == CTXB else 0
            if l == 0 and b in (0, 11):
                if b == 0:
                    for kc in range(KC):
                        P.dma("sp", xtmp[:, kc, :], xT[kc, :, b * BLK:(b + 1) * BLK], [], ["xtmp"], "xtmp")
                hsrc = lambda kc: xtmp[:, kc, :]
                hkeys = lambda kc: ["xtmp"]
            else:
                s_ = hslot(b)
                hsrc = lambda kc, s_=s_: H[:, kc, s_ * BLK:(s_ + 1) * BLK]
                hkeys = lambda kc, s_=s_: ["H%d_%d" % (s_, kc)]
            (sb_, sh_) = rot_s.next()
            ssp, ssk = ps_slot(sb_, sh_)
            for kc in range(KC):
                i = cnt2[0] % 2
                cnt2[0] += 1
                if kc % 2 == 0:
                    P.op("act", lambda e, kc=kc, i=i, hsrc=hsrc: e.activation(out=sqk[i], in_=hsrc(kc), func=AF.Square), hkeys(kc), ["sqk%d" % i])
                else:
                    P.op("dve", lambda e, kc=kc, i=i, hsrc=hsrc: e.tensor_tensor(out=sqk[i], in0=hsrc(kc), in1=hsrc(kc), op=ALU.mult), hkeys(kc), ["sqk%d" % i])
                P.op("pe", lambda e, kc=kc, i=i, ssp=ssp: e.matmul(ssp, onesA, sqk[i], start=(kc == 0), stop=(kc == KC - 1)), ["sqk%d" % i, "cbf"], [ssk])
            P.op("act", lambda e, ssp=ssp: e.activation(out=rr, in_=ssp, func=AF.Ln, bias=epsT, scale=1.0 / D), [ssk, "eps"], ["rr"])
            P.op("act", lambda e: e.activation(out=rstd, in_=rr, func=AF.Exp, scale=-0.5), ["rr"], ["rstd"])
            for kc in range(KC):
                i = kc % 2
                if kc % 2 == 0:
                    P.op("dve", lambda e, kc=kc, i=i, hsrc=hsrc: e.tensor_tensor(out=tmpk[i], in0=hsrc(kc), in1=rstd, op=ALU.mult), hkeys(kc) + ["rstd"], ["tmpk%d" % i])
                    P.op("act", lambda e, kc=kc, i=i, w=w, hn_=hn_: e.activation(out=hn_[:, kc, :], in_=tmpk[i], func=AF.Identity, bias=B1(kc, w), scale=A1t[:, kc, w:w + 1]),
                         ["tmpk%d" % i, "adaS", "A1t"], ["hn%d_%d" % (hb, kc)])
                else:
                    P.op("pool", lambda e, kc=kc, i=i, hsrc=hsrc: e.tensor_tensor(out=tmpk[i], in0=hsrc(kc), in1=rstd, op=ALU.mult), hkeys(kc) + ["rstd"], ["tmpk%d" % i])
                    P.op("dve", lambda e, kc=kc, i=i, w=w, hn_=hn_: e.tensor_scalar(out=hn_[:, kc, :], in0=tmpk[i], scalar1=A1t[:, kc, w:w + 1], scalar2=B1(kc, w), op0=ALU.mult, op1=ALU.add),
                         ["tmpk%d" % i, "adaS", "A1t"], ["hn%d_%d" % (hb, kc)])

        a1_norm(0)
        for bi, (b, mode) in enumerate(a1_blocks):
            if bi + 1 < len(a1_blocks):
                a1_norm(bi + 1)
            if l == 0 and bi == 1:
                for kc in range(KC):
                    P.dma("sp", xtmp[:, kc, :], xT[kc, :, 11 * BLK:12 * BLK], [], ["xtmp"], "xtmp")
            hb = bi % 2
            hn = hn_bufs[hb]
            w = 1 if b == CTXB else 0
            tok0 = b * BLK

            def fchunk(col0):
                (fb, fh) = rot_f.next()
                pp, pk = ps_slot(fb, fh)
                for kc in range(KC):
                    P.op("pe", lambda e, kc=kc, pp=pp, hn=hn: e.matmul(pp, Win[:, kc, col0:col0 + 128], hn[:, kc, :], start=(kc == 0), stop=(kc == KC - 1)),
                         ["Win", "hn%d_%d" % (hb, kc)], [pk])
                return pp, pk

            if mode in ("full", "kvp"):
                for c in range(2):
                    pp, pk = fchunk(c * 128)
                    i = c
                    P.op("dve", lambda e, pp=pp, i=i, b=b: e.tensor_scalar(out=pis[i], in0=pp, scalar1=bvalid[:, b:b + 1], scalar2=None, op0=ALU.mult), [pk, "bvalid"], ["pis%d" % i])
                    pt0 = (PI_CTX if b == CTXB else 8 + b * BLK)
                    P.dma("sp", PId[c, :, pt0:pt0 + BLK], pis[i], ["pis%d" % i], ["PI_%d" % b], "pis%d" % i)
            qk_list = []
            if mode == "full":
                qk_list += [("q", c) for c in range(4)]
            qk_list += [("k", c) for c in range(4)]
            def qk_finish(pend):
                (which, c, pp, pk, i) = pend
                (s2b, s2h) = rot_s2.next()
                sp2, sk2 = ps_slot(s2b, s2h)
                P.op("pe", lambda e, sp2=sp2, i=i: e.matmul(sp2, onesBD, sqb[i], start=True, stop=True), ["sqb%d" % i, "cbf"], [sk2])
                P.op("act", lambda e, sp2=sp2, i=i: e.activation(out=r2[i], in_=sp2, func=AF.Ln, bias=epsT, scale=1.0 / 64), [sk2, "eps"], ["r2%d" % i])
                P.op("act", lambda e, i=i: e.activation(out=ri2[i], in_=r2[i], func=AF.Exp, scale=-0.5), ["r2%d" % i], ["ri2%d" % i])
                gtab = qg if which == "q" else kg
                P.op("dve", lambda e, pp=pp, i=i, c=c, gtab=gtab: e.scalar_tensor_tensor(out=qs[i], in0=pp, scalar=gtab[:, c:c + 1], in1=ri2[i], op0=ALU.mult, op1=ALU.mult),
                     [pk, "ri2%d" % i, "qg", "kg"], ["qs%d" % i])
                dst = (QTd if which == "q" else KTd)[c, :, tok0:tok0 + BLK]
                P.dma("sp", dst, qs[i], ["qs%d" % i], ["%s_%d" % (which.upper(), b)], "qs%d" % i)

            pend = None
            for (which, c) in qk_list:
                col0 = (256 if which == "q" else 768) + c * 128
                pp, pk = fchunk(col0)
                i = cnt2[0] % 2
                cnt2[0] += 1
                P.op("act", lambda e, pp=pp, i=i: e.activation(out=sqb[i], in_=pp, func=AF.Square), [pk], ["sqb%d" % i])
                if pend is not None:
                    qk_finish(pend)
                pend = (which, c, pp, pk, i)
            qk_finish(pend)
            if mode == "full":
                for c in range(2):
                    pp, pk = fchunk(1792 + c * 128)
                    i = c
                    P.op("act", lambda e, pp=pp, i=i: e.activation(out=us[i], in_=pp, func=AF.Gelu_apprx_tanh), [pk], ["us%d" % i])
                    P.dma("sp", Ud[c, :, tok0:tok0 + BLK], us[i], ["us%d" % i], ["U_%d" % b], "us%d" % i)
            for th in range(2):
                vb = rot_v.next()
                pv, pvk = ps_slot(vb)
                for kc in range(KC):
                    P.op("pe", lambda e, kc=kc, pv=pv, th=th, hn=hn: e.matmul(pv, hn[:, kc, th * 128:(th + 1) * 128], Win[:, kc, 1280:1792], start=(kc == 0), stop=(kc == KC - 1)),
                         ["Win", "hn%d_%d" % (hb, kc)], [pvk])
                i = th
                if th == 0:
                    P.op("act", lambda e, pv=pv, i=i: e.activation(out=vun[i], in_=pv, func=AF.Copy), [pvk], ["vun%d" % i])
                else:
                    P.op("dve", lambda e, pv=pv, i=i: e.tensor_copy(out=vun[i], in_=pv), [pvk], ["vun%d" % i])
                P.dma("sp", Vd[tok0 + th * 128:tok0 + (th + 1) * 128, :], vun[i], ["vun%d" % i], ["V_%d" % b], "vun%d" % i)
            if mode == "full":
                for th in range(2):
                    (gb_, gh_) = rot_s2.next()
                    pg, pgk = ps_slot(gb_, gh_)
                    for kc in range(KC):
                        P.op("pe", lambda e, kc=kc, pg=pg, th=th, hn=hn: e.matmul(pg, hn[:, kc, th * 128:(th + 1) * 128], Win[:, kc, 2048:2304], start=(kc == 0), stop=(kc == KC - 1)),
                             ["Win", "hn%d_%d" % (hb, kc)], [pgk])
                    P.op("act", lambda e, pg=pg, th=th: e.activation(out=ggs[th], in_=pg, func=AF.Gelu_apprx_tanh), [pgk], ["gg%d" % th])
                for th in range(2):
                    gg_ = ggs[th]
                    ggk = "gg%d" % th
                    P.op("pool", lambda e, gg_=gg_: e.tensor_tensor(out=gsq, in0=gg_, in1=gg_, op=ALU.mult), [ggk], ["gsq"])
                    P.op("dve", lambda e: e.tensor_reduce(out=ss4, in_=gsq.rearrange("p (a b) -> p a b", b=64), axis=AX.X, op=ALU.add), ["gsq"], ["ss4"])
                    P.op("act", lambda e: e.activation(out=ss4, in_=ss4, func=AF.Ln, bias=epsT, scale=1.0 / 64), ["ss4", "eps"], ["ss4"])
                    P.op("act", lambda e: e.activation(out=ss4, in_=ss4, func=AF.Exp, scale=-0.5), ["ss4"], ["ss4"])
                    P.op("dve", lambda e, gg_=gg_: e.tensor_tensor(out=gsq.rearrange("p (a b) -> p a b", b=64), in0=gg_.rearrange("p (a b) -> p a b", b=64),
                                                         in1=ss4.unsqueeze(2).to_broadcast([128, 4, 64]), op=ALU.mult), [ggk, "ss4"], ["gsq"])
                    i = th
                    for s in range(2):
                        srcg = gsq.rearrange("p (j s d) -> p j s d", s=2, d=64)[:, :, s, :]
                        gng = sgG.rearrange("p (j s d) -> p j s d", s=2, d=64)[:, :, s, :]
                        dstg = vspad[i].rearrange("p (j s) c -> p j s c", s=2)[:, :, s, 64 * s:64 * s + 64]
                        P.op("dve", lambda e, srcg=srcg, gng=gng, dstg=dstg: e.tensor_tensor(out=dstg, in0=srcg, in1=gng, op=ALU.mult), ["gsq", "sgG"], ["vspad%d" % i])
                    P.dma("sp", VSd[tok0 + th * 128:tok0 + (th + 1) * 128, :], vspad[i].rearrange("p h c -> p (h c)"), ["vspad%d" % i], ["VS_%d" % b], "vspad%d" % i)
        P.fence()
        A.release()
        if stop == "A1_%d" % l:
            done[0] = True
            break

        A.mark()
        PW = A.alloc([128, 2, 128], BF16)
        P.dma("pool", PW, pw_bd[l], [], ["PW"], "PW")
        SGW = A.alloc([128, 4, 128], BF16)
        P.dma("pool", SGW, sgwT[l], [], ["SGW"], "SGW")
        KTc = A.alloc([128, 4, BLK], BF16)
        Vc = A.alloc([128, 2, 512], BF16)
        P.dma("sp", KTc, KTd[:, :, CTXB * BLK:(CTXB + 1) * BLK].rearrange("c p t -> p c t"), ["K_%d" % CTXB], ["KTc"], "KTc")
        P.dma("sp", Vc, Vd[CTXB * BLK:(CTXB + 1) * BLK, :].rearrange("(c p) n -> p c n", p=128), ["V_%d" % CTXB], ["Vc"], "Vc")
        QTz = A.alloc([128, 4, 2 * BLK], BF16)
        P.op("pool", lambda e: e.memset(QTz, 0.0), [], ["QTz"])
        KTw = A.alloc([128, 4, 3 * BLK], BF16)
        Vw = A.alloc([128, 6, 512], BF16)
        PIw = A.alloc([128, 2, BLK + 16], F32)
        Ub = A.alloc([128, 2, BLK], BF16)
        VSp = A.alloc([128, 2, 4 * 128], BF16)
        mix = A.alloc([128, KC, BLK], BF16)
        ym = A.alloc([128, BLK], F32)
        ymb = A.alloc([128, BLK], BF16)
        tS = [A.alloc([128, 2 * BLK], F32) for _ in range(4)]
        eS = [A.alloc([128, 2 * BLK], BF16) for _ in range(4)]
        pT = [A.alloc([128, 2 * BLK], BF16) for _ in range(8)]
        rden = A.alloc([128, 2 * BLK], F32)
        rdi = A.alloc([128, 2 * BLK], F32)
        sl = [tS[0][:, 0:BLK + 16], tS[1][:, 0:BLK + 16], rden[:, 0:BLK + 16], rdi[:, 0:BLK + 16]]
        slk = ["tS0", "tS1", "rden", "rdi"]
        sgt = A.alloc([128, 128], F32)
        hn2b = A.alloc([128, KC, BLK], BF16)
        hn2f = [A.alloc([128, BLK], F32) for _ in range(2)]
        sq2 = [A.alloc([128, BLK], BF16) for _ in range(2)]
        tmp2 = [A.alloc([128, BLK], F32) for _ in range(2)]
        rr2 = A.alloc([128, BLK], F32)
        rstd2 = A.alloc([128, BLK], F32)
        lg = A.alloc([128, 2, NE], F32)
        ex = A.alloc([128, 2, NE], F32)
        mx = A.alloc([128, 8, 2], F32)
        sc = A.alloc([128, 2, NE], F32)
        sc2 = A.alloc([128, 2, NE], F32)
        gs = A.alloc([128, 8], F32)
        gm = A.alloc([128, 8], F32)
        msk = A.alloc([128, 2, NE], F32)
        gts_bufs = [A.alloc([128, 2, NE], F32) for _ in range(2)]
        rctr = [0]
        pending_tail = []

        rot_st = Rot([(0, 0), (1, 0), (4, 0), (5, 0)])
        rot_o = Rot([(2, 0), (6, 0)])
        rot_d = Rot([(3, 0), (7, 0)])
        rot_w = Rot([(4, 0), (5, 0)])
        rot_m = Rot([(6, 0)])
        rot_r = Rot([(7, 0)])
        ctr = [0]

        a2_blocks = ([CTXB] + list(range(1, 11))) if l == 0 else list(range(2, 10))

        def load_att(b):
            tok0 = b * BLK
            P.dma("sp", QTz[0:64, :, 0:BLK], QTd[:, 0:64, tok0:tok0 + BLK].rearrange("c p t -> p c t"), ["Q_%d" % b], ["QTz"], "QTz")
            P.dma("sp", QTz[64:128, :, BLK:2 * BLK], QTd[:, 64:128, tok0:tok0 + BLK].rearrange("c p t -> p c t"), ["Q_%d" % b], ["QTz"], "QTz")
            if b != CTXB:
                P.dma("sp", KTw, KTd[:, :, tok0 - BLK:tok0 + 2 * BLK].rearrange("c p t -> p c t"), ["K_%d" % (b - 1), "K_%d" % b, "K_%d" % (b + 1)], ["KTw"], "KTw")
                P.dma("sp", Vw, Vd[tok0 - BLK:tok0 + 2 * BLK, :].rearrange("(c p) n -> p c n", p=128), ["V_%d" % (b - 1), "V_%d" % b, "V_%d" % (b + 1)], ["Vw"], "Vw")

        def load_pool(b):
            if b != CTXB:
                P.dma("sp", PIw, PId[:, :, b * BLK:b * BLK + BLK + 16].rearrange("c p t -> p c t"), ["PI_%d" % (b - 1), "PI_%d" % b, "PI_%d" % (b + 1), "PIpad"], ["PIw"], "PIw")
            else:
                P.dma("sp", PIw, PId[:, :, PI_CTX - 8:PI_CTX + BLK + 8].rearrange("c p t -> p c t"), ["PI_%d" % b, "PIpad"], ["PIw"], "PIw")

        def load_sg(b):
            tok0 = b * BLK
            P.dma("sp", Ub, Ud[:, :, tok0:tok0 + BLK].rearrange("c p t -> p c t"), ["U_%d" % b], ["Ub"], "Ub")
            P.dma("sp", VSp, VSd[tok0:tok0 + BLK, :].rearrange("(c p) n -> p c n", p=128), ["VS_%d" % b], ["VSp"], "VSp")

        for b in a2_blocks:
            isctx = b == CTXB
            w = 1 if isctx else 0
            tok0 = b * BLK
            s_ = hslot(b)
            if b == a2_blocks[0]:
                load_att(b)
                load_pool(b)
                load_sg(b)

            rcslot = 3 if isctx else (1 if b == 2 else (2 if b == 9 else 0))
            for c in range(2):
                x_ = PIw[:, c, :]
                P.op("pool", lambda e, x_=x_: e.tensor_tensor(out=sl[0][:, 1:272], in0=x_[:, 0:271], in1=x_[:, 1:272], op=ALU.add), ["PIw"], [slk[0]])
                P.op("pool", lambda e: e.tensor_tensor(out=sl[1][:, 2:271], in0=sl[0][:, 1:270], in1=sl[0][:, 3:272], op=ALU.add), [slk[0]], [slk[1]])
                lev = [0, 1]
                if c == 1:
                    P.op("pool", lambda e: e.tensor_tensor(out=sl[2][:, 4:269], in0=sl[1][:, 2:267], in1=sl[1][:, 6:271], op=ALU.add), [slk[1]], [slk[2]])
                    P.op("pool", lambda e: e.tensor_tensor(out=sl[3][:, 8:265], in0=sl[2][:, 4:261], in1=sl[2][:, 12:269], op=ALU.add), [slk[2]], [slk[3]])
                    lev = [2, 3]
                for hh in range(2):
                    pr = slice(64 * hh, 64 * hh + 64)
                    lv = lev[hh]
                    P.op("pool", lambda e, pr=pr, lv=lv, c=c: e.tensor_tensor(out=ym[pr, :], in0=sl[lv][pr, 8:264], in1=RCW[pr, c:c + 1].to_broadcast([64, BLK]), op=ALU.mult),
                         [slk[lv], "RC"], ["ym"])
                    if rcslot in (1, 3):
                        P.op("pool", lambda e, pr=pr, lv=lv, c=c, rcslot=rcslot: e.tensor_tensor(out=ym[pr, 0:8], in0=sl[lv][pr, 8:16], in1=RCB[pr, rcslot - 1, c, 0:8], op=ALU.mult),
                             [slk[lv], "RC", "ym"], ["ym"])
                    if rcslot in (2, 3):
                        P.op("pool", lambda e, pr=pr, lv=lv, c=c, rcslot=rcslot: e.tensor_tensor(out=ym[pr, 248:256], in0=sl[lv][pr, 256:264], in1=RCB[pr, rcslot - 1, c, 8:16], op=ALU.mult),
                             [slk[lv], "RC", "ym"], ["ym"])
                P.op("pool", lambda e, x_=x_: e.tensor_tensor(out=ymb, in0=ym, in1=x_[:, 8:264], op=ALU.subtract), ["ym", "PIw"], ["ymb"])
                (mb, mh) = rot_m.next()
                pm, pmk = ps_slot(mb, mh)
                P.op("pe", lambda e, pm=pm, c=c: e.matmul(pm, PW[:, c, :], ymb, start=True, stop=True), ["PW", "ymb"], [pmk])
                P.op("act", lambda e, pm=pm, c=c: e.activation(out=mix[:, c, :], in_=pm, func=AF.Copy, scale=pscale[:, c:c + 1]), [pmk, "pscale"], ["mix%d" % c])

            nxt_ = a2_blocks[a2_blocks.index(b) + 1] if a2_blocks.index(b) + 1 < len(a2_blocks) else None
            if nxt_ is not None:
                load_pool(nxt_)
            for th in range(2):
                for j in range(2):
                    (mb, mh) = rot_m.next()
                    pm, pmk = ps_slot(mb, mh)
                    pmv = pm[:, 0:128]
                    for s in range(2):
                        hh = 2 * j + s
                        P.op("pe", lambda e, pmv=pmv, th=th, hh=hh, s=s: e.matmul(pmv, VSp[:, th, hh * 128:(hh + 1) * 128], SGW[:, hh, :], start=(s == 0), stop=(s == 1)),
                             ["VSp", "SGW"], [pmk])
                    P.op("dve", lambda e, pmv=pmv, j=j: e.tensor_tensor(out=sgt, in0=pmv, in1=sgB[:, j, :], op=ALU.add), [pmk, "sgB"], ["sgt"])
                    P.op("dve", lambda e, th=th, j=j: e.tensor_tensor(out=mix[:, 6 + j, th * 128:(th + 1) * 128], in0=sgt, in1=Ub[:, j, th * 128:(th + 1) * 128], op=ALU.mult),
                         ["sgt", "Ub"], ["mix%d" % (6 + j)])

            if nxt_ is not None:
                load_sg(nxt_)
            chunks = ([] if isctx else [("l", c) for c in range(6)]) + [("c", c) for c in range(2)]
            nch = len(chunks)
            items = []
            for j in range(4):
                for ic, (kind, c) in enumerate(chunks):
                    items.append((j, kind, c, ic))
            DEPTH = 6
            pair_ps = {}
            item_buf = {}

            def att_stage1(idx):
                (j, kind, c, ic) = items[idx]
                (sb2, sh2) = rot_st.next()
                pst, pstk = ps_slot(sb2)
                ii = ctr[0] % 4
                i4 = ctr[0] % 8
                ctr[0] += 1
                item_buf[idx] = i4
                if kind == "l":
                    P.op("pe", lambda e, pst=pst, j=j, c=c: e.matmul(pst, KTw[:, j, c * 128:(c + 1) * 128], QTz[:, j, :], start=True, stop=True),
                         ["KTw", "QTz"], [pstk])
                    P.op("dve", lambda e, pst=pst, ii=ii, j=j, c=c: e.tensor_tensor(out=tS[ii].rearrange("p (h q) -> p h q", h=2), in0=pst.rearrange("p (h q) -> p h q", h=2),
                                                                               in1=BT[:, 2 * j:2 * j + 2, (10 - 2 * c) * 64:(14 - 2 * c) * 64], op=ALU.add),
                         [pstk, "BT"], ["tS%d" % ii])
                    P.op("act", lambda e, ii=ii: e.activation(out=eS[ii], in_=tS[ii], func=AF.Exp), ["tS%d" % ii], ["eS%d" % ii])
                    P.op("pool", lambda e, ii=ii, i4=i4, c=c, b=b: e.tensor_tensor(out=pT[i4].rearrange("p (h a q) -> p h a q", h=2, q=64), in0=eS[ii].rearrange("p (h a q) -> p h a q", h=2, q=64),
                                                                            in1=RM[:, b - 1, c, :].unsqueeze(1).unsqueeze(3).to_broadcast([128, 2, 4, 64]), op=ALU.mult),
                         ["eS%d" % ii, "RM"], ["pT%d" % i4])
                else:
                    P.op("pe", lambda e, pst=pst, j=j, c=c: e.matmul(pst, KTc[:, j, c * 128:(c + 1) * 128], QTz[:, j, :], start=True, stop=True),
                         ["KTc", "QTz"], [pstk])
                    P.op("act", lambda e, pst=pst, i4=i4: e.activation(out=pT[i4], in_=pst, func=AF.Exp), [pstk], ["pT%d" % i4])

            def att_stage2(idx):
                (j, kind, c, ic) = items[idx]
                i4 = item_buf[idx]
                if ic == 0:
                    pair_ps[j] = (ps_slot(rot_o.next()[0]), ps_slot(rot_d.next()[0]))
                (po, pok), (pd, pdk) = pair_ps[j]
                if kind == "l":
                    vl, vk = Vw[:, c, j * 128:(j + 1) * 128], "Vw"
                else:
                    vl, vk = Vc[:, c, j * 128:(j + 1) * 128], "Vc"
                P.op("pe", lambda e, po=po, vl=vl, i4=i4, ic=ic, nch=nch: e.matmul(po, vl, pT[i4], start=(ic == 0), stop=(ic == nch - 1)), [vk, "pT%d" % i4], [pok])
                P.op("pe", lambda e, pd=pd, i4=i4, ic=ic, nch=nch: e.matmul(pd, onesA, pT[i4], start=(ic == 0), stop=(ic == nch - 1)), ["cbf", "pT%d" % i4], [pdk])
                if ic == nch - 1:
                    P.op("act", lambda e, pd=pd: e.activation(out=rden, in_=pd, func=AF.Ln), [pdk], ["rden"])
                    P.op("act", lambda e: e.activation(out=rdi, in_=rden, func=AF.Exp, scale=-1.0), ["rden"], ["rdi"])
                    for s in range(2):
                        pr = slice(64 * s, 64 * s + 64)
                        P.op("dve", lambda e, po=po, j=j, pr=pr, s=s: e.tensor_tensor(out=mix[pr, 2 + j, :], in0=po[pr, s * 256:(s + 1) * 256], in1=rdi[pr, s * 256:(s + 1) * 256], op=ALU.mult),
                             [pok, "rdi"], ["mix%d" % (2 + j)])

            for idx in range(len(items) + DEPTH):
                if idx < len(items):
                    att_stage1(idx)
                if idx - DEPTH >= 0:
                    att_stage2(idx - DEPTH)
                if idx == 3:
                    while pending_tail:
                        pending_tail.pop(0)()
            nxt = a2_blocks[a2_blocks.index(b) + 1] if a2_blocks.index(b) + 1 < len(a2_blocks) else None
            if nxt is not None:
                load_att(nxt)

            for dc in range(KC):
                (wb_, wh_) = rot_w.next()
                pw_, pwk = ps_slot(wb_, wh_)
                for m in range(KC):
                    P.op("pe", lambda e, pw_=pw_, m=m, dc=dc: e.matmul(pw_, Wout[:, m, dc * 128:(dc + 1) * 128], mix[:, m, :], start=(m == 0), stop=(m == KC - 1)),
                         ["Wout", "mix%d" % m], [pwk])
                hv = H[:, dc, s_ * BLK:(s_ + 1) * BLK]
                P.op("dve", lambda e, pw_=pw_, hv=hv, dc=dc, w=w: e.scalar_tensor_tensor(out=hv, in0=pw_, scalar=G1(dc, w), in1=hv, op0=ALU.mult, op1=ALU.add),
                     [pwk, "adaS", "H%d_%d" % (s_, dc)], ["H%d_%d" % (s_, dc)])

            (rb_, rh_) = rot_r.next()
            ssp, ssk = ps_slot(rb_, rh_)
            for kc in range(KC):
                i = kc % 2
                hv = H[:, kc, s_ * BLK:(s_ + 1) * BLK]
                if kc % 2 == 0:
                    P.op("act", lambda e, hv=hv, i=i: e.activation(out=sq2[i], in_=hv, func=AF.Square), ["H%d_%d" % (s_, kc)], ["sq2%d" % i])
                else:
                    P.op("dve", lambda e, hv=hv, i=i: e.tensor_tensor(out=sq2[i], in0=hv, in1=hv, op=ALU.mult), ["H%d_%d" % (s_, kc)], ["sq2%d" % i])
                P.op("pe", lambda e, ssp=ssp, i=i, kc=kc: e.matmul(ssp, onesA, sq2[i], start=(kc == 0), stop=(kc == KC - 1)), ["sq2%d" % i, "cbf"], [ssk])
            P.op("act", lambda e, ssp=ssp: e.activation(out=rr2, in_=ssp, func=AF.Ln, bias=epsT, scale=1.0 / D), [ssk, "eps"], ["rr2"])
            P.op("act", lambda e: e.activation(out=rstd2, in_=rr2, func=AF.Exp, scale=-0.5), ["rr2"], ["rstd2"])
            plgs = [ps_slot(*rot_r.next()), ps_slot(*rot_m.next())]
            for kc in range(KC):
                i = kc % 2
                hv = H[:, kc, s_ * BLK:(s_ + 1) * BLK]
                if kc % 2 == 0:
                    P.op("dve", lambda e, hv=hv, i=i: e.tensor_tensor(out=tmp2[i], in0=hv, in1=rstd2, op=ALU.mult), ["H%d_%d" % (s_, kc), "rstd2"], ["tmp2%d" % i])
                else:
                    P.op("pool", lambda e, hv=hv, i=i: e.tensor_tensor(out=tmp2[i], in0=hv, in1=rstd2, op=ALU.mult), ["H%d_%d" % (s_, kc), "rstd2"], ["tmp2%d" % i])
                P.op("dve", lambda e, kc=kc, i=i, w=w: e.tensor_scalar(out=hn2f[i], in0=tmp2[i], scalar1=A2t[:, kc, w:w + 1], scalar2=B2(kc, w), op0=ALU.mult, op1=ALU.add),
                     ["tmp2%d" % i, "adaS", "A2t"], ["hn2f%d" % i])
                P.op("act", lambda e, kc=kc, i=i, w=w: e.activation(out=hn2b[:, kc, :], in_=tmp2[i], func=AF.Identity, bias=B2(kc, w), scale=A2t[:, kc, w:w + 1]),
                     ["tmp2%d" % i, "adaS", "A2t"], ["hn2b"])
                for th in range(2):
                    P.op("pe", lambda e, plg=plgs[th][0], i=i, kc=kc, th=th: e.matmul(plg[:, 0:NE], hn2f[i][:, th * 128:(th + 1) * 128], Wr[:, kc, :], start=(kc == 0), stop=(kc == KC - 1)),
                         ["hn2f%d" % i, "Wr"], [plgs[th][1]])
            P.dma("sp", HN2d[:, :, s_ * BLK:(s_ + 1) * BLK].rearrange("c p t -> p c t"), hn2b, ["hn2b"], ["HN2_%d" % s_], "hn2b")
            gp = gts_bufs[rctr[0] % 2]
            gpk = "gts%d" % (rctr[0] % 2)
            rctr[0] += 1
            for th in range(2):
                lgp = plgs[th][0][:, 0:NE]
                P.op("dve", lambda e, lgp=lgp, th=th: e.tensor_tensor(out=lg[:, th, :], in0=lgp, in1=brbc, op=ALU.add), [plgs[th][1], "brbc"], ["lg"])
            def bc2(a):
                return a.unsqueeze(2).to_broadcast([128, 2, NE])
            def bc8(a):
                return a.unsqueeze(2).to_broadcast([128, 8, 4])
            sc3 = sc.rearrange("p t (g k) -> p (t g) k", k=4)
            sc23 = sc2.rearrange("p t (g k) -> p (t g) k", k=4)
            msk3 = msk.rearrange("p t (g k) -> p (t g) k", k=4)
            P.op("dve", lambda e: e.tensor_reduce(out=mx[:, 0, :], in_=lg, axis=AX.X, op=ALU.max), ["lg"], ["mx"])
            P.op("dve", lambda e: e.tensor_tensor(out=lg, in0=lg, in1=bc2(mx[:, 0, :]), op=ALU.subtract), ["lg", "mx"], ["lg"])
            P.op("act", lambda e: e.activation(out=ex, in_=lg, func=AF.Exp), ["lg"], ["ex"])
            P.op("dve", lambda e: e.tensor_reduce(out=mx[:, 1, :], in_=ex, axis=AX.X, op=ALU.add), ["ex"], ["mx"])
            P.op("dve", lambda e: e.reciprocal(out=mx[:, 2, :], in_=mx[:, 1, :]), ["mx"], ["mx"])
            P.op("dve", lambda e: e.tensor_tensor(out=sc, in0=ex, in1=bc2(mx[:, 2, :]), op=ALU.mult), ["ex", "mx"], ["sc"])
            P.op("dve", lambda e: e.tensor_reduce(out=gs, in_=sc3, axis=AX.X, op=ALU.max), ["sc"], ["gs"])
            P.op("dve", lambda e: e.tensor_tensor(out=sc23, in0=sc3, in1=bc8(gs), op=ALU.is_equal), ["sc", "gs"], ["sc2"])
            P.op("dve", lambda e: e.scalar_tensor_tensor(out=sc2, in0=sc2, scalar=-4.0, in1=sc, op0=ALU.mult, op1=ALU.add), ["sc2", "sc"], ["sc2"])
            P.op("dve", lambda e: e.tensor_reduce(out=gm, in_=sc23, axis=AX.X, op=ALU.max), ["sc2"], ["gm"])
            P.op("dve", lambda e: e.tensor_tensor(out=gs, in0=gs, in1=gm, op=ALU.add), ["gs", "gm"], ["gs"])
            P.op("dve", lambda e: e.tensor_reduce(out=mx[:, 3, :], in_=gs.rearrange("p (t g) -> p t g", g=4), axis=AX.X, op=ALU.max), ["gs"], ["mx"])
            P.op("dve", lambda e: e.tensor_tensor(out=gm.rearrange("p (t g) -> p t g", g=4), in0=gs.rearrange("p (t g) -> p t g", g=4),
                                                 in1=mx[:, 3, :].unsqueeze(2).to_broadcast([128, 2, 4]), op=ALU.is_equal), ["gs", "mx"], ["gm"])
            P.op("dve", lambda e: e.scalar_tensor_tensor(out=msk3, in0=sc3, scalar=1.0, in1=bc8(gm), op0=ALU.add, op1=ALU.mult), ["sc", "gm"], ["msk"])
            P.op("dve", lambda e: e.tensor_scalar(out=msk, in0=msk, scalar1=-1.0, scalar2=None, op0=ALU.add), ["msk"], ["msk"])
            P.op("dve", lambda e: e.tensor_reduce(out=mx[:, 4, :], in_=msk, axis=AX.X, op=ALU.max), ["msk"], ["mx"])
            P.op("dve", lambda e: e.tensor_tensor(out=sc2, in0=msk, in1=bc2(mx[:, 4, :]), op=ALU.is_equal), ["msk", "mx"], ["sc2"])
            P.op("dve", lambda e: e.scalar_tensor_tensor(out=sc2, in0=sc2, scalar=-4.0, in1=msk, op0=ALU.mult, op1=ALU.add), ["sc2", "msk"], ["sc2"])
            P.op("dve", lambda e: e.tensor_reduce(out=mx[:, 5, :], in_=sc2, axis=AX.X, op=ALU.max), ["sc2"], ["mx"])
            P.op("dve", lambda e: e.tensor_tensor(out=sc2, in0=msk, in1=bc2(mx[:, 5, :]), op=ALU.is_ge), ["msk", "mx"], ["sc2"])
            P.op("dve", lambda e: e.tensor_tensor(out=mx[:, 6, :], in0=mx[:, 4, :], in1=mx[:, 5, :], op=ALU.add), ["mx"], ["mx"])
            P.op("dve", lambda e: e.reciprocal(out=mx[:, 7, :], in_=mx[:, 6, :]), ["mx"], ["mx"])
            P.op("dve", lambda e: e.tensor_tensor(out=sc2, in0=sc2, in1=msk, op=ALU.mult), ["sc2", "msk"], ["sc2"])
            P.op("dve", lambda e, gp=gp: e.tensor_tensor(out=gp, in0=sc2, in1=bc2(mx[:, 7, :]), op=ALU.mult), ["sc2", "mx"], [gpk])

            def gate_tail(gp=gp, gpk=gpk, s_=s_):
                for th in range(2):
                    (tb_, thh_) = rot_w.next()
                    ptg, ptgk = ps_slot(tb_, thh_)
                    P.op("pe", lambda e, ptg=ptg, gp=gp, th=th: e.matmul(ptg[0:NE, 0:128], gp[:, th, :], identF, start=True, stop=True), [gpk, "CONST"], [ptgk])
                    t0 = s_ * BLK + th * 128
                    P.op("act", lambda e, ptg=ptg, t0=t0: e.activation(out=GT[:, t0:t0 + 128], in_=ptg[0:NE, 0:128], func=AF.Copy), [ptgk], ["GT"])
            pending_tail.append(gate_tail)
        while pending_tail:
            pending_tail.pop(0)()
        P.fence()
        A.release()
        A.hi = hi_save
        if stop == "A2_%d" % l:
            done[0] = True
            break

        A.mark()
        SEL = A.alloc([NE, NE, 128], BF16)
        P.dma("pool", SEL, sel_in, [], ["SEL"], "SEL")
        ring = [A.alloc([128, 4096], BF16) for _ in range(5)]
        ringi = [0]
        he = [A.alloc([128, 4, 512], BF16) for _ in range(2)]
        hn2t = [A.alloc([128, KC, 512], BF16) for _ in range(2)]
        sgs = [A.alloc([128, 512], F32) for _ in range(2)]
        tts = [A.alloc([128, 512], F32) for _ in range(2)]
        if l == 0:
            tiles = [(i * 512, 512, 0) for i in range(5)] + [(2560, 256, 1)]
        else:
            tiles = [(256 + i * 512, 512, 0) for i in range(4)]
        rot_g = Rot([0, 1])
        rot_u = Rot([2, 3])
        rot_y = Rot([4, 5])
        rot_gb = Rot([6, 7])
        uctr = 0

        def ring_next():
            i = ringi[0] % 5
            ringi[0] += 1
            return ring[i], "ring%d" % i

        units = [(ex_, t) for ex_ in range(b_experts) for t in tiles]
        wcur = {}

        def moe_gu(u):
            (ex_, (t0, n, w)) = units[u]
            if ex_ not in wcur:
                wg_t, wgk = ring_next()
                wg_ = wg_t.rearrange("p (k f) -> p k f", f=FE)
                P.dma("pool", wg_, w_gate[l, ex_].rearrange("(kc p) f -> p kc f", p=128), [], [wgk], wgk)
                wu_t, wuk = ring_next()
                wu_ = wu_t.rearrange("p (k f) -> p k f", f=FE)
                P.dma("pool", wu_, w_up[l, ex_].rearrange("(kc p) f -> p kc f", p=128), [], [wuk], wuk)
                wd_t, wdk = ring_next()
                wd_ = wd_t.rearrange("p (k f) -> p k f", f=D)
                P.dma("pool", wd_, w_down[l, ex_].rearrange("(fc p) d -> p fc d", p=128), [], [wdk], wdk)
                wcur[ex_] = (wg_, wgk, wu_, wuk, wd_, wdk)
            (wg_, wgk, wu_, wuk, wd_, wdk) = wcur[ex_]
            ui = u % 2
            hk = "hn2t%d" % ui
            slots_ = sorted(set([t0 // BLK, (t0 + n - 1) // BLK]))
            P.dma("sp", hn2t[ui][:, :, 0:n], HN2d[:, :, t0:t0 + n].rearrange("c p t -> p c t"), ["HN2_%d" % s for s in slots_], [hk], hk)
            gbb = rot_gb.next()
            pgb, pgbk = ps_slot(gbb)
            P.op("pe", lambda e, pgb=pgb, ex_=ex_, t0=t0, n=n: e.matmul(pgb[:, 0:n], SEL[:, ex_, :], GT[:, t0:t0 + n], start=True, stop=True), ["SEL", "GT"], [pgbk])
            for fc in range(4):
                gbk_ = rot_g.next()
                pgg, pggk = ps_slot(gbk_)
                ubk_ = rot_u.next()
                puu, puuk = ps_slot(ubk_)
                for kc in range(KC):
                    P.op("pe", lambda e, pgg=pgg, kc=kc, fc=fc, ui=ui, n=n, wg_=wg_: e.matmul(pgg[:, 0:n], wg_[:, kc, fc * 128:(fc + 1) * 128], hn2t[ui][:, kc, 0:n], start=(kc == 0), stop=(kc == KC - 1)),
                         [wgk, hk], [pggk])
                for kc in range(KC):
                    P.op("pe", lambda e, puu=puu, kc=kc, fc=fc, ui=ui, n=n, wu_=wu_: e.matmul(puu[:, 0:n], wu_[:, kc, fc * 128:(fc + 1) * 128], hn2t[ui][:, kc, 0:n], start=(kc == 0), stop=(kc == KC - 1)),
                         [wuk, hk], [puuk])
                i = fc % 2
                P.op("act", lambda e, pgg=pgg, i=i, n=n: e.activation(out=sgs[i][:, 0:n], in_=pgg[:, 0:n], func=AF.Silu), [pggk], ["sgs%d" % i])
                P.op("dve", lambda e, puu=puu, i=i, n=n: e.tensor_tensor(out=tts[i][:, 0:n], in0=puu[:, 0:n], in1=sgs[i][:, 0:n], op=ALU.mult), [puuk, "sgs%d" % i], ["tts%d" % i])
                P.op("dve", lambda e, pgb=pgb, i=i, n=n, ui=ui, fc=fc: e.tensor_tensor(out=he[ui][:, fc, 0:n], in0=pgb[:, 0:n], in1=tts[i][:, 0:n], op=ALU.mult),
                     [pgbk, "tts%d" % i], ["he%d_%d" % (ui, fc)])

        def moe_dn(u):
            (ex_, (t0, n, w)) = units[u]
            (wg_, wgk, wu_, wuk, wd_, wdk) = wcur[ex_]
            ui = u % 2
            slots_ = sorted(set([t0 // BLK, (t0 + n - 1) // BLK]))
            for dc in range(KC):
                ybk = rot_y.next()
                py, pyk = ps_slot(ybk)
                for fc in range(4):
                    P.op("pe", lambda e, py=py, fc=fc, dc=dc, ui=ui, n=n, wd_=wd_: e.matmul(py[:, 0:n], wd_[:, fc, dc * 128:(dc + 1) * 128], he[ui][:, fc, 0:n], start=(fc == 0), stop=(fc == 3)),
                         [wdk, "he%d_%d" % (ui, fc)], [pyk])
                hv = H[:, dc, t0:t0 + n]
                hkeys_ = ["H%d_%d" % (s, dc) for s in slots_]
                P.op("dve", lambda e, py=py, hv=hv, dc=dc, w=w, n=n: e.scalar_tensor_tensor(out=hv, in0=py[:, 0:n], scalar=G2(dc, w), in1=hv, op0=ALU.mult, op1=ALU.add),
                     [pyk, "adaS"] + hkeys_, hkeys_)

        for u in range(len(units)):
            moe_gu(u)
            if u >= 1:
                moe_dn(u - 1)
        moe_dn(len(units) - 1)
        P.fence()
        A.release()
        if stop == "B_%d" % l:
            done[0] = True
            break

    if debug:
        for kc in range(KC):
            P.dma("sp", dbgH[kc], H[:, kc, :], ["H%d_%d" % (s, kc) for s in range(11)], ["dbgH"], "dbgH")
        P.dma("sp", dbgGT, GT, ["GT"], ["dbgGT"], "dbgGT")
    for kc in range(KC):
        P.dma("sp", yT[kc], H[:, kc, BLK:9 * BLK], ["H%d_%d" % (s, kc) for s in range(1, 9)], ["yout"], "yout")
    P.fence()
    P.emit()
    es.close()
    return nc


def _consts():
    c = np.zeros((128, 5 * 128), np.float32)
    c[:, 0:128] = np.eye(128, dtype=np.float32)
    bd = np.zeros((128, 128), np.float32)
    bd[0:64, 0:64] = 1.0
    bd[64:128, 64:128] = 1.0
    c[:, 128:256] = bd
    c[:, 256:384] = 1.0
    c[:, 384:448] = 1.0
    c[:, 512 + 64:640] = 1.0
    sel = np.zeros((NE, NE, 128), np.float32)
    for e in range(NE):
        sel[e, e, :] = 1.0
    return c, sel


def _bias_table(rpb_l):
    bt = np.full((128, 8, 14, 64), NEG, np.float32)
    qc = np.arange(64)
    cs = np.clip(qc - 8, 0, 48)
    for s in range(2):
        for jj in range(14):
            dr = 13 + s - jj
            if dr < 0 or dr > 14:
                continue
            for kc in range(64):
                valid = (kc >= cs) & (kc < cs + 16)
                dc = kc - qc + 15
                vq = qc[valid]
                bt[s * 64 + kc, :, jj, vq] = rpb_l[:, dr, dc[valid]].T
    return bt.reshape(128, 8, 14 * 64)


def _row_masks(r0):
    rm = np.zeros((128, 10, 6, 4), np.float32)
    for b in range(1, 11):
        r = r0 - 8 + 4 * b
        for qr in range(4):
            q = r + qr
            if q < 0 or q > 255:
                rm[:, b - 1, :, qr] = 1.0
                continue
            rs = min(max(q - 4, 0), 248)
            for c in range(6):
                for s in range(2):
                    a = r - 4 + 2 * c + s
                    if 0 <= a <= 255 and rs <= a < rs + 8:
                        rm[s * 64:(s + 1) * 64, b - 1, c, qr] = 1.0
    return rm


def _rc_tables(core):
    wins = (2, 4, 8, 16)
    rc = np.zeros((128, 2 + 3 * 2 * 16), np.float32)
    rcb = np.zeros((128, 3, 2, 16), np.float32)
    t8 = np.arange(8)
    for g, wd in enumerate(wins):
        c, hh = g // 2, g % 2
        pr = slice(64 * hh, 64 * hh + 64)
        rc[pr, c] = 1.0 / wd
        rcb[pr, :, c, :] = 1.0 / wd
        if core == 0:
            lo = np.clip(t8 - wd // 2, 0, None)
            hi = t8 + wd // 2
            rcb[pr, 0, c, 0:8] = 1.0 / (hi - lo)
        if core == NCORES - 1:
            tt = 16384 - 8 + t8
            lo = tt - wd // 2
            hi = np.clip(tt + wd // 2, None, 16384)
            rcb[pr, 1, c, 8:16] = 1.0 / (hi - lo)
        lo = np.clip(t8 - wd // 2, 0, 256)
        hi = np.clip(t8 + wd // 2, 0, 256)
        rcb[pr, 2, c, 0:8] = 1.0 / (hi - lo)
        tt = 248 + t8
        lo = np.clip(tt - wd // 2, 0, 256)
        hi = np.clip(tt + wd // 2, 0, 256)
        rcb[pr, 2, c, 8:16] = 1.0 / (hi - lo)
    rc[:, 2:] = rcb.reshape(128, -1)
    return rc


def _vecT(v):
    return np.ascontiguousarray(v.reshape(-1, 128).T)


_NC_CACHE = {}


def kernel(x, c, ctx, c_ctx, w_ada, b_ada, norm1, w_in, pool_w, pool_scale, q_norm, k_norm, rpb,
           sg_w, sg_b, sg_norm, w_out, norm2, w_router, b_router, w_gate, w_up, w_down):
    f = lambda a: np.ascontiguousarray(np.asarray(a, dtype=np.float32))
    x, c, ctx, c_ctx = f(x), f(c), f(ctx), f(c_ctx)
    w_ada, b_ada, norm1, w_in, pool_w, pool_scale = f(w_ada), f(b_ada), f(norm1), f(w_in), f(pool_w), f(pool_scale)
    q_norm, k_norm, rpb, sg_w, sg_b, sg_norm = f(q_norm), f(k_norm), f(rpb), f(sg_w), f(sg_b), f(sg_norm)
    w_out, norm2, w_router, b_router, w_gate, w_up, w_down = f(w_out), f(norm2), f(w_router), f(b_router), f(w_gate), f(w_up), f(w_down)

    consts, sel = _consts()
    xg = x[0]
    ctxT = np.ascontiguousarray(ctx[0].T.reshape(KC, 128, BLK))
    cT = np.stack([_vecT(c[0]), _vecT(c_ctx)], axis=-1)
    b_adaT = np.stack([np.ascontiguousarray(b_ada[l].reshape(48, 128).T) for l in range(2)])
    n1T = np.stack([_vecT(norm1[l]) for l in range(2)])
    n2T = np.stack([_vecT(norm2[l]) for l in range(2)])
    pw_bd = np.zeros((2, 128, 2, 128), np.float32)
    for l in range(2):
        for g in range(4):
            cc, hh = g // 2, g % 2
            pw_bd[l, 64 * hh:64 * hh + 64, cc, 64 * hh:64 * hh + 64] = pool_w[l, g]
    pscaleT = np.stack([np.ascontiguousarray(pool_scale[l].reshape(2, 128).T) for l in range(2)])
    qgT = np.stack([np.ascontiguousarray(q_norm[l].reshape(4, 128).T) for l in range(2)])
    kgT = np.stack([np.ascontiguousarray(k_norm[l].reshape(4, 128).T) for l in range(2)])
    bt = np.stack([_bias_table(rpb[l]) for l in range(2)])
    sgwT = np.stack([np.ascontiguousarray(np.transpose(sg_w[l], (2, 0, 1))) for l in range(2)])
    sgbT = np.zeros((2, 128, 2, 128), np.float32)
    for l in range(2):
        for j in range(2):
            for s in range(2):
                sgbT[l, 64 * s:64 * s + 64, j, :] = sg_b[l, 2 * j + s][None, :]
    sggain = np.ascontiguousarray(sg_norm.reshape(2, 256))

    key = "main"
    if key not in _NC_CACHE:
        _NC_CACHE[key] = build_program()
    nc = _NC_CACHE[key]

    in_maps = []
    for core in range(NCORES):
        r0 = 32 * core
        xw = np.zeros((NBL * BLK, D), np.float32)
        ra, rb = r0 - 8, r0 + 40
        va, vb = max(ra, 0), min(rb, 256)
        xw[(va - ra) * 64:(vb - ra) * 64] = xg[va * 64:vb * 64]
        xT = np.ascontiguousarray(xw.T.reshape(KC, 128, NBL * BLK))
        bvalid = np.zeros((128, 13), np.float32)
        for b in range(12):
            r = r0 - 8 + 4 * b
            bvalid[:, b] = 1.0 if 0 <= r <= 252 else 0.0
        bvalid[:, 12] = 1.0
        in_maps.append({
            "xT": xT, "ctxT": ctxT, "cT": cT, "w_ada": w_ada, "b_adaT": b_adaT, "n1T": n1T, "n2T": n2T,
            "w_in": w_in, "pw_bd": pw_bd, "pscaleT": pscaleT, "qgT": qgT, "kgT": kgT, "bt": bt,
            "rm": _row_masks(r0), "rc": _rc_tables(core), "bvalid": bvalid, "sgwT": sgwT, "sgbT": sgbT,
            "sggain": sggain, "w_out": w_out, "w_router": w_router, "b_router": b_router,
            "w_gate": w_gate, "w_up": w_up, "w_down": w_down, "consts": consts, "sel": sel,
        })
    res = run_bass_kernel_spmd(nc, in_maps, core_ids=list(range(NCORES)))
    out = np.zeros((1, 16384, D), np.float32)
    for core in range(NCORES):
        yT = res.results[core]["yT"]
        out[0, core * 2048:(core + 1) * 2048] = yT.reshape(D, 2048).T
    return out
```

```python
from contextlib import ExitStack
import numpy as np
import concourse.bass as bass
import concourse.mybir as mybir
from concourse.bass_utils import run_bass_kernel_spmd

F32 = mybir.dt.float32
BF16 = mybir.dt.bfloat16
AF = mybir.ActivationFunctionType
ALU = mybir.AluOpType
AX = mybir.AxisListType

NCORES = 8
D = 1024
KC = 8
BLK = 256
NBL = 12
CTXB = 12
TA = 13 * BLK
TH = 11 * BLK
PIT = 8 + 12 * BLK + 16 + BLK + 8
PI_CTX = 8 + 12 * BLK + 16
NEG = -30000.0
EPS = 1e-6
NE = 16
FE = 512
HIDE_ADA = True


def hslot(b):
    return 10 if b == CTXB else b - 1


class Prog:
    def __init__(self, nc, es):
        self.nc = nc
        self.es = es
        self.eng = {"pe": nc.tensor, "act": nc.scalar, "dve": nc.vector, "pool": nc.gpsimd, "sp": nc.sync}
        self.stream = {k: [] for k in self.eng}
        self.cnt = {k: 0 for k in self.eng}
        self.sems = {}
        self.dcnt = {}
        self.waited = {k: {} for k in self.eng}
        self.lastw = {}
        self.readers = {}
        for k in self.eng:
            self._sem("E_" + k)

    def _sem(self, key):
        if key not in self.sems:
            self.sems[key] = self.es.enter_context(self.nc.semaphore("s_" + key))
        return self.sems[key]

    def _cur(self, semkey):
        if semkey.startswith("E_"):
            return self.cnt[semkey[2:]]
        return 16 * self.dcnt[semkey]

    def _deps(self, eng, reads, writes):
        deps = {}
        def add(tok):
            if tok is None:
                return
            sk, val = tok
            if sk.startswith("D_"):
                val = self._cur(sk)
            if eng == "pe" and sk == "E_pe":
                return
            if deps.get(sk, 0) < val:
                deps[sk] = val
        for k in reads:
            add(self.lastw.get(k))
        for k in writes:
            add(self.lastw.get(k))
            for t in self.readers.get(k, ()):
                add(t)
        for sk, val in deps.items():
            if self.waited[eng].get(sk, 0) < val:
                self.waited[eng][sk] = val
                self.stream[eng].append(("w", sk, val))

    def _commit(self, tok, reads, writes):
        for k in reads:
            self.readers.setdefault(k, []).append(tok)
        for k in writes:
            self.lastw[k] = tok
            self.readers[k] = []

    def op(self, eng, fn, reads=(), writes=()):
        self._deps(eng, reads, writes)
        self.cnt[eng] += 1
        tok = ("E_" + eng, self.cnt[eng])
        self.stream[eng].append(("o", fn, "E_" + eng, 1))
        self._commit(tok, reads, writes)

    def dma(self, q, out, in_, reads, writes, dkey):
        sk = "D_" + dkey
        self._sem(sk)
        self.dcnt.setdefault(sk, 0)
        self._deps(q, reads, writes)
        self.dcnt[sk] += 1
        tok = (sk, 16 * self.dcnt[sk])
        self.stream[q].append(("o", lambda e: e.dma_start(out=out, in_=in_), sk, 16))
        self._commit(tok, reads, writes)

    def fence(self):
        for e in self.eng:
            for o in self.eng:
                if o == "sp":
                    continue
                v = self.cnt[o]
                if (e != o or e != "pe") and self.waited[e].get("E_" + o, 0) < v:
                    self.waited[e]["E_" + o] = v
                    self.stream[e].append(("w", "E_" + o, v))
            for sk, c in self.dcnt.items():
                if self.waited[e].get(sk, 0) < 16 * c:
                    self.waited[e][sk] = 16 * c
                    self.stream[e].append(("w", sk, 16 * c))

    def emit(self):
        nc = self.nc
        with nc.Block() as block:
            def replay(name):
                def body(e):
                    for it in self.stream[name]:
                        if it[0] == "w":
                            e.wait_ge(self.sems[it[1]], it[2])
                        else:
                            it[1](e).then_inc(self.sems[it[2]], it[3])
                return body
            block.tensor(replay("pe"))
            block.scalar(replay("act"))
            block.vector(replay("dve"))
            block.gpsimd(replay("pool"))
            block.sync(replay("sp"))


class Arena:
    def __init__(self, nc, nbytes):
        self.t = nc.alloc_sbuf_tensor("arena", [128, nbytes // 2], BF16)
        self.nbytes = nbytes
        self.top = 0
        self.hi = nbytes
        self.marks = []

    def alloc(self, shape, dt, hi=False):
        esz = 4 if dt == F32 else 2
        n = 1
        for s in shape[1:]:
            n *= s
        nb = (n * esz + 31) // 32 * 32
        if hi:
            self.hi -= nb
            off = self.hi
        else:
            off = self.top
            self.top += nb
        assert self.top <= self.hi, ("arena overflow", self.top, self.hi, self.nbytes)
        ap = self.t[:, off // 2: off // 2 + n * esz // 2]
        if dt == F32:
            ap = ap.bitcast(F32)
        ap = ap[0:shape[0], :]
        if len(shape) == 3:
            ap = ap.rearrange("p (a b) -> p a b", b=shape[2])
        elif len(shape) == 4:
            ap = ap.rearrange("p (a b c) -> p a b c", b=shape[2], c=shape[3])
        return ap

    def mark(self):
        self.marks.append(self.top)

    def release(self):
        self.top = self.marks.pop()


def build_program(debug=False, stop=None, b_experts=NE):
    nc = bass.Bass("TRN2", target_bir_lowering=False)
    es = ExitStack()

    def din(name, shape, dt=F32):
        return nc.dram_tensor(name, list(shape), dt, kind="ExternalInput").ap()

    xT = din("xT", [KC, 128, NBL * BLK])
    ctxT = din("ctxT", [KC, 128, BLK])
    cT = din("cT", [128, KC, 2])
    w_ada = din("w_ada", [2, D, 6 * D])
    b_adaT = din("b_adaT", [2, 128, 48])
    n1T = din("n1T", [2, 128, KC])
    n2T = din("n2T", [2, 128, KC])
    w_in = din("w_in", [2, D, 2304])
    pw_bd = din("pw_bd", [2, 128, 2, 128])
    pscaleT = din("pscaleT", [2, 128, 2])
    qgT = din("qgT", [2, 128, 4])
    kgT = din("kgT", [2, 128, 4])
    bt_in = din("bt", [2, 128, 8, 14 * 64])
    rm_in = din("rm", [128, 10, 6, 4])
    rc_in = din("rc", [128, 2 + 3 * 2 * 16])
    bvalid_in = din("bvalid", [128, 13])
    sgwT = din("sgwT", [2, 128, 4, 128])
    sgbT = din("sgbT", [2, 128, 2, 128])
    sggain = din("sggain", [2, 256])
    w_out = din("w_out", [2, D, D])
    w_router = din("w_router", [D, NE])
    b_router = din("b_router", [NE])
    w_gate = din("w_gate", [2, NE, D, FE])
    w_up = din("w_up", [2, NE, D, FE])
    w_down = din("w_down", [2, NE, FE, D])
    consts = din("consts", [128, 3 * 128 + 2 * 128])
    sel_in = din("sel", [NE, NE, 128])
    yT = nc.dram_tensor("yT", [KC, 128, 8 * BLK], F32, kind="ExternalOutput").ap()

    skind = "ExternalOutput" if debug else "Internal"
    QTd = nc.dram_tensor("QTd", [4, 128, TA], BF16, kind=skind).ap()
    KTd = nc.dram_tensor("KTd", [4, 128, TA], BF16, kind=skind).ap()
    Vd = nc.dram_tensor("Vd", [TA, 512], BF16, kind=skind).ap()
    VSd = nc.dram_tensor("VSd", [TA, 4 * 128], BF16, kind=skind).ap()
    PId = nc.dram_tensor("PId", [2, 128, PIT], F32, kind=skind).ap()
    Ud = nc.dram_tensor("Ud", [2, 128, TA], BF16, kind=skind).ap()
    HN2d = nc.dram_tensor("HN2d", [KC, 128, TH], BF16, kind=skind).ap()
    if debug:
        dbgH = nc.dram_tensor("dbgH", [KC, 128, TH], F32, kind="ExternalOutput").ap()
        dbgGT = nc.dram_tensor("dbgGT", [NE, TH], BF16, kind="ExternalOutput").ap()
        dbgAda = nc.dram_tensor("dbgAda", [128, 96], F32, kind="ExternalOutput").ap()
    done = [False]

    P = Prog(nc, es)
    A = Arena(nc, (nc.sbuf_bytes_remaining - 512) // 64 * 64)
    banks = [nc.alloc_psum_tensor("bank%d" % i, [128, 512], F32) for i in range(8)]

    H = A.alloc([128, KC, TH], F32)
    GT = A.alloc([NE, TH], BF16)
    RC = A.alloc([128, 2 + 3 * 2 * 16], F32)
    RCW = RC[:, 0:2]
    RCB = RC[:, 2:98].rearrange("p (s c t) -> p s c t", s=3, c=2)
    CONST = A.alloc([128, 128], F32)
    identF = CONST
    cbf = A.alloc([128, 4 * 128], BF16)
    onesBD = cbf[:, 0:128]
    onesA = cbf[:, 128:256]
    half = [cbf[:, 256:384], cbf[:, 384:512]]
    cTs = A.alloc([128, KC, 2], F32)
    condT = A.alloc([128, KC, 2], BF16)
    adaS = A.alloc([128, 48, 2], F32)
    A1t = A.alloc([128, KC, 2], F32)
    A2t = A.alloc([128, KC, 2], F32)
    bAda = A.alloc([128, 48], F32)
    adaraw = A.alloc([128, 96], F32)
    ada1_done = [False]
    n1s = A.alloc([128, KC], F32)
    n2s = A.alloc([128, KC], F32)
    qg = A.alloc([128, 4], F32)
    kg = A.alloc([128, 4], F32)
    pscale = A.alloc([128, 2], F32)
    bvalid = A.alloc([128, 13], F32)
    epsT = A.alloc([128, 1], F32)
    brbc = A.alloc([128, NE], F32)
    Wr = A.alloc([128, KC, NE], F32)
    RM = A.alloc([128, 10, 6, 4], BF16)
    sgB = A.alloc([128, 2, 128], F32)

    def ps_slot(bank, halfi=None):
        if halfi is None:
            return banks[bank][:, :], "PS%d" % bank
        return banks[bank][:, halfi * 256:(halfi + 1) * 256], "PS%d" % bank

    class Rot:
        def __init__(self, items):
            self.items = items
            self.i = 0
        def next(self):
            it = self.items[self.i % len(self.items)]
            self.i += 1
            return it

    P.dma("sp", CONST, consts[:, 0:128], [], ["CONST"], "CONST")
    P.dma("pool", cbf, consts[:, 128:640], [], ["cbf"], "cbf")
    P.dma("sp", RC, rc_in, [], ["RC"], "RC")
    P.dma("sp", cTs, cT, [], ["cTs"], "cTs")
    P.dma("sp", bvalid, bvalid_in, [], ["bvalid"], "bvalid")
    P.dma("sp", brbc, b_router.partition_broadcast(128), [], ["brbc"], "brbc")
    P.dma("sp", Wr, w_router.rearrange("(kc p) e -> p kc e", p=128), [], ["Wr"], "Wr")
    P.dma("pool", RM, rm_in, [], ["RM"], "RM")
    P.op("pool", lambda e: e.memset(epsT, EPS), [], ["eps"])
    if debug:
        P.op("pool", lambda e: e.memset(GT, 0.0), [], ["GT"])
    for kc in range(KC):
        P.dma("sp", H[:, kc, 0:10 * BLK], xT[kc, :, BLK:11 * BLK], [], ["H%d_%d" % (s, kc) for s in range(10)], "Hinit")
        P.dma("sp", H[:, kc, 10 * BLK:11 * BLK], ctxT[kc], [], ["H10_%d" % kc], "Hinit")
    P.op("act", lambda e: e.activation(out=condT, in_=cTs, func=AF.Silu), ["cTs"], ["condT"])

    A.mark()
    zt = A.alloc([128, 2, 16], F32)
    P.op("pool", lambda e: e.memset(zt, 0.0), [], ["zt"])
    P.dma("sp", PId[:, :, 0:8].rearrange("c p t -> p c t"), zt[:, :, 0:8], ["zt"], ["PIpad"], "zt")
    P.dma("sp", PId[:, :, 8 + 12 * BLK:8 + 12 * BLK + 16].rearrange("c p t -> p c t"), zt, ["zt"], ["PIpad"], "zt")
    P.dma("sp", PId[:, :, PI_CTX + BLK:PI_CTX + BLK + 8].rearrange("c p t -> p c t"), zt[:, :, 0:8], ["zt"], ["PIpad"], "zt")
    P.fence()
    A.release()

    dbg = {}

    for l in range(2):
        last = l == 1
        if done[0]:
            break
        A.mark()
        P.dma("sp", bAda, b_adaT[l], [], ["bAda"], "bAda")
        P.dma("sp", n1s, n1T[l], [], ["n1s"], "n1s")
        P.dma("sp", n2s, n2T[l], [], ["n2s"], "n2s")
        P.dma("sp", qg, qgT[l], [], ["qg"], "qg")
        P.dma("sp", kg, kgT[l], [], ["kg"], "kg")
        P.dma("sp", pscale, pscaleT[l], [], ["pscale"], "pscale")
        P.dma("sp", sgB, sgbT[l], [], ["sgB"], "sgB")
        adaps, adak = ps_slot(7)
        if l == 1 and ada1_done[0]:
            P.op("dve", lambda e: e.tensor_tensor(out=adaS, in0=adaraw.rearrange("p (j w) -> p j w", w=2), in1=bAda.unsqueeze(2).to_broadcast([128, 48, 2]), op=ALU.add),
                 ["adaraw", "bAda"], ["adaS"])
        else:
            wa = [A.alloc([128, KC, 768], BF16) for _ in range(2)]
            for pc in range(8):
                wt = wa[pc % 2]
                wk = "wa%d" % (pc % 2)
                P.dma("pool", wt, w_ada[l, :, pc * 768:(pc + 1) * 768].rearrange("(kc p) n -> p kc n", p=128), [], [wk], wk)
                for jj in range(6):
                    j = pc * 6 + jj
                    for kc in range(KC):
                        P.op("pe", lambda e, wt=wt, jj=jj, kc=kc, j=j: e.matmul(adaps[:, 2 * j:2 * j + 2], wt[:, kc, jj * 128:(jj + 1) * 128], condT[:, kc, :], start=(kc == 0), stop=(kc == KC - 1)),
                             [wk, "condT"], [adak])
            P.op("dve", lambda e: e.tensor_tensor(out=adaS, in0=adaps[:, 0:96].rearrange("p (j w) -> p j w", w=2), in1=bAda.unsqueeze(2).to_broadcast([128, 48, 2]), op=ALU.add),
                 [adak, "bAda"], ["adaS"])
        P.op("dve", lambda e: e.scalar_tensor_tensor(out=A1t, in0=adaS[:, 8:16, :], scalar=1.0, in1=n1s.unsqueeze(2).to_broadcast([128, KC, 2]), op0=ALU.add, op1=ALU.mult),
             ["adaS", "n1s"], ["A1t"])
        P.op("dve", lambda e: e.scalar_tensor_tensor(out=A2t, in0=adaS[:, 32:40, :], scalar=1.0, in1=n2s.unsqueeze(2).to_broadcast([128, KC, 2]), op0=ALU.add, op1=ALU.mult),
             ["adaS", "n2s"], ["A2t"])
        P.op("dve", lambda e: e.tensor_scalar(out=qg, in0=qg, scalar1=0.125, scalar2=None, op0=ALU.mult), ["qg"], ["qg"])
        P.fence()
        A.release()

        if debug and l == 0:
            P.dma("sp", dbgAda, adaS.rearrange("p j w -> p (j w)"), ["adaS"], ["dbgAda"], "dbgAda")
        if stop == "ada%d" % l:
            done[0] = True
            break

        def B1(kc, w): return adaS[:, kc, w:w + 1]
        def G1(kc, w): return adaS[:, 16 + kc, w:w + 1]
        def B2(kc, w): return adaS[:, 24 + kc, w:w + 1]
        def G2(kc, w): return adaS[:, 40 + kc, w:w + 1]

        A.mark()
        Win = A.alloc([128, KC, 2304], BF16)
        for kc in range(KC):
            P.dma("pool", Win[:, kc, :], w_in[l, kc * 128:(kc + 1) * 128, :], [], ["Win"], "Win")
        hi_save = A.hi
        Wout = A.alloc([128, KC, D], BF16, hi=True)
        BT = A.alloc([128, 8, 14 * 64], BF16, hi=True)
        P.dma("pool", BT, bt_in[l], [], ["BT"], "BT")
        for kc in range(KC):
            P.dma("pool", Wout[:, kc, :], w_out[l, kc * 128:(kc + 1) * 128, :], [], ["Wout"], "Wout")
        sgG = A.alloc([128, 256], F32)
        P.dma("sp", sgG, sggain[l].partition_broadcast(128), [], ["sgG"], "sgG")
        hn = A.alloc([128, KC, BLK], BF16)
        hn_b = A.alloc([128, KC, BLK], BF16)
        sqk = [A.alloc([128, BLK], BF16) for _ in range(2)]
        tmpk = [A.alloc([128, BLK], F32) for _ in range(2)]
        xtmp = A.alloc([128, KC, BLK], F32)
        rr = A.alloc([128, BLK], F32)
        rstd = A.alloc([128, BLK], F32)
        pis = [A.alloc([128, BLK], F32) for _ in range(2)]
        qs = [A.alloc([128, BLK], BF16) for _ in range(2)]
        sqb = [A.alloc([128, BLK], BF16) for _ in range(2)]
        r2 = [A.alloc([128, BLK], F32) for _ in range(2)]
        ri2 = [A.alloc([128, BLK], F32) for _ in range(2)]
        us = [A.alloc([128, BLK], BF16) for _ in range(2)]
        vun = [A.alloc([128, 512], BF16) for _ in range(2)]
        vspad = [A.alloc([128, 4, 128], BF16) for _ in range(2)]
        ggs = [A.alloc([128, 256], F32) for _ in range(2)]
        gsq = A.alloc([128, 256], F32)
        ss4 = A.alloc([128, 4], F32)
        for i in range(2):
            P.op("pool", lambda e, i=i: e.memset(vspad[i], 0.0), [], ["vspad%d" % i])
        rot_f = Rot([(1, 0), (2, 0), (3, 0)])
        rot_s = Rot([(0, 0)])
        rot_s2 = Rot([(6, 0), (7, 0)])
        rot_v = Rot([4, 5])
        cnt2 = [0]

        if l == 0:
            a1_blocks = [(CTXB, "full"), (0, "kvp")] + [(b, "full") for b in range(1, 11)] + [(11, "kvp")]
        else:
            a1_blocks = [(CTXB, "kv"), (1, "kvp")] + [(b, "full") for b in range(2, 10)] + [(10, "kvp")]

        hn_bufs = [hn, hn_b]

        def a1_norm(bi):
            (b, mode) = a1_blocks[bi]
            hb = bi % 2
            hn_ = hn_bufs[hb]
            w = 1 if b == CTXB else 0
            if l == 0 and b in (0, 11):
                if b == 0:
                    for kc in range(KC):
                        P.dma("sp", xtmp[:, kc, :], xT[kc, :, b * BLK:(b + 1) * BLK], [], ["xtmp"], "xtmp")
                hsrc = lambda kc: xtmp[:, kc, :]
                hkeys = lambda kc: ["xtmp"]
            else:
                s_ = hslot(b)
                hsrc = lambda kc, s_=s_: H[:, kc, s_ * BLK:(s_ + 1) * BLK]
                hkeys = lambda kc, s_=s_: ["H%d_%d" % (s_, kc)]
            (sb_, sh_) = rot_s.next()
            ssp, ssk = ps_slot(sb_, sh_)
            for kc in range(KC):
                i = cnt2[0] % 2
                cnt2[0] += 1
                if kc % 2 == 0:
                    P.op("act", lambda e, kc=kc, i=i, hsrc=hsrc: e.activation(out=sqk[i], in_=hsrc(kc), func=AF.Square), hkeys(kc), ["sqk%d" % i])
                else:
                    P.op("dve", lambda e, kc=kc, i=i, hsrc=hsrc: e.tensor_tensor(out=sqk[i], in0=hsrc(kc), in1=hsrc(kc), op=ALU.mult), hkeys(kc), ["sqk%d" % i])
                P.op("pe", lambda e, kc=kc, i=i, ssp=ssp: e.matmul(ssp, onesA, sqk[i], start=(kc == 0), stop=(kc == KC - 1)), ["sqk%d" % i, "cbf"], [ssk])
            P.op("act", lambda e, ssp=ssp: e.activation(out=rr, in_=ssp, func=AF.Ln, bias=epsT, scale=1.0 / D), [ssk, "eps"], ["rr"])
            P.op("act", lambda e: e.activation(out=rstd, in_=rr, func=AF.Exp, scale=-0.5), ["rr"], ["rstd"])
            for kc in range(KC):
                i = kc % 2
                if kc % 2 == 0:
                    P.op("dve", lambda e, kc=kc, i=i, hsrc=hsrc: e.tensor_tensor(out=tmpk[i], in0=hsrc(kc), in1=rstd, op=ALU.mult), hkeys(kc) + ["rstd"], ["tmpk%d" % i])
                    P.op("act", lambda e, kc=kc, i=i, w=w, hn_=hn_: e.activation(out=hn_[:, kc, :], in_=tmpk[i], func=AF.Identity, bias=B1(kc, w), scale=A1t[:, kc, w:w + 1]),
                         ["tmpk%d" % i, "adaS", "A1t"], ["hn%d_%d" % (hb, kc)])
                else:
                    P.op("pool", lambda e, kc=kc, i=i, hsrc=hsrc: e.tensor_tensor(out=tmpk[i], in0=hsrc(kc), in1=rstd, op=ALU.mult), hkeys(kc) + ["rstd"], ["tmpk%d" % i])
                    P.op("dve", lambda e, kc=kc, i=i, w=w, hn_=hn_: e.tensor_scalar(out=hn_[:, kc, :], in0=tmpk[i], scalar1=A1t[:, kc, w:w + 1], scalar2=B1(kc, w), op0=ALU.mult, op1=ALU.add),
                         ["tmpk%d" % i, "adaS", "A1t"], ["hn%d_%d" % (hb, kc)])

        a1_norm(0)
        for bi, (b, mode) in enumerate(a1_blocks):
            if bi + 1 < len(a1_blocks):
                a1_norm(bi + 1)
            if l == 0 and bi == 1:
                for kc in range(KC):
                    P.dma("sp", xtmp[:, kc, :], xT[kc, :, 11 * BLK:12 * BLK], [], ["xtmp"], "xtmp")
            hb = bi % 2
            hn = hn_bufs[hb]
            w = 1 if b == CTXB else 0
            tok0 = b * BLK

            def fchunk(col0):
                (fb, fh) = rot_f.next()
                pp, pk = ps_slot(fb, fh)
                for kc in range(KC):
                    P.op("pe", lambda e, kc=kc, pp=pp, hn=hn: e.matmul(pp, Win[:, kc, col0:col0 + 128], hn[:, kc, :], start=(kc == 0), stop=(kc == KC - 1)),
                         ["Win", "hn%d_%d" % (hb, kc)], [pk])
                return pp, pk

            if mode in ("full", "kvp"):
                for c in range(2):
                    pp, pk = fchunk(c * 128)
                    i = c
                    P.op("dve", lambda e, pp=pp, i=i, b=b: e.tensor_scalar(out=pis[i], in0=pp, scalar1=bvalid[:, b:b + 1], scalar2=None, op0=ALU.mult), [pk, "bvalid"], ["pis%d" % i])
                    pt0 = (PI_CTX if b == CTXB else 8 + b * BLK)
                    P.dma("sp", PId[c, :, pt0:pt0 + BLK], pis[i], ["pis%d" % i], ["PI_%d" % b], "pis%d" % i)
            qk_list = []
            if mode == "full":
                qk_list += [("q", c) for c in range(4)]
            qk_list += [("k", c) for c in range(4)]
            def qk_finish(pend):
                (which, c, pp, pk, i) = pend
                (s2b, s2h) = rot_s2.next()
                sp2, sk2 = ps_slot(s2b, s2h)
                P.op("pe", lambda e, sp2=sp2, i=i: e.matmul(sp2, onesBD, sqb[i], start=True, stop=True), ["sqb%d" % i, "cbf"], [sk2])
                P.op("act", lambda e, sp2=sp2, i=i: e.activation(out=r2[i], in_=sp2, func=AF.Ln, bias=epsT, scale=1.0 / 64), [sk2, "eps"], ["r2%d" % i])
                P.op("act", lambda e, i=i: e.activation(out=ri2[i], in_=r2[i], func=AF.Exp, scale=-0.5), ["r2%d" % i], ["ri2%d" % i])
                gtab = qg if which == "q" else kg
                P.op("dve", lambda e, pp=pp, i=i, c=c, gtab=gtab: e.scalar_tensor_tensor(out=qs[i], in0=pp, scalar=gtab[:, c:c + 1], in1=ri2[i], op0=ALU.mult, op1=ALU.mult),
                     [pk, "ri2%d" % i, "qg", "kg"], ["qs%d" % i])
                dst = (QTd if which == "q" else KTd)[c, :, tok0:tok0 + BLK]
                P.dma("sp", dst, qs[i], ["qs%d" % i], ["%s_%d" % (which.upper(), b)], "qs%d" % i)

            pend = None
            for (which, c) in qk_list:
                col0 = (256 if which == "q" else 768) + c * 128
                pp, pk = fchunk(col0)
                i = cnt2[0] % 2
                cnt2[0] += 1
                P.op("act", lambda e, pp=pp, i=i: e.activation(out=sqb[i], in_=pp, func=AF.Square), [pk], ["sqb%d" % i])
                if pend is not None:
                    qk_finish(pend)
                pend = (which, c, pp, pk, i)
            qk_finish(pend)
            if mode == "full":
                for c in range(2):
                    pp, pk = fchunk(1792 + c * 128)
                    i = c
                    P.op("act", lambda e, pp=pp, i=i: e.activation(out=us[i], in_=pp, func=AF.Gelu_apprx_tanh), [pk], ["us%d" % i])
                    P.dma("sp", Ud[c, :, tok0:tok0 + BLK], us[i], ["us%d" % i], ["U_%d" % b], "us%d" % i)
            for th in range(2):
                vb = rot_v.next()
                pv, pvk = ps_slot(vb)
                for kc in range(KC):
                    P.op("pe", lambda e, kc=kc, pv=pv, th=th, hn=hn: e.matmul(pv, hn[:, kc, th * 128:(th + 1) * 128], Win[:, kc, 1280:1792], start=(kc == 0), stop=(kc == KC - 1)),
                         ["Win", "hn%d_%d" % (hb, kc)], [pvk])
                i = th
                if th == 0:
                    P.op("act", lambda e, pv=pv, i=i: e.activation(out=vun[i], in_=pv, func=AF.Copy), [pvk], ["vun%d" % i])
                else:
                    P.op("dve", lambda e, pv=pv, i=i: e.tensor_copy(out=vun[i], in_=pv), [pvk], ["vun%d" % i])
                P.dma("sp", Vd[tok0 + th * 128:tok0 + (th + 1) * 128, :], vun[i], ["vun%d" % i], ["V_%d" % b], "vun%d" % i)
            if mode == "full":
                for th in range(2):
                    (gb_, gh_) = rot_s2.next()
                    pg, pgk = ps_slot(gb_, gh_)
                    for kc in range(KC):
                        P.op("pe", lambda e, kc=kc, pg=pg, th=th, hn=hn: e.matmul(pg, hn[:, kc, th * 128:(th + 1) * 128], Win[:, kc, 2048:2304], start=(kc == 0), stop=(kc == KC - 1)),
                             ["Win", "hn%d_%d" % (hb, kc)], [pgk])
                    P.op("act", lambda e, pg=pg, th=th: e.activation(out=ggs[th], in_=pg, func=AF.Gelu_apprx_tanh), [pgk], ["gg%d" % th])
                for th in range(2):
                    gg_ = ggs[th]
                    ggk = "gg%d" % th
                    P.op("pool", lambda e, gg_=gg_: e.tensor_tensor(out=gsq, in0=gg_, in1=gg_, op=ALU.mult), [ggk], ["gsq"])
                    P.op("dve", lambda e: e.tensor_reduce(out=ss4, in_=gsq.rearrange("p (a b) -> p a b", b=64), axis=AX.X, op=ALU.add), ["gsq"], ["ss4"])
                    P.op("act", lambda e: e.activation(out=ss4, in_=ss4, func=AF.Ln, bias=epsT, scale=1.0 / 64), ["ss4", "eps"], ["ss4"])
                    P.op("act", lambda e: e.activation(out=ss4, in_=ss4, func=AF.Exp, scale=-0.5), ["ss4"], ["ss4"])
                    P.op("dve", lambda e, gg_=gg_: e.tensor_tensor(out=gsq.rearrange("p (a b) -> p a b", b=64), in0=gg_.rearrange("p (a b) -> p a b", b=64),
                                                         in1=ss4.unsqueeze(2).to_broadcast([128, 4, 64]), op=ALU.mult), [ggk, "ss4"], ["gsq"])
                    i = th
                    for s in range(2):
                        srcg = gsq.rearrange("p (j s d) -> p j s d", s=2, d=64)[:, :, s, :]
                        gng = sgG.rearrange("p (j s d) -> p j s d", s=2, d=64)[:, :, s, :]
                        dstg = vspad[i].rearrange("p (j s) c -> p j s c", s=2)[:, :, s, 64 * s:64 * s + 64]
                        P.op("dve", lambda e, srcg=srcg, gng=gng, dstg=dstg: e.tensor_tensor(out=dstg, in0=srcg, in1=gng, op=ALU.mult), ["gsq", "sgG"], ["vspad%d" % i])
                    P.dma("sp", VSd[tok0 + th * 128:tok0 + (th + 1) * 128, :], vspad[i].rearrange("p h c -> p (h c)"), ["vspad%d" % i], ["VS_%d" % b], "vspad%d" % i)
        P.fence()
        A.release()
        if stop == "A1_%d" % l:
            done[0] = True
            break

        A.mark()
        PW = A.alloc([128, 2, 128], BF16)
        P.dma("pool", PW, pw_bd[l], [], ["PW"], "PW")
        SGW = A.alloc([128, 4, 128], BF16)
        P.dma("pool", SGW, sgwT[l], [], ["SGW"], "SGW")
        KTc = A.alloc([128, 4, BLK], BF16)
        Vc = A.alloc([128, 2, 512], BF16)
        P.dma("sp", KTc, KTd[:, :, CTXB * BLK:(CTXB + 1) * BLK].rearrange("c p t -> p c t"), ["K_%d" % CTXB], ["KTc"], "KTc")
        P.dma("sp", Vc, Vd[CTXB * BLK:(CTXB + 1) * BLK, :].rearrange("(c p) n -> p c n", p=128), ["V_%d" % CTXB], ["Vc"], "Vc")
        QTz = A.alloc([128, 4, 2 * BLK], BF16)
        P.op("pool", lambda e: e.memset(QTz, 0.0), [], ["QTz"])
        KTw = A.alloc([128, 4, 3 * BLK], BF16)
        Vw = A.alloc([128, 6, 512], BF16)
        PIw = A.alloc([128, 2, BLK + 16], F32)
        Ub = A.alloc([128, 2, BLK], BF16)
        VSp = A.alloc([128, 2, 4 * 128], BF16)
        mix = A.alloc([128, KC, BLK], BF16)
        ym = A.alloc([128, BLK], F32)
        ymb = A.alloc([128, BLK], BF16)
        tS = [A.alloc([128, 2 * BLK], F32) for _ in range(4)]
        eS = [A.alloc([128, 2 * BLK], BF16) for _ in range(4)]
        pT = [A.alloc([128, 2 * BLK], BF16) for _ in range(8)]
        rden = A.alloc([128, 2 * BLK], F32)
        rdi = A.alloc([128, 2 * BLK], F32)
        sl = [tS[0][:, 0:BLK + 16], tS[1][:, 0:BLK + 16], rden[:, 0:BLK + 16], rdi[:, 0:BLK + 16]]
        slk = ["tS0", "tS1", "rden", "rdi"]
        sgt = A.alloc([128, 128], F32)
        hn2b = A.alloc([128, KC, BLK], BF16)
        hn2f = [A.alloc([128, BLK], F32) for _ in range(2)]
        sq2 = [A.alloc([128, BLK], BF16) for _ in range(2)]
        tmp2 = [A.alloc([128, BLK], F32) for _ in range(2)]
        rr2 = A.alloc([128, BLK], F32)
        rstd2 = A.alloc([128, BLK], F32)
        lg = A.alloc([128, 2, NE], F32)
        ex = A.alloc([128, 2, NE], F32)
        mx = A.alloc([128, 8, 2], F32)
        sc = A.alloc([128, 2, NE], F32)
        sc2 = A.alloc([128, 2, NE], F32)
        gs = A.alloc([128, 8], F32)
        gm = A.alloc([128, 8], F32)
        msk = A.alloc([128, 2, NE], F32)
        gts_bufs = [A.alloc([128, 2, NE], F32) for _ in range(2)]
        rctr = [0]
        pending_tail = []

        rot_st = Rot([(0, 0), (1, 0), (4, 0), (5, 0)])
        rot_o = Rot([(2, 0), (6, 0)])
        rot_d = Rot([(3, 0), (7, 0)])
        rot_w = Rot([(4, 0), (5, 0)])
        rot_m = Rot([(6, 0)])
        rot_r = Rot([(7, 0)])
        ctr = [0]

        a2_blocks = ([CTXB] + list(range(1, 11))) if l == 0 else list(range(2, 10))

        def load_att(b):
            tok0 = b * BLK
            P.dma("sp", QTz[0:64, :, 0:BLK], QTd[:, 0:64, tok0:tok0 + BLK].rearrange("c p t -> p c t"), ["Q_%d" % b], ["QTz"], "QTz")
            P.dma("sp", QTz[64:128, :, BLK:2 * BLK], QTd[:, 64:128, tok0:tok0 + BLK].rearrange("c p t -> p c t"), ["Q_%d" % b], ["QTz"], "QTz")
            if b != CTXB:
                P.dma("sp", KTw, KTd[:, :, tok0 - BLK:tok0 + 2 * BLK].rearrange("c p t -> p c t"), ["K_%d" % (b - 1), "K_%d" % b, "K_%d" % (b + 1)], ["KTw"], "KTw")
                P.dma("sp", Vw, Vd[tok0 - BLK:tok0 + 2 * BLK, :].rearrange("(c p) n -> p c n", p=128), ["V_%d" % (b - 1), "V_%d" % b, "V_%d" % (b + 1)], ["Vw"], "Vw")

        def load_pool(b):
            if b != CTXB:
                P.dma("sp", PIw, PId[:, :, b * BLK:b * BLK + BLK + 16].rearrange("c p t -> p c t"), ["PI_%d" % (b - 1), "PI_%d" % b, "PI_%d" % (b + 1), "PIpad"], ["PIw"], "PIw")
            else:
                P.dma("sp", PIw, PId[:, :, PI_CTX - 8:PI_CTX + BLK + 8].rearrange("c p t -> p c t"), ["PI_%d" % b, "PIpad"], ["PIw"], "PIw")

        def load_sg(b):
            tok0 = b * BLK
            P.dma("sp", Ub, Ud[:, :, tok0:tok0 + BLK].rearrange("c p t -> p c t"), ["U_%d" % b], ["Ub"], "Ub")
            P.dma("sp", VSp, VSd[tok0:tok0 + BLK, :].rearrange("(c p) n -> p c n", p=128), ["VS_%d" % b], ["VSp"], "VSp")

        for b in a2_blocks:
            isctx = b == CTXB
            w = 1 if isctx else 0
            tok0 = b * BLK
            s_ = hslot(b)
            if b == a2_blocks[0]:
                load_att(b)
                load_pool(b)
                load_sg(b)

            rcslot = 3 if isctx else (1 if b == 2 else (2 if b == 9 else 0))
            for c in range(2):
                x_ = PIw[:, c, :]
                P.op("pool", lambda e, x_=x_: e.tensor_tensor(out=sl[0][:, 1:272], in0=x_[:, 0:271], in1=x_[:, 1:272], op=ALU.add), ["PIw"], [slk[0]])
                P.op("pool", lambda e: e.tensor_tensor(out=sl[1][:, 2:271], in0=sl[0][:, 1:270], in1=sl[0][:, 3:272], op=ALU.add), [slk[0]], [slk[1]])
                lev = [0, 1]
                if c == 1:
                    P.op("pool", lambda e: e.tensor_tensor(out=sl[2][:, 4:269], in0=sl[1][:, 2:267], in1=sl[1][:, 6:271], op=ALU.add), [slk[1]], [slk[2]])
                    P.op("pool", lambda e: e.tensor_tensor(out=sl[3][:, 8:265], in0=sl[2][:, 4:261], in1=sl[2][:, 12:269], op=ALU.add), [slk[2]], [slk[3]])
                    lev = [2, 3]
                for hh in range(2):
                    pr = slice(64 * hh, 64 * hh + 64)
                    lv = lev[hh]
                    P.op("pool", lambda e, pr=pr, lv=lv, c=c: e.tensor_tensor(out=ym[pr, :], in0=sl[lv][pr, 8:264], in1=RCW[pr, c:c + 1].to_broadcast([64, BLK]), op=ALU.mult),
                         [slk[lv], "RC"], ["ym"])
                    if rcslot in (1, 3):
                        P.op("pool", lambda e, pr=pr, lv=lv, c=c, rcslot=rcslot: e.tensor_tensor(out=ym[pr, 0:8], in0=sl[lv][pr, 8:16], in1=RCB[pr, rcslot - 1, c, 0:8], op=ALU.mult),
                             [slk[lv], "RC", "ym"], ["ym"])
                    if rcslot in (2, 3):
                        P.op("pool", lambda e, pr=pr, lv=lv, c=c, rcslot=rcslot: e.tensor_tensor(out=ym[pr, 248:256], in0=sl[lv][pr, 256:264], in1=RCB[pr, rcslot - 1, c, 8:16], op=ALU.mult),
                             [slk[lv], "RC", "ym"], ["ym"])
                P.op("pool", lambda e, x_=x_: e.tensor_tensor(out=ymb, in0=ym, in1=x_[:, 8:264], op=ALU.subtract), ["ym", "PIw"], ["ymb"])
                (mb, mh) = rot_m.next()
                pm, pmk = ps_slot(mb, mh)
                P.op("pe", lambda e, pm=pm, c=c: e.matmul(pm, PW[:, c, :], ymb, start=True, stop=True), ["PW", "ymb"], [pmk])
                P.op("act", lambda e, pm=pm, c=c: e.activation(out=mix[:, c, :], in_=pm, func=AF.Copy, scale=pscale[:, c:c + 1]), [pmk, "pscale"], ["mix%d" % c])

            nxt_ = a2_blocks[a2_blocks.index(b) + 1] if a2_blocks.index(b) + 1 < len(a2_blocks) else None
            if nxt_ is not None:
                load_pool(nxt_)
            for th in range(2):
                for j in range(2):
                    (mb, mh) = rot_m.next()
                    pm, pmk = ps_slot(mb, mh)
                    pmv = pm[:, 0:128]
                    for s in range(2):
                        hh = 2 * j + s
                        P.op("pe", lambda e, pmv=pmv, th=th, hh=hh, s=s: e.matmul(pmv, VSp[:, th, hh * 128:(hh + 1) * 128], SGW[:, hh, :], start=(s == 0), stop=(s == 1)),
                             ["VSp", "SGW"], [pmk])
                    P.op("dve", lambda e, pmv=pmv, j=j: e.tensor_tensor(out=sgt, in0=pmv, in1=sgB[:, j, :], op=ALU.add), [pmk, "sgB"], ["sgt"])
                    P.op("dve", lambda e, th=th, j=j: e.tensor_tensor(out=mix[:, 6 + j, th * 128:(th + 1) * 128], in0=sgt, in1=Ub[:, j, th * 128:(th + 1) * 128], op=ALU.mult),
                         ["sgt", "Ub"], ["mix%d" % (6 + j)])

            if nxt_ is not None:
                load_sg(nxt_)
            chunks = ([] if isctx else [("l", c) for c in range(6)]) + [("c", c) for c in range(2)]
            nch = len(chunks)
            items = []
            for j in range(4):
                for ic, (kind, c) in enumerate(chunks):
                    items.append((j, kind, c, ic))
            DEPTH = 6
            pair_ps = {}
            item_buf = {}

            def att_stage1(idx):
                (j, kind, c, ic) = items[idx]
                (sb2, sh2) = rot_st.next()
                pst, pstk = ps_slot(sb2)
                ii = ctr[0] % 4
                i4 = ctr[0] % 8
                ctr[0] += 1
                item_buf[idx] = i4
                if kind == "l":
                    P.op("pe", lambda e, pst=pst, j=j, c=c: e.matmul(pst, KTw[:, j, c * 128:(c + 1) * 128], QTz[:, j, :], start=True, stop=True),
                         ["KTw", "QTz"], [pstk])
                    P.op("dve", lambda e, pst=pst, ii=ii, j=j, c=c: e.tensor_tensor(out=tS[ii].rearrange("p (h q) -> p h q", h=2), in0=pst.rearrange("p (h q) -> p h q", h=2),
                                                                               in1=BT[:, 2 * j:2 * j + 2, (10 - 2 * c) * 64:(14 - 2 * c) * 64], op=ALU.add),
                         [pstk, "BT"], ["tS%d" % ii])
                    P.op("act", lambda e, ii=ii: e.activation(out=eS[ii], in_=tS[ii], func=AF.Exp), ["tS%d" % ii], ["eS%d" % ii])
                    P.op("pool", lambda e, ii=ii, i4=i4, c=c, b=b: e.tensor_tensor(out=pT[i4].rearrange("p (h a q) -> p h a q", h=2, q=64), in0=eS[ii].rearrange("p (h a q) -> p h a q", h=2, q=64),
                                                                            in1=RM[:, b - 1, c, :].unsqueeze(1).unsqueeze(3).to_broadcast([128, 2, 4, 64]), op=ALU.mult),
                         ["eS%d" % ii, "RM"], ["pT%d" % i4])
                else:
                    P.op("pe", lambda e, pst=pst, j=j, c=c: e.matmul(pst, KTc[:, j, c * 128:(c + 1) * 128], QTz[:, j, :], start=True, stop=True),
                         ["KTc", "QTz"], [pstk])
                    P.op("act", lambda e, pst=pst, i4=i4: e.activation(out=pT[i4], in_=pst, func=AF.Exp), [pstk], ["pT%d" % i4])

            def att_stage2(idx):
                (j, kind, c, ic) = items[idx]
                i4 = item_buf[idx]
                if ic == 0:
                    pair_ps[j] = (ps_slot(rot_o.next()[0]), ps_slot(rot_d.next()[0]))
                (po, pok), (pd, pdk) = pair_ps[j]
                if kind == "l":
                    vl, vk = Vw[:, c, j * 128:(j + 1) * 128], "Vw"
                else:
                    vl, vk = Vc[:, c, j * 128:(j + 1) * 128], "Vc"
                P.op("pe", lambda e, po=po, vl=vl, i4=i4, ic=ic, nch=nch: e.matmul(po, vl, pT[i4], start=(ic == 0), stop=(ic == nch - 1)), [vk, "pT%d" % i4], [pok])
                P.op("pe", lambda e, pd=pd, i4=i4, ic=ic, nch=nch: e.matmul(pd, onesA, pT[i4], start=(ic == 0), stop=(ic == nch - 1)), ["cbf", "pT%d" % i4], [pdk])
                if ic == nch - 1:
                    P.op("act", lambda e, pd=pd: e.activation(out=rden, in_=pd, func=AF.Ln), [pdk], ["rden"])
                    P.op("act", lambda e: e.activation(out=rdi, in_=rden, func=AF.Exp, scale=-1.0), ["rden"], ["rdi"])
                    for s in range(2):
                        pr = slice(64 * s, 64 * s + 64)
                        P.op("dve", lambda e, po=po, j=j, pr=pr, s=s: e.tensor_tensor(out=mix[pr, 2 + j, :], in0=po[pr, s * 256:(s + 1) * 256], in1=rdi[pr, s * 256:(s + 1) * 256], op=ALU.mult),
                             [pok, "rdi"], ["mix%d" % (2 + j)])

            for idx in range(len(items) + DEPTH):
                if idx < len(items):
                    att_stage1(idx)
                if idx - DEPTH >= 0:
                    att_stage2(idx - DEPTH)
                if idx == 3:
                    while pending_tail:
                        pending_tail.pop(0)()
            nxt = a2_blocks[a2_blocks.index(b) + 1] if a2_blocks.index(b) + 1 < len(a2_blocks) else None
            if nxt is not None:
                load_att(nxt)

            for dc in range(KC):
                (wb_, wh_) = rot_w.next()
                pw_, pwk = ps_slot(wb_, wh_)
                for m in range(KC):
                    P.op("pe", lambda e, pw_=pw_, m=m, dc=dc: e.matmul(pw_, Wout[:, m, dc * 128:(dc + 1) * 128], mix[:, m, :], start=(m == 0), stop=(m == KC - 1)),
                         ["Wout", "mix%d" % m], [pwk])
                hv = H[:, dc, s_ * BLK:(s_ + 1) * BLK]
                P.op("dve", lambda e, pw_=pw_, hv=hv, dc=dc, w=w: e.scalar_tensor_tensor(out=hv, in0=pw_, scalar=G1(dc, w), in1=hv, op0=ALU.mult, op1=ALU.add),
                     [pwk, "adaS", "H%d_%d" % (s_, dc)], ["H%d_%d" % (s_, dc)])

            (rb_, rh_) = rot_r.next()
            ssp, ssk = ps_slot(rb_, rh_)
            for kc in range(KC):
                i = kc % 2
                hv = H[:, kc, s_ * BLK:(s_ + 1) * BLK]
                if kc % 2 == 0:
                    P.op("act", lambda e, hv=hv, i=i: e.activation(out=sq2[i], in_=hv, func=AF.Square), ["H%d_%d" % (s_, kc)], ["sq2%d" % i])
                else:
                    P.op("dve", lambda e, hv=hv, i=i: e.tensor_tensor(out=sq2[i], in0=hv, in1=hv, op=ALU.mult), ["H%d_%d" % (s_, kc)], ["sq2%d" % i])
                P.op("pe", lambda e, ssp=ssp, i=i, kc=kc: e.matmul(ssp, onesA, sq2[i], start=(kc == 0), stop=(kc == KC - 1)), ["sq2%d" % i, "cbf"], [ssk])
            P.op("act", lambda e, ssp=ssp: e.activation(out=rr2, in_=ssp, func=AF.Ln, bias=epsT, scale=1.0 / D), [ssk, "eps"], ["rr2"])
            P.op("act", lambda e: e.activation(out=rstd2, in_=rr2, func=AF.Exp, scale=-0.5), ["rr2"], ["rstd2"])
            plgs = [ps_slot(*rot_r.next()), ps_slot(*rot_m.next())]
            for kc in range(KC):
                i = kc % 2
                hv = H[:, kc, s_ * BLK:(s_ + 1) * BLK]
                if kc % 2 == 0:
                    P.op("dve", lambda e, hv=hv, i=i: e.tensor_tensor(out=tmp2[i], in0=hv, in1=rstd2, op=ALU.mult), ["H%d_%d" % (s_, kc), "rstd2"], ["tmp2%d" % i])
                else:
                    P.op("pool", lambda e, hv=hv, i=i: e.tensor_tensor(out=tmp2[i], in0=hv, in1=rstd2, op=ALU.mult), ["H%d_%d" % (s_, kc), "rstd2"], ["tmp2%d" % i])
                P.op("dve", lambda e, kc=kc, i=i, w=w: e.tensor_scalar(out=hn2f[i], in0=tmp2[i], scalar1=A2t[:, kc, w:w + 1], scalar2=B2(kc, w), op0=ALU.mult, op1=ALU.add),
                     ["tmp2%d" % i, "adaS", "A2t"], ["hn2f%d" % i])
                P.op("act", lambda e, kc=kc, i=i, w=w: e.activation(out=hn2b[:, kc, :], in_=tmp2[i], func=AF.Identity, bias=B2(kc, w), scale=A2t[:, kc, w:w + 1]),
                     ["tmp2%d" % i, "adaS", "A2t"], ["hn2b"])
                for th in range(2):
                    P.op("pe", lambda e, plg=plgs[th][0], i=i, kc=kc, th=th: e.matmul(plg[:, 0:NE], hn2f[i][:, th * 128:(th + 1) * 128], Wr[:, kc, :], start=(kc == 0), stop=(kc == KC - 1)),
                         ["hn2f%d" % i, "Wr"], [plgs[th][1]])
            P.dma("sp", HN2d[:, :, s_ * BLK:(s_ + 1) * BLK].rearrange("c p t -> p c t"), hn2b, ["hn2b"], ["HN2_%d" % s_], "hn2b")
            gp = gts_bufs[rctr[0] % 2]
            gpk = "gts%d" % (rctr[0] % 2)
            rctr[0] += 1
            for th in range(2):
                lgp = plgs[th][0][:, 0:NE]
                P.op("dve", lambda e, lgp=lgp, th=th: e.tensor_tensor(out=lg[:, th, :], in0=lgp, in1=brbc, op=ALU.add), [plgs[th][1], "brbc"], ["lg"])
            def bc2(a):
                return a.unsqueeze(2).to_broadcast([128, 2, NE])
            def bc8(a):
                return a.unsqueeze(2).to_broadcast([128, 8, 4])
            sc3 = sc.rearrange("p t (g k) -> p (t g) k", k=4)
            sc23 = sc2.rearrange("p t (g k) -> p (t g) k", k=4)
            msk3 = msk.rearrange("p t (g k) -> p (t g) k", k=4)
            P.op("dve", lambda e: e.tensor_reduce(out=mx[:, 0, :], in_=lg, axis=AX.X, op=ALU.max), ["lg"], ["mx"])
            P.op("dve", lambda e: e.tensor_tensor(out=lg, in0=lg, in1=bc2(mx[:, 0, :]), op=ALU.subtract), ["lg", "mx"], ["lg"])
            P.op("act", lambda e: e.activation(out=ex, in_=lg, func=AF.Exp), ["lg"], ["ex"])
            P.op("dve", lambda e: e.tensor_reduce(out=mx[:, 1, :], in_=ex, axis=AX.X, op=ALU.add), ["ex"], ["mx"])
            P.op("dve", lambda e: e.reciprocal(out=mx[:, 2, :], in_=mx[:, 1, :]), ["mx"], ["mx"])
            P.op("dve", lambda e: e.tensor_tensor(out=sc, in0=ex, in1=bc2(mx[:, 2, :]), op=ALU.mult), ["ex", "mx"], ["sc"])
            P.op("dve", lambda e: e.tensor_reduce(out=gs, in_=sc3, axis=AX.X, op=ALU.max), ["sc"], ["gs"])
            P.op("dve", lambda e: e.tensor_tensor(out=sc23, in0=sc3, in1=bc8(gs), op=ALU.is_equal), ["sc", "gs"], ["sc2"])
            P.op("dve", lambda e: e.scalar_tensor_tensor(out=sc2, in0=sc2, scalar=-4.0, in1=sc, op0=ALU.mult, op1=ALU.add), ["sc2", "sc"], ["sc2"])
            P.op("dve", lambda e: e.tensor_reduce(out=gm, in_=sc23, axis=AX.X, op=ALU.max), ["sc2"], ["gm"])
            P.op("dve", lambda e: e.tensor_tensor(out=gs, in0=gs, in1=gm, op=ALU.add), ["gs", "gm"], ["gs"])
            P.op("dve", lambda e: e.tensor_reduce(out=mx[:, 3, :], in_=gs.rearrange("p (t g) -> p t g", g=4), axis=AX.X, op=ALU.max), ["gs"], ["mx"])
            P.op("dve", lambda e: e.tensor_tensor(out=gm.rearrange("p (t g) -> p t g", g=4), in0=gs.rearrange("p (t g) -> p t g", g=4),
                                                 in1=mx[:, 3, :].unsqueeze(2).to_broadcast([128, 2, 4]), op=ALU.is_equal), ["gs", "mx"], ["gm"])
            P.op("dve", lambda e: e.scalar_tensor_tensor(out=msk3, in0=sc3, scalar=1.0, in1=bc8(gm), op0=ALU.add, op1=ALU.mult), ["sc", "gm"], ["msk"])
            P.op("dve", lambda e: e.tensor_scalar(out=msk, in0=msk, scalar1=-1.0, scalar2=None, op0=ALU.add), ["msk"], ["msk"])
            P.op("dve", lambda e: e.tensor_reduce(out=mx[:, 4, :], in_=msk, axis=AX.X, op=ALU.max), ["msk"], ["mx"])
            P.op("dve", lambda e: e.tensor_tensor(out=sc2, in0=msk, in1=bc2(mx[:, 4, :]), op=ALU.is_equal), ["msk", "mx"], ["sc2"])
            P.op("dve", lambda e: e.scalar_tensor_tensor(out=sc2, in0=sc2, scalar=-4.0, in1=msk, op0=ALU.mult, op1=ALU.add), ["sc2", "msk"], ["sc2"])
            P.op("dve", lambda e: e.tensor_reduce(out=mx[:, 5, :], in_=sc2, axis=AX.X, op=ALU.max), ["sc2"], ["mx"])
            P.op("dve", lambda e: e.tensor_tensor(out=sc2, in0=msk, in1=bc2(mx[:, 5, :]), op=ALU.is_ge), ["msk", "mx"], ["sc2"])
            P.op("dve", lambda e: e.tensor_tensor(out=mx[:, 6, :], in0=mx[:, 4, :], in1=mx[:, 5, :], op=ALU.add), ["mx"], ["mx"])
            P.op("dve", lambda e: e.reciprocal(out=mx[:, 7, :], in_=mx[:, 6, :]), ["mx"], ["mx"])
            P.op("dve", lambda e: e.tensor_tensor(out=sc2, in0=sc2, in1=msk, op=ALU.mult), ["sc2", "msk"], ["sc2"])
            P.op("dve", lambda e, gp=gp: e.tensor_tensor(out=gp, in0=sc2, in1=bc2(mx[:, 7, :]), op=ALU.mult), ["sc2", "mx"], [gpk])

            def gate_tail(gp=gp, gpk=gpk, s_=s_):
                for th in range(2):
                    (tb_, thh_) = rot_w.next()
                    ptg, ptgk = ps_slot(tb_, thh_)
                    P.op("pe", lambda e, ptg=ptg, gp=gp, th=th: e.matmul(ptg[0:NE, 0:128], gp[:, th, :], identF, start=True, stop=True), [gpk, "CONST"], [ptgk])
                    t0 = s_ * BLK + th * 128
                    P.op("act", lambda e, ptg=ptg, t0=t0: e.activation(out=GT[:, t0:t0 + 128], in_=ptg[0:NE, 0:128], func=AF.Copy), [ptgk], ["GT"])
            pending_tail.append(gate_tail)
        while pending_tail:
            pending_tail.pop(0)()
        P.fence()
        A.release()
        A.hi = hi_save
        if stop == "A2_%d" % l:
            done[0] = True
            break

        A.mark()
        SEL = A.alloc([NE, NE, 128], BF16)
        P.dma("pool", SEL, sel_in, [], ["SEL"], "SEL")
        ring = [A.alloc([128, 4096], BF16) for _ in range(5)]
        ringi = [0]
        he = [A.alloc([128, 4, 512], BF16) for _ in range(2)]
        hn2t = [A.alloc([128, KC, 512], BF16) for _ in range(2)]
        sgs = [A.alloc([128, 512], F32) for _ in range(2)]
        tts = [A.alloc([128, 512], F32) for _ in range(2)]
        if l == 0:
            tiles = [(i * 512, 512, 0) for i in range(5)] + [(2560, 256, 1)]
        else:
            tiles = [(256 + i * 512, 512, 0) for i in range(4)]
        rot_g = Rot([0, 1])
        rot_u = Rot([2, 3])
        rot_y = Rot([4, 5])
        rot_gb = Rot([6, 7])
        uctr = 0

        def ring_next():
            i = ringi[0] % 5
            ringi[0] += 1
            return ring[i], "ring%d" % i

        units = [(ex_, t) for ex_ in range(b_experts) for t in tiles]
        wcur = {}
        hide_ada = (l == 0 and b_experts >= 9 and stop is None and HIDE_ADA)
        if hide_ada:
            wa1 = A.alloc([128, KC, 768], BF16)
            ps7, ps7k = ps_slot(7)

        def ada_piece_dma(pc):
            P.dma("pool", wa1, w_ada[1, :, pc * 768:(pc + 1) * 768].rearrange("(kc p) n -> p kc n", p=128), [], ["wa1"], "wa1")

        def ada_piece_mm(pc):
            for jj in range(6):
                j = pc * 6 + jj
                for kc in range(KC):
                    P.op("pe", lambda e, jj=jj, kc=kc, j=j: e.matmul(ps7[:, 2 * j:2 * j + 2], wa1[:, kc, jj * 128:(jj + 1) * 128], condT[:, kc, :], start=(kc == 0), stop=(kc == KC - 1)),
                         ["wa1", "condT"], [ps7k])
            P.op("dve", lambda e, pc=pc: e.tensor_copy(out=adaraw[:, 12 * pc:12 * pc + 12], in_=ps7[:, 12 * pc:12 * pc + 12]), [ps7k], ["adaraw"])
            if pc == 7:
                ada1_done[0] = True

        def moe_gu(u):
            (ex_, (t0, n, w)) = units[u]
            if ex_ not in wcur:
                wg_t, wgk = ring_next()
                wg_ = wg_t.rearrange("p (k f) -> p k f", f=FE)
                P.dma("pool", wg_, w_gate[l, ex_].rearrange("(kc p) f -> p kc f", p=128), [], [wgk], wgk)
                wu_t, wuk = ring_next()
                wu_ = wu_t.rearrange("p (k f) -> p k f", f=FE)
                P.dma("pool", wu_, w_up[l, ex_].rearrange("(kc p) f -> p kc f", p=128), [], [wuk], wuk)
                wd_t, wdk = ring_next()
                wd_ = wd_t.rearrange("p (k f) -> p k f", f=D)
                P.dma("pool", wd_, w_down[l, ex_].rearrange("(fc p) d -> p fc d", p=128), [], [wdk], wdk)
                wcur[ex_] = (wg_, wgk, wu_, wuk, wd_, wdk)
            (wg_, wgk, wu_, wuk, wd_, wdk) = wcur[ex_]
            ui = u % 2
            hk = "hn2t%d" % ui
            slots_ = sorted(set([t0 // BLK, (t0 + n - 1) // BLK]))
            P.dma("sp", hn2t[ui][:, :, 0:n], HN2d[:, :, t0:t0 + n].rearrange("c p t -> p c t"), ["HN2_%d" % s for s in slots_], [hk], hk)
            gbb = rot_gb.next()
            pgb, pgbk = ps_slot(gbb)
            P.op("pe", lambda e, pgb=pgb, ex_=ex_, t0=t0, n=n: e.matmul(pgb[:, 0:n], SEL[:, ex_, :], GT[:, t0:t0 + n], start=True, stop=True), ["SEL", "GT"], [pgbk])
            for fc in range(4):
                gbk_ = rot_g.next()
                pgg, pggk = ps_slot(gbk_)
                ubk_ = rot_u.next()
                puu, puuk = ps_slot(ubk_)
                for kc in range(KC):
                    P.op("pe", lambda e, pgg=pgg, kc=kc, fc=fc, ui=ui, n=n, wg_=wg_: e.matmul(pgg[:, 0:n], wg_[:, kc, fc * 128:(fc + 1) * 128], hn2t[ui][:, kc, 0:n], start=(kc == 0), stop=(kc == KC - 1)),
                         [wgk, hk], [pggk])
                for kc in range(KC):
                    P.op("pe", lambda e, puu=puu, kc=kc, fc=fc, ui=ui, n=n, wu_=wu_: e.matmul(puu[:, 0:n], wu_[:, kc, fc * 128:(fc + 1) * 128], hn2t[ui][:, kc, 0:n], start=(kc == 0), stop=(kc == KC - 1)),
                         [wuk, hk], [puuk])
                i = fc % 2
                P.op("act", lambda e, pgg=pgg, i=i, n=n: e.activation(out=sgs[i][:, 0:n], in_=pgg[:, 0:n], func=AF.Silu), [pggk], ["sgs%d" % i])
                P.op("dve", lambda e, puu=puu, i=i, n=n: e.tensor_tensor(out=tts[i][:, 0:n], in0=puu[:, 0:n], in1=sgs[i][:, 0:n], op=ALU.mult), [puuk, "sgs%d" % i], ["tts%d" % i])
                P.op("dve", lambda e, pgb=pgb, i=i, n=n, ui=ui, fc=fc: e.tensor_tensor(out=he[ui][:, fc, 0:n], in0=pgb[:, 0:n], in1=tts[i][:, 0:n], op=ALU.mult),
                     [pgbk, "tts%d" % i], ["he%d_%d" % (ui, fc)])

        def moe_dn(u):
            (ex_, (t0, n, w)) = units[u]
            (wg_, wgk, wu_, wuk, wd_, wdk) = wcur[ex_]
            ui = u % 2
            slots_ = sorted(set([t0 // BLK, (t0 + n - 1) // BLK]))
            for dc in range(KC):
                ybk = rot_y.next()
                py, pyk = ps_slot(ybk)
                for fc in range(4):
                    P.op("pe", lambda e, py=py, fc=fc, dc=dc, ui=ui, n=n, wd_=wd_: e.matmul(py[:, 0:n], wd_[:, fc, dc * 128:(dc + 1) * 128], he[ui][:, fc, 0:n], start=(fc == 0), stop=(fc == 3)),
                         [wdk, "he%d_%d" % (ui, fc)], [pyk])
                hv = H[:, dc, t0:t0 + n]
                hkeys_ = ["H%d_%d" % (s, dc) for s in slots_]
                P.op("dve", lambda e, py=py, hv=hv, dc=dc, w=w, n=n: e.scalar_tensor_tensor(out=hv, in0=py[:, 0:n], scalar=G2(dc, w), in1=hv, op0=ALU.mult, op1=ALU.add),
                     [pyk, "adaS"] + hkeys_, hkeys_)

        TPE = len(tiles)
        for u in range(len(units)):
            moe_gu(u)
            if hide_ada and 1 <= units[u][0] <= 8:
                if u % TPE == 0:
                    ada_piece_dma(units[u][0] - 1)
                if u % TPE == TPE - 1:
                    ada_piece_mm(units[u][0] - 1)
            if u >= 1:
                moe_dn(u - 1)
        moe_dn(len(units) - 1)
        P.fence()
        A.release()
        if stop == "B_%d" % l:
            done[0] = True
            break

    if debug:
        for kc in range(KC):
            P.dma("sp", dbgH[kc], H[:, kc, :], ["H%d_%d" % (s, kc) for s in range(11)], ["dbgH"], "dbgH")
        P.dma("sp", dbgGT, GT, ["GT"], ["dbgGT"], "dbgGT")
    for kc in range(KC):
        P.dma("sp", yT[kc], H[:, kc, BLK:9 * BLK], ["H%d_%d" % (s, kc) for s in range(1, 9)], ["yout"], "yout")
    P.fence()
    P.emit()
    es.close()
    return nc


def _consts():
    c = np.zeros((128, 5 * 128), np.float32)
    c[:, 0:128] = np.eye(128, dtype=np.float32)
    bd = np.zeros((128, 128), np.float32)
    bd[0:64, 0:64] = 1.0
    bd[64:128, 64:128] = 1.0
    c[:, 128:256] = bd
    c[:, 256:384] = 1.0
    c[:, 384:448] = 1.0
    c[:, 512 + 64:640] = 1.0
    sel = np.zeros((NE, NE, 128), np.float32)
    for e in range(NE):
        sel[e, e, :] = 1.0
    return c, sel


def _bias_table(rpb_l):
    bt = np.full((128, 8, 14, 64), NEG, np.float32)
    qc = np.arange(64)
    cs = np.clip(qc - 8, 0, 48)
    for s in range(2):
        for jj in range(14):
            dr = 13 + s - jj
            if dr < 0 or dr > 14:
                continue
            for kc in range(64):
                valid = (kc >= cs) & (kc < cs + 16)
                dc = kc - qc + 15
                vq = qc[valid]
                bt[s * 64 + kc, :, jj, vq] = rpb_l[:, dr, dc[valid]].T
    return bt.reshape(128, 8, 14 * 64)


def _row_masks(r0):
    rm = np.zeros((128, 10, 6, 4), np.float32)
    for b in range(1, 11):
        r = r0 - 8 + 4 * b
        for qr in range(4):
            q = r + qr
            if q < 0 or q > 255:
                rm[:, b - 1, :, qr] = 1.0
                continue
            rs = min(max(q - 4, 0), 248)
            for c in range(6):
                for s in range(2):
                    a = r - 4 + 2 * c + s
                    if 0 <= a <= 255 and rs <= a < rs + 8:
                        rm[s * 64:(s + 1) * 64, b - 1, c, qr] = 1.0
    return rm


def _rc_tables(core):
    wins = (2, 4, 8, 16)
    rc = np.zeros((128, 2 + 3 * 2 * 16), np.float32)
    rcb = np.zeros((128, 3, 2, 16), np.float32)
    t8 = np.arange(8)
    for g, wd in enumerate(wins):
        c, hh = g // 2, g % 2
        pr = slice(64 * hh, 64 * hh + 64)
        rc[pr, c] = 1.0 / wd
        rcb[pr, :, c, :] = 1.0 / wd
        if core == 0:
            lo = np.clip(t8 - wd // 2, 0, None)
            hi = t8 + wd // 2
            rcb[pr, 0, c, 0:8] = 1.0 / (hi - lo)
        if core == NCORES - 1:
            tt = 16384 - 8 + t8
            lo = tt - wd // 2
            hi = np.clip(tt + wd // 2, None, 16384)
            rcb[pr, 1, c, 8:16] = 1.0 / (hi - lo)
        lo = np.clip(t8 - wd // 2, 0, 256)
        hi = np.clip(t8 + wd // 2, 0, 256)
        rcb[pr, 2, c, 0:8] = 1.0 / (hi - lo)
        tt = 248 + t8
        lo = np.clip(tt - wd // 2, 0, 256)
        hi = np.clip(tt + wd // 2, 0, 256)
        rcb[pr, 2, c, 8:16] = 1.0 / (hi - lo)
    rc[:, 2:] = rcb.reshape(128, -1)
    return rc


def _vecT(v):
    return np.ascontiguousarray(v.reshape(-1, 128).T)


_NC_CACHE = {}


def kernel(x, c, ctx, c_ctx, w_ada, b_ada, norm1, w_in, pool_w, pool_scale, q_norm, k_norm, rpb,
           sg_w, sg_b, sg_norm, w_out, norm2, w_router, b_router, w_gate, w_up, w_down):
    f = lambda a: np.ascontiguousarray(np.asarray(a, dtype=np.float32))
    x, c, ctx, c_ctx = f(x), f(c), f(ctx), f(c_ctx)
    w_ada, b_ada, norm1, w_in, pool_w, pool_scale = f(w_ada), f(b_ada), f(norm1), f(w_in), f(pool_w), f(pool_scale)
    q_norm, k_norm, rpb, sg_w, sg_b, sg_norm = f(q_norm), f(k_norm), f(rpb), f(sg_w), f(sg_b), f(sg_norm)
    w_out, norm2, w_router, b_router, w_gate, w_up, w_down = f(w_out), f(norm2), f(w_router), f(b_router), f(w_gate), f(w_up), f(w_down)

    consts, sel = _consts()
    xg = x[0]
    ctxT = np.ascontiguousarray(ctx[0].T.reshape(KC, 128, BLK))
    cT = np.stack([_vecT(c[0]), _vecT(c_ctx)], axis=-1)
    b_adaT = np.stack([np.ascontiguousarray(b_ada[l].reshape(48, 128).T) for l in range(2)])
    n1T = np.stack([_vecT(norm1[l]) for l in range(2)])
    n2T = np.stack([_vecT(norm2[l]) for l in range(2)])
    pw_bd = np.zeros((2, 128, 2, 128), np.float32)
    for l in range(2):
        for g in range(4):
            cc, hh = g // 2, g % 2
            pw_bd[l, 64 * hh:64 * hh + 64, cc, 64 * hh:64 * hh + 64] = pool_w[l, g]
    pscaleT = np.stack([np.ascontiguousarray(pool_scale[l].reshape(2, 128).T) for l in range(2)])
    qgT = np.stack([np.ascontiguousarray(q_norm[l].reshape(4, 128).T) for l in range(2)])
    kgT = np.stack([np.ascontiguousarray(k_norm[l].reshape(4, 128).T) for l in range(2)])
    bt = np.stack([_bias_table(rpb[l]) for l in range(2)])
    sgwT = np.stack([np.ascontiguousarray(np.transpose(sg_w[l], (2, 0, 1))) for l in range(2)])
    sgbT = np.zeros((2, 128, 2, 128), np.float32)
    for l in range(2):
        for j in range(2):
            for s in range(2):
                sgbT[l, 64 * s:64 * s + 64, j, :] = sg_b[l, 2 * j + s][None, :]
    sggain = np.ascontiguousarray(sg_norm.reshape(2, 256))

    key = "main"
    if key not in _NC_CACHE:
        _NC_CACHE[key] = build_program()
    nc = _NC_CACHE[key]

    in_maps = []
    for core in range(NCORES):
        r0 = 32 * core
        xw = np.zeros((NBL * BLK, D), np.float32)
        ra, rb = r0 - 8, r0 + 40
        va, vb = max(ra, 0), min(rb, 256)
        xw[(va - ra) * 64:(vb - ra) * 64] = xg[va * 64:vb * 64]
        xT = np.ascontiguousarray(xw.T.reshape(KC, 128, NBL * BLK))
        bvalid = np.zeros((128, 13), np.float32)
        for b in range(12):
            r = r0 - 8 + 4 * b
            bvalid[:, b] = 1.0 if 0 <= r <= 252 else 0.0
        bvalid[:, 12] = 1.0
        in_maps.append({
            "xT": xT, "ctxT": ctxT, "cT": cT, "w_ada": w_ada, "b_adaT": b_adaT, "n1T": n1T, "n2T": n2T,
            "w_in": w_in, "pw_bd": pw_bd, "pscaleT": pscaleT, "qgT": qgT, "kgT": kgT, "bt": bt,
            "rm": _row_masks(r0), "rc": _rc_tables(core), "bvalid": bvalid, "sgwT": sgwT, "sgbT": sgbT,
            "sggain": sggain, "w_out": w_out, "w_router": w_router, "b_router": b_router,
            "w_gate": w_gate, "w_up": w_up, "w_down": w_down, "consts": consts, "sel": sel,
        })
    res = run_bass_kernel_spmd(nc, in_maps, core_ids=list(range(NCORES)))
    out = np.zeros((1, 16384, D), np.float32)
    for core in range(NCORES):
        yT = res.results[core]["yT"]
        out[0, core * 2048:(core + 1) * 2048] = yT.reshape(D, 2048).T
    return out
```

```python
from contextlib import ExitStack
import numpy as np
import concourse.bass as bass
import concourse.mybir as mybir
from concourse.bass_utils import run_bass_kernel_spmd

F32 = mybir.dt.float32
BF16 = mybir.dt.bfloat16
AF = mybir.ActivationFunctionType
ALU = mybir.AluOpType
AX = mybir.AxisListType

NCORES = 8
D = 1024
KC = 8
BLK = 256
NBL = 12
CTXB = 12
TA = 13 * BLK
TH = 11 * BLK
PIT = 8 + 12 * BLK + 16 + BLK + 8
PI_CTX = 8 + 12 * BLK + 16
NEG = -30000.0
EPS = 1e-6
NE = 16
FE = 512
HIDE_ADA = True


def hslot(b):
    return 10 if b == CTXB else b - 1


class Prog:
    def __init__(self, nc, es):
        self.nc = nc
        self.es = es
        self.eng = {"pe": nc.tensor, "act": nc.scalar, "dve": nc.vector, "pool": nc.gpsimd, "sp": nc.sync}
        self.stream = {k: [] for k in self.eng}
        self.cnt = {k: 0 for k in self.eng}
        self.sems = {}
        self.dcnt = {}
        self.waited = {k: {} for k in self.eng}
        self.lastw = {}
        self.readers = {}
        for k in self.eng:
            self._sem("E_" + k)

    def _sem(self, key):
        if key not in self.sems:
            self.sems[key] = self.es.enter_context(self.nc.semaphore("s_" + key))
        return self.sems[key]

    def _cur(self, semkey):
        if semkey.startswith("E_"):
            return self.cnt[semkey[2:]]
        return 16 * self.dcnt[semkey]

    def _deps(self, eng, reads, writes):
        deps = {}
        def add(tok):
            if tok is None:
                return
            sk, val = tok
            if sk.startswith("D_"):
                val = self._cur(sk)
            if eng == "pe" and sk == "E_pe":
                return
            if deps.get(sk, 0) < val:
                deps[sk] = val
        for k in reads:
            add(self.lastw.get(k))
        for k in writes:
            add(self.lastw.get(k))
            for t in self.readers.get(k, ()):
                add(t)
        for sk, val in deps.items():
            if self.waited[eng].get(sk, 0) < val:
                self.waited[eng][sk] = val
                self.stream[eng].append(("w", sk, val))

    def _commit(self, tok, reads, writes):
        for k in reads:
            self.readers.setdefault(k, []).append(tok)
        for k in writes:
            self.lastw[k] = tok
            self.readers[k] = []

    def op(self, eng, fn, reads=(), writes=()):
        self._deps(eng, reads, writes)
        self.cnt[eng] += 1
        tok = ("E_" + eng, self.cnt[eng])
        self.stream[eng].append(("o", fn, "E_" + eng, 1))
        self._commit(tok, reads, writes)

    def dma(self, q, out, in_, reads, writes, dkey):
        sk = "D_" + dkey
        self._sem(sk)
        self.dcnt.setdefault(sk, 0)
        self._deps(q, reads, writes)
        self.dcnt[sk] += 1
        tok = (sk, 16 * self.dcnt[sk])
        self.stream[q].append(("o", lambda e: e.dma_start(out=out, in_=in_), sk, 16))
        self._commit(tok, reads, writes)

    def fence(self):
        for e in self.eng:
            for o in self.eng:
                if o == "sp":
                    continue
                v = self.cnt[o]
                if (e != o or e != "pe") and self.waited[e].get("E_" + o, 0) < v:
                    self.waited[e]["E_" + o] = v
                    self.stream[e].append(("w", "E_" + o, v))
            for sk, c in self.dcnt.items():
                if self.waited[e].get(sk, 0) < 16 * c:
                    self.waited[e][sk] = 16 * c
                    self.stream[e].append(("w", sk, 16 * c))

    def emit(self):
        nc = self.nc
        with nc.Block() as block:
            def replay(name):
                def body(e):
                    for it in self.stream[name]:
                        if it[0] == "w":
                            e.wait_ge(self.sems[it[1]], it[2])
                        else:
                            it[1](e).then_inc(self.sems[it[2]], it[3])
                return body
            block.tensor(replay("pe"))
            block.scalar(replay("act"))
            block.vector(replay("dve"))
            block.gpsimd(replay("pool"))
            block.sync(replay("sp"))


class Arena:
    def __init__(self, nc, nbytes):
        self.t = nc.alloc_sbuf_tensor("arena", [128, nbytes // 2], BF16)
        self.nbytes = nbytes
        self.top = 0
        self.hi = nbytes
        self.marks = []

    def alloc(self, shape, dt, hi=False):
        esz = 4 if dt == F32 else 2
        n = 1
        for s in shape[1:]:
            n *= s
        nb = (n * esz + 31) // 32 * 32
        if hi:
            self.hi -= nb
            off = self.hi
        else:
            off = self.top
            self.top += nb
        assert self.top <= self.hi, ("arena overflow", self.top, self.hi, self.nbytes)
        ap = self.t[:, off // 2: off // 2 + n * esz // 2]
        if dt == F32:
            ap = ap.bitcast(F32)
        ap = ap[0:shape[0], :]
        if len(shape) == 3:
            ap = ap.rearrange("p (a b) -> p a b", b=shape[2])
        elif len(shape) == 4:
            ap = ap.rearrange("p (a b c) -> p a b c", b=shape[2], c=shape[3])
        return ap

    def mark(self):
        self.marks.append(self.top)

    def release(self):
        self.top = self.marks.pop()


def build_program(debug=False, stop=None, b_experts=NE):
    nc = bass.Bass("TRN2", target_bir_lowering=False)
    es = ExitStack()

    def din(name, shape, dt=F32):
        return nc.dram_tensor(name, list(shape), dt, kind="ExternalInput").ap()

    xT = din("xT", [KC, 128, NBL * BLK])
    ctxT = din("ctxT", [KC, 128, BLK])
    cT = din("cT", [128, KC, 2])
    w_ada = din("w_ada", [2, D, 6 * D])
    b_adaT = din("b_adaT", [2, 128, 48])
    n1T = din("n1T", [2, 128, KC])
    n2T = din("n2T", [2, 128, KC])
    w_in = din("w_in", [2, D, 2304])
    pw_bd = din("pw_bd", [2, 128, 2, 128])
    pscaleT = din("pscaleT", [2, 128, 2])
    qgT = din("qgT", [2, 128, 4])
    kgT = din("kgT", [2, 128, 4])
    bt_in = din("bt", [2, 128, 8, 14 * 64])
    rm_in = din("rm", [128, 10, 6, 4])
    rc_in = din("rc", [128, 2 + 3 * 2 * 16])
    bvalid_in = din("bvalid", [128, 13])
    sgwT = din("sgwT", [2, 128, 4, 128])
    sgbT = din("sgbT", [2, 128, 2, 128])
    sggain = din("sggain", [2, 256])
    w_out = din("w_out", [2, D, D])
    w_router = din("w_router", [D, NE])
    b_router = din("b_router", [NE])
    w_gate = din("w_gate", [2, NE, D, FE])
    w_up = din("w_up", [2, NE, D, FE])
    w_down = din("w_down", [2, NE, FE, D])
    consts = din("consts", [128, 3 * 128 + 2 * 128])
    sel_in = din("sel", [NE, NE, 128])
    yT = nc.dram_tensor("yT", [KC, 128, 8 * BLK], F32, kind="ExternalOutput").ap()

    skind = "ExternalOutput" if debug else "Internal"
    QTd = nc.dram_tensor("QTd", [4, 128, TA], BF16, kind=skind).ap()
    KTd = nc.dram_tensor("KTd", [4, 128, TA], BF16, kind=skind).ap()
    Vd = nc.dram_tensor("Vd", [TA, 512], BF16, kind=skind).ap()
    VSd = nc.dram_tensor("VSd", [TA, 4 * 128], BF16, kind=skind).ap()
    PId = nc.dram_tensor("PId", [2, 128, PIT], F32, kind=skind).ap()
    Ud = nc.dram_tensor("Ud", [2, 128, TA], BF16, kind=skind).ap()
    HN2d = nc.dram_tensor("HN2d", [KC, 128, TH], BF16, kind=skind).ap()
    if debug:
        dbgH = nc.dram_tensor("dbgH", [KC, 128, TH], F32, kind="ExternalOutput").ap()
        dbgGT = nc.dram_tensor("dbgGT", [NE, TH], BF16, kind="ExternalOutput").ap()
        dbgAda = nc.dram_tensor("dbgAda", [128, 96], F32, kind="ExternalOutput").ap()
    done = [False]

    P = Prog(nc, es)
    A = Arena(nc, (nc.sbuf_bytes_remaining - 512) // 64 * 64)
    banks = [nc.alloc_psum_tensor("bank%d" % i, [128, 512], F32) for i in range(8)]

    H = A.alloc([128, KC, TH], F32)
    GT = A.alloc([NE, TH], BF16)
    RC = A.alloc([128, 2 + 3 * 2 * 16], F32)
    RCW = RC[:, 0:2]
    RCB = RC[:, 2:98].rearrange("p (s c t) -> p s c t", s=3, c=2)
    CONST = A.alloc([128, 128], F32)
    identF = CONST
    cbf = A.alloc([128, 4 * 128], BF16)
    onesBD = cbf[:, 0:128]
    onesA = cbf[:, 128:256]
    half = [cbf[:, 256:384], cbf[:, 384:512]]
    cTs = A.alloc([128, KC, 2], F32)
    condT = A.alloc([128, KC, 2], BF16)
    adaS = A.alloc([128, 48, 2], F32)
    A1t = A.alloc([128, KC, 2], F32)
    A2t = A.alloc([128, KC, 2], F32)
    bAda = A.alloc([128, 48], F32)
    adaraw = A.alloc([128, 96], F32)
    ada1_done = [False]
    n1s = A.alloc([128, KC], F32)
    n2s = A.alloc([128, KC], F32)
    qg = A.alloc([128, 4], F32)
    kg = A.alloc([128, 4], F32)
    pscale = A.alloc([128, 2], F32)
    bvalid = A.alloc([128, 13], F32)
    epsT = A.alloc([128, 1], F32)
    brbc = A.alloc([128, NE], F32)
    Wr = A.alloc([128, KC, NE], F32)
    RM = A.alloc([128, 10, 6, 4], BF16)
    sgB = A.alloc([128, 2, 128], F32)

    def ps_slot(bank, halfi=None):
        if halfi is None:
            return banks[bank][:, :], "PS%d" % bank
        return banks[bank][:, halfi * 256:(halfi + 1) * 256], "PS%d" % bank

    class Rot:
        def __init__(self, items):
            self.items = items
            self.i = 0
        def next(self):
            it = self.items[self.i % len(self.items)]
            self.i += 1
            return it

    P.dma("sp", CONST, consts[:, 0:128], [], ["CONST"], "CONST")
    P.dma("pool", cbf, consts[:, 128:640], [], ["cbf"], "cbf")
    P.dma("sp", RC, rc_in, [], ["RC"], "RC")
    P.dma("sp", cTs, cT, [], ["cTs"], "cTs")
    P.dma("sp", bvalid, bvalid_in, [], ["bvalid"], "bvalid")
    P.dma("sp", brbc, b_router.partition_broadcast(128), [], ["brbc"], "brbc")
    P.dma("sp", Wr, w_router.rearrange("(kc p) e -> p kc e", p=128), [], ["Wr"], "Wr")
    P.dma("pool", RM, rm_in, [], ["RM"], "RM")
    P.op("pool", lambda e: e.memset(epsT, EPS), [], ["eps"])
    if debug:
        P.op("pool", lambda e: e.memset(GT, 0.0), [], ["GT"])
    for kc in range(KC):
        P.dma("sp", H[:, kc, 0:10 * BLK], xT[kc, :, BLK:11 * BLK], [], ["H%d_%d" % (s, kc) for s in range(10)], "Hinit")
        P.dma("sp", H[:, kc, 10 * BLK:11 * BLK], ctxT[kc], [], ["H10_%d" % kc], "Hinit")
    P.op("act", lambda e: e.activation(out=condT, in_=cTs, func=AF.Silu), ["cTs"], ["condT"])

    A.mark()
    zt = A.alloc([128, 2, 16], F32)
    P.op("pool", lambda e: e.memset(zt, 0.0), [], ["zt"])
    P.dma("sp", PId[:, :, 0:8].rearrange("c p t -> p c t"), zt[:, :, 0:8], ["zt"], ["PIpad"], "zt")
    P.dma("sp", PId[:, :, 8 + 12 * BLK:8 + 12 * BLK + 16].rearrange("c p t -> p c t"), zt, ["zt"], ["PIpad"], "zt")
    P.dma("sp", PId[:, :, PI_CTX + BLK:PI_CTX + BLK + 8].rearrange("c p t -> p c t"), zt[:, :, 0:8], ["zt"], ["PIpad"], "zt")
    P.fence()
    A.release()

    dbg = {}

    for l in range(2):
        last = l == 1
        if done[0]:
            break
        A.mark()
        P.dma("sp", bAda, b_adaT[l], [], ["bAda"], "bAda")
        P.dma("sp", n1s, n1T[l], [], ["n1s"], "n1s")
        P.dma("sp", n2s, n2T[l], [], ["n2s"], "n2s")
        P.dma("sp", qg, qgT[l], [], ["qg"], "qg")
        P.dma("sp", kg, kgT[l], [], ["kg"], "kg")
        P.dma("sp", pscale, pscaleT[l], [], ["pscale"], "pscale")
        P.dma("sp", sgB, sgbT[l], [], ["sgB"], "sgB")
        adaps, adak = ps_slot(7)
        if l == 1 and ada1_done[0]:
            P.op("dve", lambda e: e.tensor_tensor(out=adaS, in0=adaraw.rearrange("p (j w) -> p j w", w=2), in1=bAda.unsqueeze(2).to_broadcast([128, 48, 2]), op=ALU.add),
                 ["adaraw", "bAda"], ["adaS"])
        else:
            wa = [A.alloc([128, KC, 768], BF16) for _ in range(2)]
            for pc in range(8):
                wt = wa[pc % 2]
                wk = "wa%d" % (pc % 2)
                P.dma("pool", wt, w_ada[l, :, pc * 768:(pc + 1) * 768].rearrange("(kc p) n -> p kc n", p=128), [], [wk], wk)
                for jj in range(6):
                    j = pc * 6 + jj
                    for kc in range(KC):
                        P.op("pe", lambda e, wt=wt, jj=jj, kc=kc, j=j: e.matmul(adaps[:, 2 * j:2 * j + 2], wt[:, kc, jj * 128:(jj + 1) * 128], condT[:, kc, :], start=(kc == 0), stop=(kc == KC - 1)),
                             [wk, "condT"], [adak])
            P.op("dve", lambda e: e.tensor_tensor(out=adaS, in0=adaps[:, 0:96].rearrange("p (j w) -> p j w", w=2), in1=bAda.unsqueeze(2).to_broadcast([128, 48, 2]), op=ALU.add),
                 [adak, "bAda"], ["adaS"])
        P.op("dve", lambda e: e.scalar_tensor_tensor(out=A1t, in0=adaS[:, 8:16, :], scalar=1.0, in1=n1s.unsqueeze(2).to_broadcast([128, KC, 2]), op0=ALU.add, op1=ALU.mult),
             ["adaS", "n1s"], ["A1t"])
        P.op("dve", lambda e: e.scalar_tensor_tensor(out=A2t, in0=adaS[:, 32:40, :], scalar=1.0, in1=n2s.unsqueeze(2).to_broadcast([128, KC, 2]), op0=ALU.add, op1=ALU.mult),
             ["adaS", "n2s"], ["A2t"])
        P.op("dve", lambda e: e.tensor_scalar(out=qg, in0=qg, scalar1=0.125, scalar2=None, op0=ALU.mult), ["qg"], ["qg"])
        P.fence()
        A.release()

        if debug and l == 0:
            P.dma("sp", dbgAda, adaS.rearrange("p j w -> p (j w)"), ["adaS"], ["dbgAda"], "dbgAda")
        if stop == "ada%d" % l:
            done[0] = True
            break

        def B1(kc, w): return adaS[:, kc, w:w + 1]
        def G1(kc, w): return adaS[:, 16 + kc, w:w + 1]
        def B2(kc, w): return adaS[:, 24 + kc, w:w + 1]
        def G2(kc, w): return adaS[:, 40 + kc, w:w + 1]

        A.mark()
        Win = A.alloc([128, KC, 2304], BF16)
        for kc in range(KC):
            P.dma("pool", Win[:, kc, :], w_in[l, kc * 128:(kc + 1) * 128, :], [], ["Win"], "Win")
        hi_save = A.hi
        Wout = A.alloc([128, KC, D], BF16, hi=True)
        BT = A.alloc([128, 8, 14 * 64], BF16, hi=True)
        P.dma("pool", BT, bt_in[l], [], ["BT"], "BT")
        for kc in range(KC):
            P.dma("pool", Wout[:, kc, :], w_out[l, kc * 128:(kc + 1) * 128, :], [], ["Wout"], "Wout")
        sgG = A.alloc([128, 256], F32)
        P.dma("sp", sgG, sggain[l].partition_broadcast(128), [], ["sgG"], "sgG")
        hn = A.alloc([128, KC, BLK], BF16)
        hn_b = A.alloc([128, KC, BLK], BF16)
        sqk = [A.alloc([128, BLK], BF16) for _ in range(2)]
        tmpk = [A.alloc([128, BLK], F32) for _ in range(2)]
        xtmp = A.alloc([128, KC, BLK], F32)
        rr = A.alloc([128, BLK], F32)
        rstd = A.alloc([128, BLK], F32)
        pis = [A.alloc([128, BLK], F32) for _ in range(2)]
        qs = [A.alloc([128, BLK], BF16) for _ in range(2)]
        sqb = [A.alloc([128, BLK], BF16) for _ in range(2)]
        r2 = [A.alloc([128, BLK], F32) for _ in range(2)]
        ri2 = [A.alloc([128, BLK], F32) for _ in range(2)]
        us = [A.alloc([128, BLK], BF16) for _ in range(2)]
        vun = [A.alloc([128, 512], BF16) for _ in range(2)]
        vspad = [A.alloc([128, 4, 128], BF16) for _ in range(2)]
        ggs = [A.alloc([128, 256], F32) for _ in range(2)]
        gsq = A.alloc([128, 256], F32)
        ss4 = A.alloc([128, 4], F32)
        for i in range(2):
            P.op("pool", lambda e, i=i: e.memset(vspad[i], 0.0), [], ["vspad%d" % i])
        rot_f = Rot([(1, 0), (2, 0), (3, 0), (4, 0), (5, 0)])
        rot_s = Rot([(0, 0)])
        rot_s2 = Rot([(6, 0), (7, 0)])
        rot_v = Rot([4, 5])
        cnt2 = [0]

        if l == 0:
            a1_blocks = [(CTXB, "full"), (0, "kvp")] + [(b, "full") for b in range(1, 11)] + [(11, "kvp")]
        else:
            a1_blocks = [(CTXB, "kv"), (1, "kvp")] + [(b, "full") for b in range(2, 10)] + [(10, "kvp")]

        hn_bufs = [hn, hn_b]

        def a1_norm(bi):
            (b, mode) = a1_blocks[bi]
            hb = bi % 2
            hn_ = hn_bufs[hb]
            w = 1 if b == CTXB else 0
            if l == 0 and b in (0, 11):
                if b == 0:
                    for kc in range(KC):
                        P.dma("sp", xtmp[:, kc, :], xT[kc, :, b * BLK:(b + 1) * BLK], [], ["xtmp"], "xtmp")
                hsrc = lambda kc: xtmp[:, kc, :]
                hkeys = lambda kc: ["xtmp"]
            else:
                s_ = hslot(b)
                hsrc = lambda kc, s_=s_: H[:, kc, s_ * BLK:(s_ + 1) * BLK]
                hkeys = lambda kc, s_=s_: ["H%d_%d" % (s_, kc)]
            (sb_, sh_) = rot_s.next()
            ssp, ssk = ps_slot(sb_, sh_)
            for kc in range(KC):
                i = cnt2[0] % 2
                cnt2[0] += 1
                if kc % 2 == 0:
                    P.op("act", lambda e, kc=kc, i=i, hsrc=hsrc: e.activation(out=sqk[i], in_=hsrc(kc), func=AF.Square), hkeys(kc), ["sqk%d" % i])
                else:
                    P.op("dve", lambda e, kc=kc, i=i, hsrc=hsrc: e.tensor_tensor(out=sqk[i], in0=hsrc(kc), in1=hsrc(kc), op=ALU.mult), hkeys(kc), ["sqk%d" % i])
                P.op("pe", lambda e, kc=kc, i=i, ssp=ssp: e.matmul(ssp, onesA, sqk[i], start=(kc == 0), stop=(kc == KC - 1)), ["sqk%d" % i, "cbf"], [ssk])
            P.op("act", lambda e, ssp=ssp: e.activation(out=rr, in_=ssp, func=AF.Ln, bias=epsT, scale=1.0 / D), [ssk, "eps"], ["rr"])
            P.op("act", lambda e: e.activation(out=rstd, in_=rr, func=AF.Exp, scale=-0.5), ["rr"], ["rstd"])
            for kc in range(KC):
                i = kc % 2
                if kc % 2 == 0:
                    P.op("dve", lambda e, kc=kc, i=i, hsrc=hsrc: e.tensor_tensor(out=tmpk[i], in0=hsrc(kc), in1=rstd, op=ALU.mult), hkeys(kc) + ["rstd"], ["tmpk%d" % i])
                    P.op("act", lambda e, kc=kc, i=i, w=w, hn_=hn_: e.activation(out=hn_[:, kc, :], in_=tmpk[i], func=AF.Identity, bias=B1(kc, w), scale=A1t[:, kc, w:w + 1]),
                         ["tmpk%d" % i, "adaS", "A1t"], ["hn%d_%d" % (hb, kc)])
                else:
                    P.op("pool", lambda e, kc=kc, i=i, hsrc=hsrc: e.tensor_tensor(out=tmpk[i], in0=hsrc(kc), in1=rstd, op=ALU.mult), hkeys(kc) + ["rstd"], ["tmpk%d" % i])
                    P.op("dve", lambda e, kc=kc, i=i, w=w, hn_=hn_: e.tensor_scalar(out=hn_[:, kc, :], in0=tmpk[i], scalar1=A1t[:, kc, w:w + 1], scalar2=B1(kc, w), op0=ALU.mult, op1=ALU.add),
                         ["tmpk%d" % i, "adaS", "A1t"], ["hn%d_%d" % (hb, kc)])

        a1_norm(0)
        for bi, (b, mode) in enumerate(a1_blocks):
            if bi + 1 < len(a1_blocks):
                a1_norm(bi + 1)
            if l == 0 and bi == 1:
                for kc in range(KC):
                    P.dma("sp", xtmp[:, kc, :], xT[kc, :, 11 * BLK:12 * BLK], [], ["xtmp"], "xtmp")
            hb = bi % 2
            hn = hn_bufs[hb]
            w = 1 if b == CTXB else 0
            tok0 = b * BLK

            def fchunk(col0):
                (fb, fh) = rot_f.next()
                pp, pk = ps_slot(fb, fh)
                for kc in range(KC):
                    P.op("pe", lambda e, kc=kc, pp=pp, hn=hn: e.matmul(pp, Win[:, kc, col0:col0 + 128], hn[:, kc, :], start=(kc == 0), stop=(kc == KC - 1)),
                         ["Win", "hn%d_%d" % (hb, kc)], [pk])
                return pp, pk

            if mode in ("full", "kvp"):
                for c in range(2):
                    pp, pk = fchunk(c * 128)
                    i = c
                    P.op("dve", lambda e, pp=pp, i=i, b=b: e.tensor_scalar(out=pis[i], in0=pp, scalar1=bvalid[:, b:b + 1], scalar2=None, op0=ALU.mult), [pk, "bvalid"], ["pis%d" % i])
                    pt0 = (PI_CTX if b == CTXB else 8 + b * BLK)
                    P.dma("sp", PId[c, :, pt0:pt0 + BLK], pis[i], ["pis%d" % i], ["PI_%d" % b], "pis%d" % i)
            qk_list = []
            if mode == "full":
                qk_list += [("q", c) for c in range(4)]
            qk_list += [("k", c) for c in range(4)]
            def qk_finish(pend):
                (which, c, pp, pk, i) = pend
                (s2b, s2h) = rot_s2.next()
                sp2, sk2 = ps_slot(s2b, s2h)
                P.op("pe", lambda e, sp2=sp2, i=i: e.matmul(sp2, onesBD, sqb[i], start=True, stop=True), ["sqb%d" % i, "cbf"], [sk2])
                P.op("act", lambda e, sp2=sp2, i=i: e.activation(out=r2[i], in_=sp2, func=AF.Ln, bias=epsT, scale=1.0 / 64), [sk2, "eps"], ["r2%d" % i])
                P.op("act", lambda e, i=i: e.activation(out=ri2[i], in_=r2[i], func=AF.Exp, scale=-0.5), ["r2%d" % i], ["ri2%d" % i])
                gtab = qg if which == "q" else kg
                P.op("dve", lambda e, pp=pp, i=i, c=c, gtab=gtab: e.scalar_tensor_tensor(out=qs[i], in0=pp, scalar=gtab[:, c:c + 1], in1=ri2[i], op0=ALU.mult, op1=ALU.mult),
                     [pk, "ri2%d" % i, "qg", "kg"], ["qs%d" % i])
                dst = (QTd if which == "q" else KTd)[c, :, tok0:tok0 + BLK]
                P.dma("sp", dst, qs[i], ["qs%d" % i], ["%s_%d" % (which.upper(), b)], "qs%d" % i)

            pend = None
            for (which, c) in qk_list:
                col0 = (256 if which == "q" else 768) + c * 128
                pp, pk = fchunk(col0)
                i = cnt2[0] % 2
                cnt2[0] += 1
                P.op("act", lambda e, pp=pp, i=i: e.activation(out=sqb[i], in_=pp, func=AF.Square), [pk], ["sqb%d" % i])
                if pend is not None:
                    qk_finish(pend)
                pend = (which, c, pp, pk, i)
            qk_finish(pend)
            if mode == "full":
                for c in range(2):
                    pp, pk = fchunk(1792 + c * 128)
                    i = c
                    P.op("act", lambda e, pp=pp, i=i: e.activation(out=us[i], in_=pp, func=AF.Gelu_apprx_tanh), [pk], ["us%d" % i])
                    P.dma("sp", Ud[c, :, tok0:tok0 + BLK], us[i], ["us%d" % i], ["U_%d" % b], "us%d" % i)
            for th in range(2):
                vb = rot_v.next()
                pv, pvk = ps_slot(vb)
                for kc in range(KC):
                    P.op("pe", lambda e, kc=kc, pv=pv, th=th, hn=hn: e.matmul(pv, hn[:, kc, th * 128:(th + 1) * 128], Win[:, kc, 1280:1792], start=(kc == 0), stop=(kc == KC - 1)),
                         ["Win", "hn%d_%d" % (hb, kc)], [pvk])
                i = th
                if th == 0:
                    P.op("act", lambda e, pv=pv, i=i: e.activation(out=vun[i], in_=pv, func=AF.Copy), [pvk], ["vun%d" % i])
                else:
                    P.op("dve", lambda e, pv=pv, i=i: e.tensor_copy(out=vun[i], in_=pv), [pvk], ["vun%d" % i])
                P.dma("sp", Vd[tok0 + th * 128:tok0 + (th + 1) * 128, :], vun[i], ["vun%d" % i], ["V_%d" % b], "vun%d" % i)
            if mode == "full":
                for th in range(2):
                    (gb_, gh_) = rot_s2.next()
                    pg, pgk = ps_slot(gb_, gh_)
                    for kc in range(KC):
                        P.op("pe", lambda e, kc=kc, pg=pg, th=th, hn=hn: e.matmul(pg, hn[:, kc, th * 128:(th + 1) * 128], Win[:, kc, 2048:2304], start=(kc == 0), stop=(kc == KC - 1)),
                             ["Win", "hn%d_%d" % (hb, kc)], [pgk])
                    P.op("act", lambda e, pg=pg, th=th: e.activation(out=ggs[th], in_=pg, func=AF.Gelu_apprx_tanh), [pgk], ["gg%d" % th])
                for th in range(2):
                    gg_ = ggs[th]
                    ggk = "gg%d" % th
                    P.op("pool", lambda e, gg_=gg_: e.tensor_tensor(out=gsq, in0=gg_, in1=gg_, op=ALU.mult), [ggk], ["gsq"])
                    P.op("dve", lambda e: e.tensor_reduce(out=ss4, in_=gsq.rearrange("p (a b) -> p a b", b=64), axis=AX.X, op=ALU.add), ["gsq"], ["ss4"])
                    P.op("act", lambda e: e.activation(out=ss4, in_=ss4, func=AF.Ln, bias=epsT, scale=1.0 / 64), ["ss4", "eps"], ["ss4"])
                    P.op("act", lambda e: e.activation(out=ss4, in_=ss4, func=AF.Exp, scale=-0.5), ["ss4"], ["ss4"])
                    P.op("dve", lambda e, gg_=gg_: e.tensor_tensor(out=gsq.rearrange("p (a b) -> p a b", b=64), in0=gg_.rearrange("p (a b) -> p a b", b=64),
                                                         in1=ss4.unsqueeze(2).to_broadcast([128, 4, 64]), op=ALU.mult), [ggk, "ss4"], ["gsq"])
                    i = th
                    for s in range(2):
                        srcg = gsq.rearrange("p (j s d) -> p j s d", s=2, d=64)[:, :, s, :]
                        gng = sgG.rearrange("p (j s d) -> p j s d", s=2, d=64)[:, :, s, :]
                        dstg = vspad[i].rearrange("p (j s) c -> p j s c", s=2)[:, :, s, 64 * s:64 * s + 64]
                        P.op("dve", lambda e, srcg=srcg, gng=gng, dstg=dstg: e.tensor_tensor(out=dstg, in0=srcg, in1=gng, op=ALU.mult), ["gsq", "sgG"], ["vspad%d" % i])
                    P.dma("sp", VSd[tok0 + th * 128:tok0 + (th + 1) * 128, :], vspad[i].rearrange("p h c -> p (h c)"), ["vspad%d" % i], ["VS_%d" % b], "vspad%d" % i)
        P.fence()
        A.release()
        if stop == "A1_%d" % l:
            done[0] = True
            break

        A.mark()
        PW = A.alloc([128, 2, 128], BF16)
        P.dma("pool", PW, pw_bd[l], [], ["PW"], "PW")
        SGW = A.alloc([128, 4, 128], BF16)
        P.dma("pool", SGW, sgwT[l], [], ["SGW"], "SGW")
        KTc = A.alloc([128, 4, BLK], BF16)
        Vc = A.alloc([128, 2, 512], BF16)
        P.dma("sp", KTc, KTd[:, :, CTXB * BLK:(CTXB + 1) * BLK].rearrange("c p t -> p c t"), ["K_%d" % CTXB], ["KTc"], "KTc")
        P.dma("sp", Vc, Vd[CTXB * BLK:(CTXB + 1) * BLK, :].rearrange("(c p) n -> p c n", p=128), ["V_%d" % CTXB], ["Vc"], "Vc")
        QTz = A.alloc([128, 4, 2 * BLK], BF16)
        P.op("pool", lambda e: e.memset(QTz, 0.0), [], ["QTz"])
        KTw = A.alloc([128, 4, 3 * BLK], BF16)
        Vw = A.alloc([128, 6, 512], BF16)
        PIw = A.alloc([128, 2, BLK + 16], F32)
        Ub = A.alloc([128, 2, BLK], BF16)
        VSp = A.alloc([128, 2, 4 * 128], BF16)
        mix = A.alloc([128, KC, BLK], BF16)
        ym = A.alloc([128, BLK], F32)
        ymb = A.alloc([128, BLK], BF16)
        tS = [A.alloc([128, 2 * BLK], F32) for _ in range(4)]
        eS = [A.alloc([128, 2 * BLK], BF16) for _ in range(4)]
        pT = [A.alloc([128, 2 * BLK], BF16) for _ in range(8)]
        rden = A.alloc([128, 2 * BLK], F32)
        rdi = A.alloc([128, 2 * BLK], F32)
        sl = [tS[0][:, 0:BLK + 16], tS[1][:, 0:BLK + 16], rden[:, 0:BLK + 16], rdi[:, 0:BLK + 16]]
        slk = ["tS0", "tS1", "rden", "rdi"]
        sgt = A.alloc([128, 128], F32)
        hn2b = A.alloc([128, KC, BLK], BF16)
        hn2f = [A.alloc([128, BLK], F32) for _ in range(2)]
        sq2 = [A.alloc([128, BLK], BF16) for _ in range(2)]
        tmp2 = [A.alloc([128, BLK], F32) for _ in range(2)]
        rr2 = A.alloc([128, BLK], F32)
        rstd2 = A.alloc([128, BLK], F32)
        lg = A.alloc([128, 2, NE], F32)
        ex = A.alloc([128, 2, NE], F32)
        mx = A.alloc([128, 8, 2], F32)
        sc = A.alloc([128, 2, NE], F32)
        sc2 = A.alloc([128, 2, NE], F32)
        gs = A.alloc([128, 8], F32)
        gm = A.alloc([128, 8], F32)
        msk = A.alloc([128, 2, NE], F32)
        gts_bufs = [A.alloc([128, 2, NE], F32) for _ in range(2)]
        rctr = [0]
        pending_tail = []

        rot_st = Rot([(0, 0), (1, 0), (4, 0), (5, 0)])
        rot_o = Rot([(2, 0), (6, 0)])
        rot_d = Rot([(3, 0), (7, 0)])
        rot_w = Rot([(4, 0), (5, 0)])
        rot_m = Rot([(6, 0)])
        rot_r = Rot([(7, 0)])
        ctr = [0]

        a2_blocks = ([CTXB] + list(range(1, 11))) if l == 0 else list(range(2, 10))

        def load_att(b):
            tok0 = b * BLK
            P.dma("sp", QTz[0:64, :, 0:BLK], QTd[:, 0:64, tok0:tok0 + BLK].rearrange("c p t -> p c t"), ["Q_%d" % b], ["QTz"], "QTz")
            P.dma("sp", QTz[64:128, :, BLK:2 * BLK], QTd[:, 64:128, tok0:tok0 + BLK].rearrange("c p t -> p c t"), ["Q_%d" % b], ["QTz"], "QTz")
            if b != CTXB:
                P.dma("sp", KTw, KTd[:, :, tok0 - BLK:tok0 + 2 * BLK].rearrange("c p t -> p c t"), ["K_%d" % (b - 1), "K_%d" % b, "K_%d" % (b + 1)], ["KTw"], "KTw")
                P.dma("sp", Vw, Vd[tok0 - BLK:tok0 + 2 * BLK, :].rearrange("(c p) n -> p c n", p=128), ["V_%d" % (b - 1), "V_%d" % b, "V_%d" % (b + 1)], ["Vw"], "Vw")

        def load_pool(b):
            if b != CTXB:
                P.dma("sp", PIw, PId[:, :, b * BLK:b * BLK + BLK + 16].rearrange("c p t -> p c t"), ["PI_%d" % (b - 1), "PI_%d" % b, "PI_%d" % (b + 1), "PIpad"], ["PIw"], "PIw")
            else:
                P.dma("sp", PIw, PId[:, :, PI_CTX - 8:PI_CTX + BLK + 8].rearrange("c p t -> p c t"), ["PI_%d" % b, "PIpad"], ["PIw"], "PIw")

        def load_sg(b):
            tok0 = b * BLK
            P.dma("sp", Ub, Ud[:, :, tok0:tok0 + BLK].rearrange("c p t -> p c t"), ["U_%d" % b], ["Ub"], "Ub")
            P.dma("sp", VSp, VSd[tok0:tok0 + BLK, :].rearrange("(c p) n -> p c n", p=128), ["VS_%d" % b], ["VSp"], "VSp")

        for b in a2_blocks:
            isctx = b == CTXB
            w = 1 if isctx else 0
            tok0 = b * BLK
            s_ = hslot(b)
            if b == a2_blocks[0]:
                load_att(b)
                load_pool(b)
                load_sg(b)

            rcslot = 3 if isctx else (1 if b == 2 else (2 if b == 9 else 0))
            for c in range(2):
                x_ = PIw[:, c, :]
                P.op("pool", lambda e, x_=x_: e.tensor_tensor(out=sl[0][:, 1:272], in0=x_[:, 0:271], in1=x_[:, 1:272], op=ALU.add), ["PIw"], [slk[0]])
                P.op("pool", lambda e: e.tensor_tensor(out=sl[1][:, 2:271], in0=sl[0][:, 1:270], in1=sl[0][:, 3:272], op=ALU.add), [slk[0]], [slk[1]])
                lev = [0, 1]
                if c == 1:
                    P.op("pool", lambda e: e.tensor_tensor(out=sl[2][:, 4:269], in0=sl[1][:, 2:267], in1=sl[1][:, 6:271], op=ALU.add), [slk[1]], [slk[2]])
                    P.op("pool", lambda e: e.tensor_tensor(out=sl[3][:, 8:265], in0=sl[2][:, 4:261], in1=sl[2][:, 12:269], op=ALU.add), [slk[2]], [slk[3]])
                    lev = [2, 3]
                for hh in range(2):
                    pr = slice(64 * hh, 64 * hh + 64)
                    lv = lev[hh]
                    P.op("pool", lambda e, pr=pr, lv=lv, c=c: e.tensor_tensor(out=ym[pr, :], in0=sl[lv][pr, 8:264], in1=RCW[pr, c:c + 1].to_broadcast([64, BLK]), op=ALU.mult),
                         [slk[lv], "RC"], ["ym"])
                    if rcslot in (1, 3):
                        P.op("pool", lambda e, pr=pr, lv=lv, c=c, rcslot=rcslot: e.tensor_tensor(out=ym[pr, 0:8], in0=sl[lv][pr, 8:16], in1=RCB[pr, rcslot - 1, c, 0:8], op=ALU.mult),
                             [slk[lv], "RC", "ym"], ["ym"])
                    if rcslot in (2, 3):
                        P.op("pool", lambda e, pr=pr, lv=lv, c=c, rcslot=rcslot: e.tensor_tensor(out=ym[pr, 248:256], in0=sl[lv][pr, 256:264], in1=RCB[pr, rcslot - 1, c, 8:16], op=ALU.mult),
                             [slk[lv], "RC", "ym"], ["ym"])
                P.op("pool", lambda e, x_=x_: e.tensor_tensor(out=ymb, in0=ym, in1=x_[:, 8:264], op=ALU.subtract), ["ym", "PIw"], ["ymb"])
                (mb, mh) = rot_m.next()
                pm, pmk = ps_slot(mb, mh)
                P.op("pe", lambda e, pm=pm, c=c: e.matmul(pm, PW[:, c, :], ymb, start=True, stop=True), ["PW", "ymb"], [pmk])
                P.op("act", lambda e, pm=pm, c=c: e.activation(out=mix[:, c, :], in_=pm, func=AF.Copy, scale=pscale[:, c:c + 1]), [pmk, "pscale"], ["mix%d" % c])

            nxt_ = a2_blocks[a2_blocks.index(b) + 1] if a2_blocks.index(b) + 1 < len(a2_blocks) else None
            if nxt_ is not None:
                load_pool(nxt_)
            for th in range(2):
                for j in range(2):
                    (mb, mh) = rot_m.next()
                    pm, pmk = ps_slot(mb, mh)
                    pmv = pm[:, 0:128]
                    for s in range(2):
                        hh = 2 * j + s
                        P.op("pe", lambda e, pmv=pmv, th=th, hh=hh, s=s: e.matmul(pmv, VSp[:, th, hh * 128:(hh + 1) * 128], SGW[:, hh, :], start=(s == 0), stop=(s == 1)),
                             ["VSp", "SGW"], [pmk])
                    P.op("dve", lambda e, pmv=pmv, j=j: e.tensor_tensor(out=sgt, in0=pmv, in1=sgB[:, j, :], op=ALU.add), [pmk, "sgB"], ["sgt"])
                    P.op("dve", lambda e, th=th, j=j: e.tensor_tensor(out=mix[:, 6 + j, th * 128:(th + 1) * 128], in0=sgt, in1=Ub[:, j, th * 128:(th + 1) * 128], op=ALU.mult),
                         ["sgt", "Ub"], ["mix%d" % (6 + j)])

            if nxt_ is not None:
                load_sg(nxt_)
            chunks = ([] if isctx else [("l", c) for c in range(6)]) + [("c", c) for c in range(2)]
            nch = len(chunks)
            items = []
            for j in range(4):
                for ic, (kind, c) in enumerate(chunks):
                    items.append((j, kind, c, ic))
            DEPTH = 6
            pair_ps = {}
            item_buf = {}

            def att_stage1(idx):
                (j, kind, c, ic) = items[idx]
                (sb2, sh2) = rot_st.next()
                pst, pstk = ps_slot(sb2)
                ii = ctr[0] % 4
                i4 = ctr[0] % 8
                ctr[0] += 1
                item_buf[idx] = i4
                if kind == "l":
                    P.op("pe", lambda e, pst=pst, j=j, c=c: e.matmul(pst, KTw[:, j, c * 128:(c + 1) * 128], QTz[:, j, :], start=True, stop=True),
                         ["KTw", "QTz"], [pstk])
                    P.op("dve", lambda e, pst=pst, ii=ii, j=j, c=c: e.tensor_tensor(out=tS[ii].rearrange("p (h q) -> p h q", h=2), in0=pst.rearrange("p (h q) -> p h q", h=2),
                                                                               in1=BT[:, 2 * j:2 * j + 2, (10 - 2 * c) * 64:(14 - 2 * c) * 64], op=ALU.add),
                         [pstk, "BT"], ["tS%d" % ii])
                    P.op("act", lambda e, ii=ii: e.activation(out=eS[ii], in_=tS[ii], func=AF.Exp), ["tS%d" % ii], ["eS%d" % ii])
                    P.op("pool", lambda e, ii=ii, i4=i4, c=c, b=b: e.tensor_tensor(out=pT[i4].rearrange("p (h a q) -> p h a q", h=2, q=64), in0=eS[ii].rearrange("p (h a q) -> p h a q", h=2, q=64),
                                                                            in1=RM[:, b - 1, c, :].unsqueeze(1).unsqueeze(3).to_broadcast([128, 2, 4, 64]), op=ALU.mult),
                         ["eS%d" % ii, "RM"], ["pT%d" % i4])
                else:
                    P.op("pe", lambda e, pst=pst, j=j, c=c: e.matmul(pst, KTc[:, j, c * 128:(c + 1) * 128], QTz[:, j, :], start=True, stop=True),
                         ["KTc", "QTz"], [pstk])
                    P.op("act", lambda e, pst=pst, i4=i4: e.activation(out=pT[i4], in_=pst, func=AF.Exp), [pstk], ["pT%d" % i4])

            def att_stage2(idx):
                (j, kind, c, ic) = items[idx]
                i4 = item_buf[idx]
                if ic == 0:
                    pair_ps[j] = (ps_slot(rot_o.next()[0]), ps_slot(rot_d.next()[0]))
                (po, pok), (pd, pdk) = pair_ps[j]
                if kind == "l":
                    vl, vk = Vw[:, c, j * 128:(j + 1) * 128], "Vw"
                else:
                    vl, vk = Vc[:, c, j * 128:(j + 1) * 128], "Vc"
                P.op("pe", lambda e, po=po, vl=vl, i4=i4, ic=ic, nch=nch: e.matmul(po, vl, pT[i4], start=(ic == 0), stop=(ic == nch - 1)), [vk, "pT%d" % i4], [pok])
                P.op("pe", lambda e, pd=pd, i4=i4, ic=ic, nch=nch: e.matmul(pd, onesA, pT[i4], start=(ic == 0), stop=(ic == nch - 1)), ["cbf", "pT%d" % i4], [pdk])
                if ic == nch - 1:
                    P.op("act", lambda e, pd=pd: e.activation(out=rden, in_=pd, func=AF.Ln), [pdk], ["rden"])
                    P.op("act", lambda e: e.activation(out=rdi, in_=rden, func=AF.Exp, scale=-1.0), ["rden"], ["rdi"])
                    for s in range(2):
                        pr = slice(64 * s, 64 * s + 64)
                        P.op("dve", lambda e, po=po, j=j, pr=pr, s=s: e.tensor_tensor(out=mix[pr, 2 + j, :], in0=po[pr, s * 256:(s + 1) * 256], in1=rdi[pr, s * 256:(s + 1) * 256], op=ALU.mult),
                             [pok, "rdi"], ["mix%d" % (2 + j)])

            for idx in range(len(items) + DEPTH):
                if idx < len(items):
                    att_stage1(idx)
                if idx - DEPTH >= 0:
                    att_stage2(idx - DEPTH)
                if idx == 3:
                    while pending_tail:
                        pending_tail.pop(0)()
            nxt = a2_blocks[a2_blocks.index(b) + 1] if a2_blocks.index(b) + 1 < len(a2_blocks) else None
            if nxt is not None:
                load_att(nxt)

            for dc in range(KC):
                (wb_, wh_) = rot_w.next()
                pw_, pwk = ps_slot(wb_, wh_)
                for m in range(KC):
                    P.op("pe", lambda e, pw_=pw_, m=m, dc=dc: e.matmul(pw_, Wout[:, m, dc * 128:(dc + 1) * 128], mix[:, m, :], start=(m == 0), stop=(m == KC - 1)),
                         ["Wout", "mix%d" % m], [pwk])
                hv = H[:, dc, s_ * BLK:(s_ + 1) * BLK]
                P.op("dve", lambda e, pw_=pw_, hv=hv, dc=dc, w=w: e.scalar_tensor_tensor(out=hv, in0=pw_, scalar=G1(dc, w), in1=hv, op0=ALU.mult, op1=ALU.add),
                     [pwk, "adaS", "H%d_%d" % (s_, dc)], ["H%d_%d" % (s_, dc)])

            (rb_, rh_) = rot_r.next()
            ssp, ssk = ps_slot(rb_, rh_)
            for kc in range(KC):
                i = kc % 2
                hv = H[:, kc, s_ * BLK:(s_ + 1) * BLK]
                if kc % 2 == 0:
                    P.op("act", lambda e, hv=hv, i=i: e.activation(out=sq2[i], in_=hv, func=AF.Square), ["H%d_%d" % (s_, kc)], ["sq2%d" % i])
                else:
                    P.op("dve", lambda e, hv=hv, i=i: e.tensor_tensor(out=sq2[i], in0=hv, in1=hv, op=ALU.mult), ["H%d_%d" % (s_, kc)], ["sq2%d" % i])
                P.op("pe", lambda e, ssp=ssp, i=i, kc=kc: e.matmul(ssp, onesA, sq2[i], start=(kc == 0), stop=(kc == KC - 1)), ["sq2%d" % i, "cbf"], [ssk])
            P.op("act", lambda e, ssp=ssp: e.activation(out=rr2, in_=ssp, func=AF.Ln, bias=epsT, scale=1.0 / D), [ssk, "eps"], ["rr2"])
            P.op("act", lambda e: e.activation(out=rstd2, in_=rr2, func=AF.Exp, scale=-0.5), ["rr2"], ["rstd2"])
            plgs = [ps_slot(*rot_r.next()), ps_slot(*rot_m.next())]
            for kc in range(KC):
                i = kc % 2
                hv = H[:, kc, s_ * BLK:(s_ + 1) * BLK]
                if kc % 2 == 0:
                    P.op("dve", lambda e, hv=hv, i=i: e.tensor_tensor(out=tmp2[i], in0=hv, in1=rstd2, op=ALU.mult), ["H%d_%d" % (s_, kc), "rstd2"], ["tmp2%d" % i])
                else:
                    P.op("pool", lambda e, hv=hv, i=i: e.tensor_tensor(out=tmp2[i], in0=hv, in1=rstd2, op=ALU.mult), ["H%d_%d" % (s_, kc), "rstd2"], ["tmp2%d" % i])
                P.op("dve", lambda e, kc=kc, i=i, w=w: e.tensor_scalar(out=hn2f[i], in0=tmp2[i], scalar1=A2t[:, kc, w:w + 1], scalar2=B2(kc, w), op0=ALU.mult, op1=ALU.add),
                     ["tmp2%d" % i, "adaS", "A2t"], ["hn2f%d" % i])
                P.op("act", lambda e, kc=kc, i=i, w=w: e.activation(out=hn2b[:, kc, :], in_=tmp2[i], func=AF.Identity, bias=B2(kc, w), scale=A2t[:, kc, w:w + 1]),
                     ["tmp2%d" % i, "adaS", "A2t"], ["hn2b"])
                for th in range(2):
                    P.op("pe", lambda e, plg=plgs[th][0], i=i, kc=kc, th=th: e.matmul(plg[:, 0:NE], hn2f[i][:, th * 128:(th + 1) * 128], Wr[:, kc, :], start=(kc == 0), stop=(kc == KC - 1)),
                         ["hn2f%d" % i, "Wr"], [plgs[th][1]])
            P.dma("sp", HN2d[:, :, s_ * BLK:(s_ + 1) * BLK].rearrange("c p t -> p c t"), hn2b, ["hn2b"], ["HN2_%d" % s_], "hn2b")
            gp = gts_bufs[rctr[0] % 2]
            gpk = "gts%d" % (rctr[0] % 2)
            rctr[0] += 1
            for th in range(2):
                lgp = plgs[th][0][:, 0:NE]
                P.op("dve", lambda e, lgp=lgp, th=th: e.tensor_tensor(out=lg[:, th, :], in0=lgp, in1=brbc, op=ALU.add), [plgs[th][1], "brbc"], ["lg"])
            def bc2(a):
                return a.unsqueeze(2).to_broadcast([128, 2, NE])
            def bc8(a):
                return a.unsqueeze(2).to_broadcast([128, 8, 4])
            sc3 = sc.rearrange("p t (g k) -> p (t g) k", k=4)
            sc23 = sc2.rearrange("p t (g k) -> p (t g) k", k=4)
            msk3 = msk.rearrange("p t (g k) -> p (t g) k", k=4)
            P.op("dve", lambda e: e.tensor_reduce(out=mx[:, 0, :], in_=lg, axis=AX.X, op=ALU.max), ["lg"], ["mx"])
            P.op("dve", lambda e: e.tensor_tensor(out=lg, in0=lg, in1=bc2(mx[:, 0, :]), op=ALU.subtract), ["lg", "mx"], ["lg"])
            P.op("act", lambda e: e.activation(out=ex, in_=lg, func=AF.Exp), ["lg"], ["ex"])
            P.op("dve", lambda e: e.tensor_reduce(out=mx[:, 1, :], in_=ex, axis=AX.X, op=ALU.add), ["ex"], ["mx"])
            P.op("dve", lambda e: e.reciprocal(out=mx[:, 2, :], in_=mx[:, 1, :]), ["mx"], ["mx"])
            P.op("dve", lambda e: e.tensor_tensor(out=sc, in0=ex, in1=bc2(mx[:, 2, :]), op=ALU.mult), ["ex", "mx"], ["sc"])
            P.op("dve", lambda e: e.tensor_reduce(out=gs, in_=sc3, axis=AX.X, op=ALU.max), ["sc"], ["gs"])
            P.op("dve", lambda e: e.tensor_tensor(out=sc23, in0=sc3, in1=bc8(gs), op=ALU.is_equal), ["sc", "gs"], ["sc2"])
            P.op("dve", lambda e: e.scalar_tensor_tensor(out=sc2, in0=sc2, scalar=-4.0, in1=sc, op0=ALU.mult, op1=ALU.add), ["sc2", "sc"], ["sc2"])
            P.op("dve", lambda e: e.tensor_reduce(out=gm, in_=sc23, axis=AX.X, op=ALU.max), ["sc2"], ["gm"])
            P.op("dve", lambda e: e.tensor_tensor(out=gs, in0=gs, in1=gm, op=ALU.add), ["gs", "gm"], ["gs"])
            P.op("dve", lambda e: e.tensor_reduce(out=mx[:, 3, :], in_=gs.rearrange("p (t g) -> p t g", g=4), axis=AX.X, op=ALU.max), ["gs"], ["mx"])
            P.op("dve", lambda e: e.tensor_tensor(out=gm.rearrange("p (t g) -> p t g", g=4), in0=gs.rearrange("p (t g) -> p t g", g=4),
                                                 in1=mx[:, 3, :].unsqueeze(2).to_broadcast([128, 2, 4]), op=ALU.is_equal), ["gs", "mx"], ["gm"])
            P.op("dve", lambda e: e.scalar_tensor_tensor(out=msk3, in0=sc3, scalar=1.0, in1=bc8(gm), op0=ALU.add, op1=ALU.mult), ["sc", "gm"], ["msk"])
            P.op("dve", lambda e: e.tensor_scalar(out=msk, in0=msk, scalar1=-1.0, scalar2=None, op0=ALU.add), ["msk"], ["msk"])
            P.op("dve", lambda e: e.tensor_reduce(out=mx[:, 4, :], in_=msk, axis=AX.X, op=ALU.max), ["msk"], ["mx"])
            P.op("dve", lambda e: e.tensor_tensor(out=sc2, in0=msk, in1=bc2(mx[:, 4, :]), op=ALU.is_equal), ["msk", "mx"], ["sc2"])
            P.op("dve", lambda e: e.scalar_tensor_tensor(out=sc2, in0=sc2, scalar=-4.0, in1=msk, op0=ALU.mult, op1=ALU.add), ["sc2", "msk"], ["sc2"])
            P.op("dve", lambda e: e.tensor_reduce(out=mx[:, 5, :], in_=sc2, axis=AX.X, op=ALU.max), ["sc2"], ["mx"])
            P.op("dve", lambda e: e.tensor_tensor(out=sc2, in0=msk, in1=bc2(mx[:, 5, :]), op=ALU.is_ge), ["msk", "mx"], ["sc2"])
            P.op("dve", lambda e: e.tensor_tensor(out=mx[:, 6, :], in0=mx[:, 4, :], in1=mx[:, 5, :], op=ALU.add), ["mx"], ["mx"])
            P.op("dve", lambda e: e.reciprocal(out=mx[:, 7, :], in_=mx[:, 6, :]), ["mx"], ["mx"])
            P.op("dve", lambda e: e.tensor_tensor(out=sc2, in0=sc2, in1=msk, op=ALU.mult), ["sc2", "msk"], ["sc2"])
            P.op("dve", lambda e, gp=gp: e.tensor_tensor(out=gp, in0=sc2, in1=bc2(mx[:, 7, :]), op=ALU.mult), ["sc2", "mx"], [gpk])

            def gate_tail(gp=gp, gpk=gpk, s_=s_):
                for th in range(2):
                    (tb_, thh_) = rot_w.next()
                    ptg, ptgk = ps_slot(tb_, thh_)
                    P.op("pe", lambda e, ptg=ptg, gp=gp, th=th: e.matmul(ptg[0:NE, 0:128], gp[:, th, :], identF, start=True, stop=True), [gpk, "CONST"], [ptgk])
                    t0 = s_ * BLK + th * 128
                    P.op("act", lambda e, ptg=ptg, t0=t0: e.activation(out=GT[:, t0:t0 + 128], in_=ptg[0:NE, 0:128], func=AF.Copy), [ptgk], ["GT"])
            pending_tail.append(gate_tail)
        while pending_tail:
            pending_tail.pop(0)()
        P.fence()
        A.release()
        A.hi = hi_save
        if stop == "A2_%d" % l:
            done[0] = True
            break

        A.mark()
        SEL = A.alloc([NE, NE, 128], BF16)
        P.dma("pool", SEL, sel_in, [], ["SEL"], "SEL")
        ring = [A.alloc([128, 4096], BF16) for _ in range(5)]
        ringi = [0]
        he = [A.alloc([128, 4, 512], BF16) for _ in range(2)]
        hn2t = [A.alloc([128, KC, 512], BF16) for _ in range(2)]
        sgs = [A.alloc([128, 512], F32) for _ in range(2)]
        tts = [A.alloc([128, 512], F32) for _ in range(2)]
        if l == 0:
            tiles = [(i * 512, 512, 0) for i in range(5)] + [(2560, 256, 1)]
        else:
            tiles = [(256 + i * 512, 512, 0) for i in range(4)]
        rot_g = Rot([0, 1])
        rot_u = Rot([2, 3])
        rot_y = Rot([4, 5])
        rot_gb = Rot([6, 7])
        uctr = 0

        def ring_next():
            i = ringi[0] % 5
            ringi[0] += 1
            return ring[i], "ring%d" % i

        units = [(ex_, t) for ex_ in range(b_experts) for t in tiles]
        wcur = {}
        hide_ada = (l == 0 and b_experts >= 9 and stop is None and HIDE_ADA)
        if hide_ada:
            wa1 = A.alloc([128, KC, 768], BF16)
            ps7, ps7k = ps_slot(7)

        def ada_piece_dma(pc):
            P.dma("pool", wa1, w_ada[1, :, pc * 768:(pc + 1) * 768].rearrange("(kc p) n -> p kc n", p=128), [], ["wa1"], "wa1")

        def ada_piece_mm(pc):
            for jj in range(6):
                j = pc * 6 + jj
                for kc in range(KC):
                    P.op("pe", lambda e, jj=jj, kc=kc, j=j: e.matmul(ps7[:, 2 * j:2 * j + 2], wa1[:, kc, jj * 128:(jj + 1) * 128], condT[:, kc, :], start=(kc == 0), stop=(kc == KC - 1)),
                         ["wa1", "condT"], [ps7k])
            P.op("dve", lambda e, pc=pc: e.tensor_copy(out=adaraw[:, 12 * pc:12 * pc + 12], in_=ps7[:, 12 * pc:12 * pc + 12]), [ps7k], ["adaraw"])
            if pc == 7:
                ada1_done[0] = True

        def moe_gu(u):
            (ex_, (t0, n, w)) = units[u]
            if ex_ not in wcur:
                wg_t, wgk = ring_next()
                wg_ = wg_t.rearrange("p (k f) -> p k f", f=FE)
                P.dma("pool", wg_, w_gate[l, ex_].rearrange("(kc p) f -> p kc f", p=128), [], [wgk], wgk)
                wu_t, wuk = ring_next()
                wu_ = wu_t.rearrange("p (k f) -> p k f", f=FE)
                P.dma("pool", wu_, w_up[l, ex_].rearrange("(kc p) f -> p kc f", p=128), [], [wuk], wuk)
                wd_t, wdk = ring_next()
                wd_ = wd_t.rearrange("p (k f) -> p k f", f=D)
                P.dma("pool", wd_, w_down[l, ex_].rearrange("(fc p) d -> p fc d", p=128), [], [wdk], wdk)
                wcur[ex_] = (wg_, wgk, wu_, wuk, wd_, wdk)
            (wg_, wgk, wu_, wuk, wd_, wdk) = wcur[ex_]
            ui = u % 2
            hk = "hn2t%d" % ui
            slots_ = sorted(set([t0 // BLK, (t0 + n - 1) // BLK]))
            P.dma("sp", hn2t[ui][:, :, 0:n], HN2d[:, :, t0:t0 + n].rearrange("c p t -> p c t"), ["HN2_%d" % s for s in slots_], [hk], hk)
            gbb = rot_gb.next()
            pgb, pgbk = ps_slot(gbb)
            P.op("pe", lambda e, pgb=pgb, ex_=ex_, t0=t0, n=n: e.matmul(pgb[:, 0:n], SEL[:, ex_, :], GT[:, t0:t0 + n], start=True, stop=True), ["SEL", "GT"], [pgbk])
            for fc in range(4):
                gbk_ = rot_g.next()
                pgg, pggk = ps_slot(gbk_)
                ubk_ = rot_u.next()
                puu, puuk = ps_slot(ubk_)
                for kc in range(KC):
                    P.op("pe", lambda e, pgg=pgg, kc=kc, fc=fc, ui=ui, n=n, wg_=wg_: e.matmul(pgg[:, 0:n], wg_[:, kc, fc * 128:(fc + 1) * 128], hn2t[ui][:, kc, 0:n], start=(kc == 0), stop=(kc == KC - 1)),
                         [wgk, hk], [pggk])
                for kc in range(KC):
                    P.op("pe", lambda e, puu=puu, kc=kc, fc=fc, ui=ui, n=n, wu_=wu_: e.matmul(puu[:, 0:n], wu_[:, kc, fc * 128:(fc + 1) * 128], hn2t[ui][:, kc, 0:n], start=(kc == 0), stop=(kc == KC - 1)),
                         [wuk, hk], [puuk])
                i = fc % 2
                P.op("act", lambda e, pgg=pgg, i=i, n=n: e.activation(out=sgs[i][:, 0:n], in_=pgg[:, 0:n], func=AF.Silu), [pggk], ["sgs%d" % i])
                P.op("dve", lambda e, puu=puu, i=i, n=n: e.tensor_tensor(out=tts[i][:, 0:n], in0=puu[:, 0:n], in1=sgs[i][:, 0:n], op=ALU.mult), [puuk, "sgs%d" % i], ["tts%d" % i])
                P.op("dve", lambda e, pgb=pgb, i=i, n=n, ui=ui, fc=fc: e.tensor_tensor(out=he[ui][:, fc, 0:n], in0=pgb[:, 0:n], in1=tts[i][:, 0:n], op=ALU.mult),
                     [pgbk, "tts%d" % i], ["he%d_%d" % (ui, fc)])

        def moe_dn(u):
            (ex_, (t0, n, w)) = units[u]
            (wg_, wgk, wu_, wuk, wd_, wdk) = wcur[ex_]
            ui = u % 2
            slots_ = sorted(set([t0 // BLK, (t0 + n - 1) // BLK]))
            for dc in range(KC):
                ybk = rot_y.next()
                py, pyk = ps_slot(ybk)
                for fc in range(4):
                    P.op("pe", lambda e, py=py, fc=fc, dc=dc, ui=ui, n=n, wd_=wd_: e.matmul(py[:, 0:n], wd_[:, fc, dc * 128:(dc + 1) * 128], he[ui][:, fc, 0:n], start=(fc == 0), stop=(fc == 3)),
                         [wdk, "he%d_%d" % (ui, fc)], [pyk])
                hv = H[:, dc, t0:t0 + n]
                hkeys_ = ["H%d_%d" % (s, dc) for s in slots_]
                P.op("dve", lambda e, py=py, hv=hv, dc=dc, w=w, n=n: e.scalar_tensor_tensor(out=hv, in0=py[:, 0:n], scalar=G2(dc, w), in1=hv, op0=ALU.mult, op1=ALU.add),
                     [pyk, "adaS"] + hkeys_, hkeys_)

        TPE = len(tiles)
        for u in range(len(units)):
            moe_gu(u)
            if hide_ada and 1 <= units[u][0] <= 8:
                if u % TPE == 0:
                    ada_piece_dma(units[u][0] - 1)
                if u % TPE == TPE - 1:
                    ada_piece_mm(units[u][0] - 1)
            if u >= 1:
                moe_dn(u - 1)
        moe_dn(len(units) - 1)
        P.fence()
        A.release()
        if stop == "B_%d" % l:
            done[0] = True
            break

    if debug:
        for kc in range(KC):
            P.dma("sp", dbgH[kc], H[:, kc, :], ["H%d_%d" % (s, kc) for s in range(11)], ["dbgH"], "dbgH")
        P.dma("sp", dbgGT, GT, ["GT"], ["dbgGT"], "dbgGT")
    for kc in range(KC):
        P.dma("sp", yT[kc], H[:, kc, BLK:9 * BLK], ["H%d_%d" % (s, kc) for s in range(1, 9)], ["yout"], "yout")
    P.fence()
    P.emit()
    es.close()
    return nc


def _consts():
    c = np.zeros((128, 5 * 128), np.float32)
    c[:, 0:128] = np.eye(128, dtype=np.float32)
    bd = np.zeros((128, 128), np.float32)
    bd[0:64, 0:64] = 1.0
    bd[64:128, 64:128] = 1.0
    c[:, 128:256] = bd
    c[:, 256:384] = 1.0
    c[:, 384:448] = 1.0
    c[:, 512 + 64:640] = 1.0
    sel = np.zeros((NE, NE, 128), np.float32)
    for e in range(NE):
        sel[e, e, :] = 1.0
    return c, sel


def _bias_table(rpb_l):
    bt = np.full((128, 8, 14, 64), NEG, np.float32)
    qc = np.arange(64)
    cs = np.clip(qc - 8, 0, 48)
    for s in range(2):
        for jj in range(14):
            dr = 13 + s - jj
            if dr < 0 or dr > 14:
                continue
            for kc in range(64):
                valid = (kc >= cs) & (kc < cs + 16)
                dc = kc - qc + 15
                vq = qc[valid]
                bt[s * 64 + kc, :, jj, vq] = rpb_l[:, dr, dc[valid]].T
    return bt.reshape(128, 8, 14 * 64)


def _row_masks(r0):
    rm = np.zeros((128, 10, 6, 4), np.float32)
    for b in range(1, 11):
        r = r0 - 8 + 4 * b
        for qr in range(4):
            q = r + qr
            if q < 0 or q > 255:
                rm[:, b - 1, :, qr] = 1.0
                continue
            rs = min(max(q - 4, 0), 248)
            for c in range(6):
                for s in range(2):
                    a = r - 4 + 2 * c + s
                    if 0 <= a <= 255 and rs <= a < rs + 8:
                        rm[s * 64:(s + 1) * 64, b - 1, c, qr] = 1.0
    return rm


def _rc_tables(core):
    wins = (2, 4, 8, 16)
    rc = np.zeros((128, 2 + 3 * 2 * 16), np.float32)
    rcb = np.zeros((128, 3, 2, 16), np.float32)
    t8 = np.arange(8)
    for g, wd in enumerate(wins):
        c, hh = g // 2, g % 2
        pr = slice(64 * hh, 64 * hh + 64)
        rc[pr, c] = 1.0 / wd
        rcb[pr, :, c, :] = 1.0 / wd
        if core == 0:
            lo = np.clip(t8 - wd // 2, 0, None)
            hi = t8 + wd // 2
            rcb[pr, 0, c, 0:8] = 1.0 / (hi - lo)
        if core == NCORES - 1:
            tt = 16384 - 8 + t8
            lo = tt - wd // 2
            hi = np.clip(tt + wd // 2, None, 16384)
            rcb[pr, 1, c, 8:16] = 1.0 / (hi - lo)
        lo = np.clip(t8 - wd // 2, 0, 256)
        hi = np.clip(t8 + wd // 2, 0, 256)
        rcb[pr, 2, c, 0:8] = 1.0 / (hi - lo)
        tt = 248 + t8
        lo = np.clip(tt - wd // 2, 0, 256)
        hi = np.clip(tt + wd // 2, 0, 256)
        rcb[pr, 2, c, 8:16] = 1.0 / (hi - lo)
    rc[:, 2:] = rcb.reshape(128, -1)
    return rc


def _vecT(v):
    return np.ascontiguousarray(v.reshape(-1, 128).T)


_NC_CACHE = {}


def kernel(x, c, ctx, c_ctx, w_ada, b_ada, norm1, w_in, pool_w, pool_scale, q_norm, k_norm, rpb,
           sg_w, sg_b, sg_norm, w_out, norm2, w_router, b_router, w_gate, w_up, w_down):
    f = lambda a: np.ascontiguousarray(np.asarray(a, dtype=np.float32))
    x, c, ctx, c_ctx = f(x), f(c), f(ctx), f(c_ctx)
    w_ada, b_ada, norm1, w_in, pool_w, pool_scale = f(w_ada), f(b_ada), f(norm1), f(w_in), f(pool_w), f(pool_scale)
    q_norm, k_norm, rpb, sg_w, sg_b, sg_norm = f(q_norm), f(k_norm), f(rpb), f(sg_w), f(sg_b), f(sg_norm)
    w_out, norm2, w_router, b_router, w_gate, w_up, w_down = f(w_out), f(norm2), f(w_router), f(b_router), f(w_gate), f(w_up), f(w_down)

    consts, sel = _consts()
    xg = x[0]
    ctxT = np.ascontiguousarray(ctx[0].T.reshape(KC, 128, BLK))
    cT = np.stack([_vecT(c[0]), _vecT(c_ctx)], axis=-1)
    b_adaT = np.stack([np.ascontiguousarray(b_ada[l].reshape(48, 128).T) for l in range(2)])
    n1T = np.stack([_vecT(norm1[l]) for l in range(2)])
    n2T = np.stack([_vecT(norm2[l]) for l in range(2)])
    pw_bd = np.zeros((2, 128, 2, 128), np.float32)
    for l in range(2):
        for g in range(4):
            cc, hh = g // 2, g % 2
            pw_bd[l, 64 * hh:64 * hh + 64, cc, 64 * hh:64 * hh + 64] = pool_w[l, g]
    pscaleT = np.stack([np.ascontiguousarray(pool_scale[l].reshape(2, 128).T) for l in range(2)])
    qgT = np.stack([np.ascontiguousarray(q_norm[l].reshape(4, 128).T) for l in range(2)])
    kgT = np.stack([np.ascontiguousarray(k_norm[l].reshape(4, 128).T) for l in range(2)])
    bt = np.stack([_bias_table(rpb[l]) for l in range(2)])
    sgwT = np.stack([np.ascontiguousarray(np.transpose(sg_w[l], (2, 0, 1))) for l in range(2)])
    sgbT = np.zeros((2, 128, 2, 128), np.float32)
    for l in range(2):
        for j in range(2):
            for s in range(2):
                sgbT[l, 64 * s:64 * s + 64, j, :] = sg_b[l, 2 * j + s][None, :]
    sggain = np.ascontiguousarray(sg_norm.reshape(2, 256))

    key = "main"
    if key not in _NC_CACHE:
        _NC_CACHE[key] = build_program()
    nc = _NC_CACHE[key]

    in_maps = []
    for core in range(NCORES):
        r0 = 32 * core
        xw = np.zeros((NBL * BLK, D), np.float32)
        ra, rb = r0 - 8, r0 + 40
        va, vb = max(ra, 0), min(rb, 256)
        xw[(va - ra) * 64:(vb - ra) * 64] = xg[va * 64:vb * 64]
        xT = np.ascontiguousarray(xw.T.reshape(KC, 128, NBL * BLK))
        bvalid = np.zeros((128, 13), np.float32)
        for b in range(12):
            r = r0 - 8 + 4 * b
            bvalid[:, b] = 1.0 if 0 <= r <= 252 else 0.0
        bvalid[:, 12] = 1.0
        in_maps.append({
            "xT": xT, "ctxT": ctxT, "cT": cT, "w_ada": w_ada, "b_adaT": b_adaT, "n1T": n1T, "n2T": n2T,
            "w_in": w_in, "pw_bd": pw_bd, "pscaleT": pscaleT, "qgT": qgT, "kgT": kgT, "bt": bt,
            "rm": _row_masks(r0), "rc": _rc_tables(core), "bvalid": bvalid, "sgwT": sgwT, "sgbT": sgbT,
            "sggain": sggain, "w_out": w_out, "w_router": w_router, "b_router": b_router,
            "w_gate": w_gate, "w_up": w_up, "w_down": w_down, "consts": consts, "sel": sel,
        })
    res = run_bass_kernel_spmd(nc, in_maps, core_ids=list(range(NCORES)))
    out = np.zeros((1, 16384, D), np.float32)
    for core in range(NCORES):
        yT = res.results[core]["yT"]
        out[0, core * 2048:(core + 1) * 2048] = yT.reshape(D, 2048).T
    return out
```
